# Optimizing a Trainium2 kernel written in Bass

```python
import math
import jax, jax.numpy as jnp
from jax import lax
import numpy as np

D_MODEL = 1024
BATCH = 2
SEQ = 8192
DEPTH = 2

N_MIXERS = 2
DN_ALPHA = (2 * DEPTH) ** 0.25
DN_BETA = (8 * DEPTH) ** -0.25
LN_EPS = 1e-5
RMS_EPS = 1e-6

NSA_HEADS = 16
NSA_HEAD_DIM = D_MODEL // NSA_HEADS
NSA_KV_GROUPS = 4
NSA_HPG = NSA_HEADS // NSA_KV_GROUPS
NSA_KV_DIM = NSA_KV_GROUPS * NSA_HEAD_DIM
CMP_LEN = 32
CMP_STRIDE = 16
CMP_HIDDEN = 256
SLC_LEN = 64
N_SEL = 16
WINDOW = 512
NSA_QBLOCK = 64
NSA_IN_DIM = D_MODEL + 6 * NSA_KV_DIM + 3 * NSA_HEADS

GLA_HEADS = 4
GLA_KEY_DIM = D_MODEL // 2
GLA_VAL_DIM = D_MODEL
GLA_DK = GLA_KEY_DIM // GLA_HEADS
GLA_DV = GLA_VAL_DIM // GLA_HEADS
GLA_GATE_RANK = 16
GLA_GATE_NORM = 16.0
GLA_CHUNK = 64
GLA_IN_DIM = 2 * GLA_KEY_DIM + 2 * GLA_VAL_DIM + GLA_GATE_RANK

FFN_DENSE = 2816
N_EXPERTS = 8
TOP_K = 2
FFN_EXPERT = 3584

kernel_name = "hybrid_nsa_gla_moe_deepnorm"


def layer_norm(x, g, b):
    xf = x.astype(jnp.float32)
    mu = xf.mean(-1, keepdims=True)
    var = jnp.square(xf - mu).mean(-1, keepdims=True)
    return ((xf - mu) * lax.rsqrt(var + LN_EPS) * g + b).astype(x.dtype)


def alibi_slopes(n):
    return 2.0 ** (-8.0 * (jnp.arange(n, dtype=jnp.float32) + 1.0) / n)


def masked_softmax(s, mask):
    s = jnp.where(mask, s.astype(jnp.float32), -1e30)
    m = s.max(-1, keepdims=True)
    e = jnp.exp(s - m) * mask
    return e / jnp.maximum(e.sum(-1, keepdims=True), 1e-30)


def compress_blocks(blk, pe, w1, w2):
    blk = blk + pe[None, None, :, None, :]
    b, nc, l, g, dk = blk.shape
    flat = blk.transpose(0, 1, 3, 2, 4).reshape(b, nc, g, l * dk)
    return jax.nn.gelu(flat @ w1) @ w2


def nsa_mixer(x, w_in, pe_k, pe_v, wk1, wk2, wv1, wv2, w_o):
    B, T, _ = x.shape
    G, HPG, DK = NSA_KV_GROUPS, NSA_HEAD_DIM and NSA_HPG, NSA_HEAD_DIM
    proj = x @ w_in
    splits = np.cumsum([D_MODEL] + [NSA_KV_DIM] * 6).tolist()
    q, kc, vc, ks, vs, kw, vw, gl = jnp.split(proj, splits, axis=-1)
    q = (q * NSA_HEAD_DIM ** -0.5).reshape(B, T, G, HPG, DK)
    kc, vc, ks, vs, kw, vw = [a.reshape(B, T, G, DK) for a in (kc, vc, ks, vs, kw, vw)]
    gates = jax.nn.sigmoid(gl.astype(jnp.float32)).reshape(B, T, G, HPG, 3)

    n_cmp = (T - CMP_LEN) // CMP_STRIDE + 1
    cmp_idx = np.arange(n_cmp)[:, None] * CMP_STRIDE + np.arange(CMP_LEN)[None, :]
    k_cmp = compress_blocks(kc[:, cmp_idx], pe_k, wk1, wk2)
    v_cmp = compress_blocks(vc[:, cmp_idx], pe_v, wv1, wv2)
    cmp_end = jnp.asarray(cmp_idx[:, -1])

    n_slc = T // SLC_LEN
    n_sel = min(N_SEL, n_slc)
    cs = np.arange(n_cmp) * CMP_STRIDE
    ce = cs + CMP_LEN - 1
    bs = np.arange(n_slc) * SLC_LEN
    be = bs + SLC_LEN - 1
    overlap = jnp.asarray(((cs[:, None] <= be[None]) & (ce[:, None] >= bs[None])).astype(np.float32))
    ks_blk = ks.reshape(B, n_slc, SLC_LEN, G, DK).transpose(0, 3, 1, 2, 4)
    vs_blk = vs.reshape(B, n_slc, SLC_LEN, G, DK).transpose(0, 3, 1, 2, 4)
    slc_start = jnp.arange(n_slc) * SLC_LEN
    jidx = jnp.arange(n_slc)[None, :]

    pad = ((0, 0), (WINDOW, 0), (0, 0), (0, 0))
    kw_pad = jnp.pad(kw, pad)
    vw_pad = jnp.pad(vw, pad)

    slopes = alibi_slopes(NSA_HEADS).reshape(G, HPG)
    bi = jnp.arange(B)[:, None, None, None]
    gi = jnp.arange(G)[None, :, None, None]

    def block(c):
        t0 = c * NSA_QBLOCK
        qb = lax.dynamic_slice_in_dim(q, t0, NSA_QBLOCK, axis=1)
        gb = lax.dynamic_slice_in_dim(gates, t0, NSA_QBLOCK, axis=1)
        tq = t0 + jnp.arange(NSA_QBLOCK)

        dist = tq[:, None] - cmp_end[None, :]
        s = jnp.einsum('bqghd,bngd->bghqn', qb, k_cmp) - slopes[None, :, :, None, None] * dist
        p_cmp = masked_softmax(s, dist >= 0)
        o_cmp = jnp.einsum('bghqn,bngd->bqghd', p_cmp, v_cmp.astype(jnp.float32))

        imp = jnp.einsum('bghqn,nj->bgqj', p_cmp, overlap)
        valid = slc_start[None, :] <= tq[:, None]
        cur = (tq // SLC_LEN)[:, None]
        forced = valid & ((jidx == 0) | (jidx == cur) | (jidx == cur - 1))
        score = jnp.where(forced, 1e9, jnp.where(valid, imp, -1e9))
        _, sel = lax.top_k(score, n_sel)
        kg = ks_blk[bi, gi, sel].reshape(B, G, NSA_QBLOCK, n_sel * SLC_LEN, DK)
        vg = vs_blk[bi, gi, sel].reshape(B, G, NSA_QBLOCK, n_sel * SLC_LEN, DK)
        pos = (sel[..., None] * SLC_LEN + jnp.arange(SLC_LEN)).reshape(B, G, NSA_QBLOCK, n_sel * SLC_LEN)
        dist = tq[None, None, :, None] - pos
        s = jnp.einsum('bqghd,bgqkd->bghqk', qb, kg) - slopes[None, :, :, None, None] * dist[:, :, None]
        p = masked_softmax(s, (dist >= 0)[:, :, None])
        o_slc = jnp.einsum('bghqk,bgqkd->bqghd', p, vg.astype(jnp.float32))

        kwb = lax.dynamic_slice_in_dim(kw_pad, t0, NSA_QBLOCK + WINDOW, axis=1)
        vwb = lax.dynamic_slice_in_dim(vw_pad, t0, NSA_QBLOCK + WINDOW, axis=1)
        kpos = t0 - WINDOW + jnp.arange(NSA_QBLOCK + WINDOW)
        dist = tq[:, None] - kpos[None, :]
        wmask = (dist >= 0) & (dist < WINDOW) & (kpos[None, :] >= 0)
        s = jnp.einsum('bqghd,bkgd->bghqk', qb, kwb) - slopes[None, :, :, None, None] * dist
        p = masked_softmax(s, wmask)
        o_win = jnp.einsum('bghqk,bkgd->bqghd', p, vwb.astype(jnp.float32))

        o = gb[..., 0:1] * o_cmp + gb[..., 1:2] * o_slc + gb[..., 2:3] * o_win
        return o.reshape(B, NSA_QBLOCK, NSA_HEADS * NSA_HEAD_DIM).astype(x.dtype)

    out = lax.map(block, jnp.arange(T // NSA_QBLOCK))
    out = out.transpose(1, 0, 2, 3).reshape(B, T, NSA_HEADS * NSA_HEAD_DIM)
    return out @ w_o


def gla_mixer(x, w_in, w_gate2, b_gate2, head_norm_g, w_o):
    B, T, _ = x.shape
    H, C = GLA_HEADS, GLA_CHUNK
    N = T // C
    proj = x @ w_in
    splits = np.cumsum([GLA_KEY_DIM, GLA_KEY_DIM, GLA_VAL_DIM, GLA_VAL_DIM]).tolist()
    q, k, v, g, a_low = jnp.split(proj, splits, axis=-1)
    log_a = jax.nn.log_sigmoid((a_low @ w_gate2 + b_gate2).astype(jnp.float32)) / GLA_GATE_NORM

    def to_chunks(a, d):
        return a.astype(jnp.float32).reshape(B, N, C, H, d).transpose(1, 0, 3, 2, 4)

    qc = to_chunks(q * GLA_DK ** -0.5, GLA_DK)
    kc = to_chunks(k, GLA_DK)
    vc = to_chunks(v, GLA_DV)
    ac = to_chunks(log_a, GLA_DK)
    causal = jnp.tril(jnp.ones((C, C), dtype=bool))
    ref = C // 2

    def step(S, inp):
        qi, ki, vi, ai = inp
        b = jnp.cumsum(ai, axis=2)
        b_ref = b[:, :, ref:ref + 1]
        b_last = b[:, :, -1:]
        A = jnp.einsum('bhid,bhjd->bhij', qi * jnp.exp(b - b_ref), ki * jnp.exp(b_ref - b))
        A = jnp.where(causal, A, 0.0)
        o = jnp.einsum('bhij,bhje->bhie', A, vi) + jnp.einsum('bhid,bhde->bhie', qi * jnp.exp(b), S)
        S = S * jnp.exp(b_last)[:, :, 0, :, None] + jnp.einsum('bhjd,bhje->bhde', ki * jnp.exp(b_last - b), vi)
        return S, o

    S0 = jnp.zeros((B, H, GLA_DK, GLA_DV), jnp.float32)
    _, o = lax.scan(step, S0, (qc, kc, vc, ac))
    o = o.transpose(1, 0, 3, 2, 4).reshape(B, T, H, GLA_DV)
    o = o * lax.rsqrt(jnp.mean(jnp.square(o), -1, keepdims=True) + RMS_EPS)
    o = o.reshape(B, T, GLA_VAL_DIM) * head_norm_g * jax.nn.silu(g.astype(jnp.float32))
    return o.astype(x.dtype) @ w_o


def swiglu(x, w_gu, w_down):
    a, b = jnp.split(x @ w_gu, 2, axis=-1)
    return (jax.nn.silu(a) * b) @ w_down


def moe_ffn(x, w_router, w_gu, w_down):
    logits = (x @ w_router).astype(jnp.float32)
    top_val, top_idx = lax.top_k(logits, TOP_K)
    w = jax.nn.softmax(top_val, axis=-1)
    gate = jnp.sum(jax.nn.one_hot(top_idx, N_EXPERTS, dtype=jnp.float32) * w[..., None], axis=-2)
    y = jnp.zeros(x.shape, jnp.float32)
    for e in range(N_EXPERTS):
        y = y + gate[..., e:e + 1] * swiglu(x, w_gu[e], w_down[e])
    return y.astype(x.dtype)


def setup_inputs(seed: int = 0) -> dict:
    key = jax.random.key(seed)
    ks = jax.random.split(key, 32)
    nrm = lambda k, shape, scale: jax.random.normal(k, shape, jnp.float32) * scale
    gain = lambda k: 1.0 + nrm(k, (D_MODEL,), 0.02)
    bias = lambda k: nrm(k, (D_MODEL,), 0.02)
    L = CMP_LEN * NSA_HEAD_DIM
    return {
        "x": nrm(ks[0], (BATCH, SEQ, D_MODEL), 1.0),
        "l0_w_in": nrm(ks[1], (D_MODEL, NSA_IN_DIM), D_MODEL ** -0.5),
        "l0_cmp_pe_k": nrm(ks[2], (CMP_LEN, NSA_HEAD_DIM), 0.02),
        "l0_cmp_pe_v": nrm(ks[3], (CMP_LEN, NSA_HEAD_DIM), 0.02),
        "l0_cmp_wk1": nrm(ks[4], (L, CMP_HIDDEN), L ** -0.5),
        "l0_cmp_wk2": nrm(ks[5], (CMP_HIDDEN, NSA_HEAD_DIM), CMP_HIDDEN ** -0.5),
        "l0_cmp_wv1": nrm(ks[6], (L, CMP_HIDDEN), L ** -0.5),
        "l0_cmp_wv2": nrm(ks[7], (CMP_HIDDEN, NSA_HEAD_DIM), CMP_HIDDEN ** -0.5),
        "l0_w_o": nrm(ks[8], (D_MODEL, D_MODEL), DN_BETA * D_MODEL ** -0.5),
        "l0_ln1_g": gain(ks[9]),
        "l0_ln1_b": bias(ks[10]),
        "l0_ffn_w_gu": nrm(ks[11], (D_MODEL, 2 * FFN_DENSE), D_MODEL ** -0.5),
        "l0_ffn_w_down": nrm(ks[12], (FFN_DENSE, D_MODEL), DN_BETA * FFN_DENSE ** -0.5),
        "l0_ln2_g": gain(ks[13]),
        "l0_ln2_b": bias(ks[14]),
        "l1_w_in": nrm(ks[15], (D_MODEL, GLA_IN_DIM), D_MODEL ** -0.5),
        "l1_w_gate2": nrm(ks[16], (GLA_GATE_RANK, GLA_KEY_DIM), GLA_GATE_RANK ** -0.5),
        "l1_b_gate2": nrm(ks[17], (GLA_KEY_DIM,), 0.1),
        "l1_head_norm_g": 1.0 + nrm(ks[18], (GLA_VAL_DIM,), 0.02),
        "l1_w_o": nrm(ks[19], (GLA_VAL_DIM, D_MODEL), DN_BETA * GLA_VAL_DIM ** -0.5),
        "l1_ln1_g": gain(ks[20]),
        "l1_ln1_b": bias(ks[21]),
        "l1_router": nrm(ks[22], (D_MODEL, N_EXPERTS), D_MODEL ** -0.5),
        "l1_moe_w_gu": nrm(ks[23], (N_EXPERTS, D_MODEL, 2 * FFN_EXPERT), D_MODEL ** -0.5),
        "l1_moe_w_down": nrm(ks[24], (N_EXPERTS, FFN_EXPERT, D_MODEL), DN_BETA * FFN_EXPERT ** -0.5),
        "l1_ln2_g": gain(ks[25]),
        "l1_ln2_b": bias(ks[26]),
    }


def reference(x, l0_w_in, l0_cmp_pe_k, l0_cmp_pe_v, l0_cmp_wk1, l0_cmp_wk2, l0_cmp_wv1, l0_cmp_wv2,
              l0_w_o, l0_ln1_g, l0_ln1_b, l0_ffn_w_gu, l0_ffn_w_down, l0_ln2_g, l0_ln2_b,
              l1_w_in, l1_w_gate2, l1_b_gate2, l1_head_norm_g, l1_w_o, l1_ln1_g, l1_ln1_b,
              l1_router, l1_moe_w_gu, l1_moe_w_down, l1_ln2_g, l1_ln2_b):
    token_mixers = [
        lambda h: nsa_mixer(h, l0_w_in, l0_cmp_pe_k, l0_cmp_pe_v, l0_cmp_wk1, l0_cmp_wk2,
                            l0_cmp_wv1, l0_cmp_wv2, l0_w_o),
        lambda h: gla_mixer(h, l1_w_in, l1_w_gate2, l1_b_gate2, l1_head_norm_g, l1_w_o),
    ]
    channel_mixers = [
        lambda h: swiglu(h, l0_ffn_w_gu, l0_ffn_w_down),
        lambda h: moe_ffn(h, l1_router, l1_moe_w_gu, l1_moe_w_down),
    ]
    norms1 = [(l0_ln1_g, l0_ln1_b), (l1_ln1_g, l1_ln1_b)]
    norms2 = [(l0_ln2_g, l0_ln2_b), (l1_ln2_g, l1_ln2_b)]
    for i in range(DEPTH):
        x = layer_norm(DN_ALPHA * x + token_mixers[i % N_MIXERS](x), *norms1[i])
        x = layer_norm(DN_ALPHA * x + channel_mixers[i][(0)] if False else DN_ALPHA * x + channel_mixers[i](x), *norms2[i])
    return x
```

```python
import math
from contextlib import ExitStack

import numpy as np
import concourse.bass as bass
import concourse.mybir as mybir
from concourse.bass_utils import run_bass_kernel_spmd

F32 = mybir.dt.float32
BF16 = mybir.dt.bfloat16
AF = mybir.ActivationFunctionType
ALU = mybir.AluOpType
AX = mybir.AxisListType

D_MODEL = 1024
BATCH = 2
SEQ = 8192
DN_ALPHA = 4 ** 0.25
LN_EPS = 1e-5
RMS_EPS = 1e-6
NCORES = 8
NT = 2048


class Buf:
    __slots__ = ("name", "last_write", "readers")

    def __init__(self, name):
        self.name = name
        self.last_write = None
        self.readers = {}


class SemState:
    def __init__(self, nc):
        self.es = ExitStack()
        self.sem = {}
        self.cnt = {}
        self.dsem = {}
        self.dsem_rr = {}
        for e in KB.ENGS:
            self.sem[e] = self.es.enter_context(nc.semaphore("s_" + e))
            self.cnt[e] = 0
        self.sem["cc"] = self.es.enter_context(nc.semaphore("s_cc"))
        self.cnt["cc"] = 0
        for q, n in (("sp", 16), ("pool", 4), ("act", 2)):
            lst = []
            for i in range(n):
                key = "d_%s_%d" % (q, i)
                self.sem[key] = self.es.enter_context(nc.semaphore(key))
                self.cnt[key] = 0
                lst.append(key)
            self.dsem[q] = lst
            self.dsem_rr[q] = 0


class KB:
    ENGS = ("pe", "act", "dve", "pool", "sp")

    def __init__(self, nc, st=None, prev=(), pfx=""):
        self.nc = nc
        self.pfx = pfx
        self.es = ExitStack()
        self.own_st = st is None
        if st is None:
            st = SemState(nc)
        self.st = st
        self.sem = st.sem
        self.cnt = st.cnt
        self.dsem = st.dsem
        self.dsem_rr = st.dsem_rr
        self.start_val = dict(st.cnt)
        self.waited = {}
        self.prog = {}
        for e in self.ENGS:
            self.waited[e] = {}
            self.prog[e] = []
        for e in self.ENGS:
            waits = self._need(e, list(prev))
            if waits:
                self.prog[e].append((waits, None, None))
        self.n_ins = 0

    def final_tickets(self):
        return [(k, v) for k, v in self.cnt.items() if v > 0]

    def sb(self, name, shape, dt):
        n = 1
        for d in shape[1:]:
            n *= d
        self.sb_bytes = getattr(self, "sb_bytes", 0) + n * (2 if dt == BF16 else 4)
        assert self.sb_bytes <= 178000, ("SBUF budget exceeded", name, self.sb_bytes)
        return self.es.enter_context(self.nc.sbuf_tensor(self.pfx + name, list(shape), dt))

    def ps(self, name, shape, dt=F32):
        return self.es.enter_context(self.nc.psum_tensor(self.pfx + name, list(shape), dt))

    def _need(self, eng, tickets):
        waits = []
        w = self.waited[eng]
        for t in tickets:
            if t is None:
                continue
            key, val = t
            if w.get(key, 0) < val:
                w[key] = val
                waits.append((key, val))
        return waits

    def _deps(self, eng, reads, writes, extra):
        tickets = list(extra)
        for b in reads:
            t = b.last_write
            if t is not None and not (t[0] == eng and eng == "pe"):
                tickets.append(t)
        for b in writes:
            t = b.last_write
            if t is not None and not (t[0] == eng and eng == "pe"):
                tickets.append(t)
            for k, v in b.readers.items():
                if not (k == eng and eng == "pe"):
                    tickets.append((k, v))
        return tickets

    def op(self, eng, fn, reads=(), writes=(), deps=()):
        waits = self._need(eng, self._deps(eng, reads, writes, deps))
        self.cnt[eng] += 1
        t = (eng, self.cnt[eng])
        self.prog[eng].append((waits, fn, (eng, 1)))
        for b in reads:
            b.readers[t[0]] = max(b.readers.get(t[0], 0), t[1])
        for b in writes:
            b.last_write = t
            b.readers = {}
        self.n_ins += 1
        return t

    def dma(self, q, out, in_, reads=(), writes=(), deps=()):
        lst = self.dsem[q]
        key = lst[self.dsem_rr[q] % len(lst)]
        self.dsem_rr[q] += 1
        tickets = self._deps("dma", reads, writes, deps)
        if self.cnt[key] > 0:
            tickets.append((key, self.cnt[key]))
        waits = self._need(q, tickets)
        self.cnt[key] += 16
        t = (key, self.cnt[key])

        def fn(e, out=out, in_=in_):
            return e.dma_start(out=out, in_=in_)

        self.prog[q].append((waits, fn, (key, 16)))
        for b in reads:
            b.readers[t[0]] = max(b.readers.get(t[0], 0), t[1])
        for b in writes:
            b.last_write = t
            b.readers = {}
        self.n_ins += 1
        return t

    def barrier(self):
        tickets = [(e, self.cnt[e]) for e in self.ENGS if self.cnt[e] > 0]
        for q in self.dsem:
            for key in self.dsem[q]:
                if self.cnt[key] > 0:
                    tickets.append((key, self.cnt[key]))
        for e in self.ENGS:
            waits = self._need(e, tickets)
            self.prog[e].append((waits, None, None))

    def collective(self, src, dst, deps=()):
        waits = self._need("pool", list(deps))
        self.cnt["cc"] += 1
        t = ("cc", self.cnt["cc"])

        def fn(e, src=src, dst=dst):
            return e.collective_compute("AllGather", ALU.bypass, replica_groups=[[0, 1, 2, 3], [4, 5, 6, 7]],
                                        ins=[src], outs=[dst])

        self.prog["pool"].append((waits, fn, ("cc", 1)))
        return t

    def wait_all(self, eng, tickets):
        waits = self._need(eng, tickets)
        self.prog[eng].append((waits, None, None))

    def check(self):
        val = dict(self.start_val)
        pc = {e: 0 for e in self.ENGS}
        progress = True
        while progress:
            progress = False
            for e in self.ENGS:
                lst = self.prog[e]
                while pc[e] < len(lst):
                    waits, fn, inc = lst[pc[e]]
                    if any(val[k] < v for k, v in waits):
                        break
                    if inc is not None:
                        val[inc[0]] += inc[1]
                    pc[e] += 1
                    progress = True
        for e in self.ENGS:
            if pc[e] < len(self.prog[e]):
                waits = self.prog[e][pc[e]][0]
                raise RuntimeError("deadlock: engine %s stuck at %d/%d waiting %s (vals %s)" % (
                    e, pc[e], len(self.prog[e]), waits, {k: val[k] for k, _ in waits}))

    def emit(self):
        self.check()
        nc = self.nc
        prog = self.prog
        sem = self.sem

        def run(e, lst):
            for waits, fn, inc in lst:
                for key, val in waits:
                    e.wait_ge(sem[key], val)
                if fn is not None:
                    ins = fn(e)
                    ins.then_inc(sem[inc[0]], inc[1])

        with nc.Block() as block:
            @block.tensor
            def _(e):
                run(e, prog["pe"])

            @block.scalar
            def _(e):
                run(e, prog["act"])

            @block.vector
            def _(e):
                run(e, prog["dve"])

            @block.gpsimd
            def _(e):
                self.freg = {v: e.to_reg(v) for v in (0.0, -30000.0, -1e9)}
                run(e, prog["pool"])

            @block.sync
            def _(e):
                run(e, prog["sp"])
        self.es.close()
        if self.own_st:
            self.st.es.close()


def bcast_rows(ap1d, nparts=128):
    n = ap1d.shape[0]
    return bass.AP(ap1d.tensor, ap1d.offset, [[0, nparts], [1, n]])


def make_ident(kb, name="ident"):
    ident = kb.sb(name, [128, 128], F32)
    b = Buf(name)
    kb.op("pool", lambda e: e.memset(ident[:], 1.0), writes=[b])
    kb.op("pool", lambda e: e.affine_select(out=ident[:], in_=ident[:], pattern=[[-1, 128]],
                                            compare_op=ALU.is_equal, fill=0.0, base=0,
                                            channel_multiplier=1), reads=[b], writes=[b])
    return ident, b


import os as _os
DBG_NOROUTER = 'norouter' in _os.environ.get('MOE_DBG', '')
DBG_H = 'bigh' in _os.environ.get('MOE_DBG', '')
DBG = _os.environ.get('MOE_DBG', '')


class Stager:
    def __init__(self, kb, n, elems, name="stg", aps=None):
        self.kb = kb
        if aps is None:
            aps = [kb.sb("%s%d" % (name, i), [128, elems], F32)[:] for i in range(n)]
        self.t = aps
        self.b = [Buf("%s%d" % (name, i)) for i in range(n)]
        self.i = 0

    def acquire(self):
        i = self.i % len(self.t)
        self.i += 1
        return i, self.t[i], self.b[i]

    def load(self, dst_ap, dst_bufs, src_ap, a, b, eng="pool", dst_reads=()):
        kb = self.kb
        i = self.i % len(self.t)
        self.i += 1
        view = self.t[i][:, 0:a * b].rearrange("p (a b) -> p a b", a=a)
        kb.dma("sp", view, src_ap, writes=[self.b[i]])
        if eng == "act":
            return kb.op("act", lambda e: e.copy(dst_ap, view), reads=[self.b[i]], writes=list(dst_bufs))
        return kb.op(eng, lambda e: e.tensor_copy(dst_ap, view), reads=[self.b[i]], writes=list(dst_bufs))


def layer_norm_tile(kb, h, hb, out, outb, gt, bt, pb, tmp):
    st, stb, mv, mvb, rs, rsb = tmp
    kb.op("dve", lambda e: e.bn_stats(st[:, 0:6], h[:, 0:512]), reads=[hb], writes=[stb])
    kb.op("dve", lambda e: e.bn_stats(st[:, 6:12], h[:, 512:1024]), reads=[hb, stb], writes=[stb])
    kb.op("dve", lambda e: e.bn_aggr(mv[:], st[:]), reads=[stb], writes=[mvb])
    kb.op("act", lambda e: e.activation(out=rs[:], in_=mv[:, 1:2], func=AF.Sqrt, bias=kb.eps_ln[:], scale=1.0),
          reads=[mvb], writes=[rsb])
    kb.op("dve", lambda e: e.reciprocal(rs[:], rs[:]), reads=[rsb], writes=[rsb])
    kb.op("dve", lambda e: e.tensor_scalar(out, h, mv[:, 0:1], rs[:, 0:1], ALU.subtract, ALU.mult),
          reads=[hb, mvb, rsb], writes=[outb])
    kb.op("pool", lambda e: e.tensor_tensor(out, out, gt[:], ALU.mult), reads=[outb, pb], writes=[outb])
    kb.op("pool", lambda e: e.tensor_tensor(out, out, bt[:], ALU.add), reads=[outb, pb], writes=[outb])


def build_ffn_phase(nc, moe, E=None, st=None, prev=(), pfx="", o_cand_fn=None, xres_fn=None, y_fn=None, gather_fn=None):
    if E is None:
        E = 8 if moe else 1
    H = 3584 if (moe or DBG_H) else 2816
    GB = 2
    NG = (H // 128) // GB
    HG = GB * 128
    NTT = NT // 128
    NCH = NT // 512

    def din(name, shape):
        return nc.dram_tensor(pfx + name, list(shape), F32, kind="ExternalInput").ap()

    fusedm = o_cand_fn is not None
    oT = din("oT", [D_MODEL, NT]) if not fusedm else None
    sel_in = din("sel4", [128, 4]) if fusedm else None
    xres = din("xres", [NT, D_MODEL]) if xres_fn is None else None
    w_o = din("w_o", [D_MODEL, D_MODEL])
    ln1_g = din("ln1_g", [D_MODEL]); ln1_b = din("ln1_b", [D_MODEL])
    ln2_g = din("ln2_g", [D_MODEL]); ln2_b = din("ln2_b", [D_MODEL])
    w_gu = din("w_gu", [E, D_MODEL, 2 * H])
    w_dn = din("w_dn", [E, H, D_MODEL])
    if moe:
        w_r = din("w_r", [D_MODEL, 8])
    y = nc.dram_tensor("y", [NT, D_MODEL], F32, kind="ExternalOutput").ap() if y_fn is None else None

    kb = KB(nc, st, prev, pfx)
    ident, identb = make_ident(kb)
    kb.eps_ln = kb.sb("eps_ln", [128, 1], F32)
    epsb = Buf("eps")
    kb.op("pool", lambda e: e.memset(kb.eps_ln[:], LN_EPS), writes=[epsb])

    acc = kb.sb("acc", [128, NTT, D_MODEL], F32)
    accb = [Buf("acc%d" % i) for i in range(NTT)]
    h1T = kb.sb("h1T", [128, 8, NT], BF16)
    h1Tb = [Buf("h1T%d" % i) for i in range(NTT)]
    stg = Stager(kb, 2, 2048)
    SLOT = 3 * 2048
    wbuf = kb.sb("wbuf", [128, 2 * SLOT], BF16)
    gt = kb.sb("gt", [128, D_MODEL], F32); bt = kb.sb("bt", [128, D_MODEL], F32)
    pb = Buf("lnparams")
    if fusedm:
        xr = [kb.sb("xr0", [128, D_MODEL], F32)] * 2
        xrb = [Buf("xr0")] * 2
        cand = kb.sb("cand", [128, D_MODEL], F32); candb = Buf("cand")
        osel = kb.sb("osel", [128, D_MODEL], F32); oselb = Buf("osel")
        oT_t = kb.sb("oT_t", [128, 8, 128], BF16); oT_tb = Buf("oT_t")
        sel = kb.sb("sel", [128, 4], F32); selb = Buf("sel")
    else:
        xr = [kb.sb("xr%d" % i, [128, D_MODEL], F32) for i in range(2)]
        xrb = [Buf("xr%d" % i) for i in range(2)]
    h1 = [kb.sb("h1_0", [128, D_MODEL], F32)] * 2
    h1b = [Buf("h1_0")] * 2
    st = kb.sb("st", [128, 12], F32); stb = Buf("st")
    mv = kb.sb("mv", [128, 2], F32); mvb = Buf("mv")
    rs = kb.sb("rs", [128, 1], F32); rsb = Buf("rs")
    lntmp = (st, stb, mv, mvb, rs, rsb)
    sil = [kb.sb("sil%d" % i, [128, 512], BF16) for i in range(2)]
    silb = [Buf("sil%d" % i) for i in range(2)]
    aT = [kb.sb("aT%d" % i, [128, GB, 512], BF16) for i in range(2)]
    aTb = [[Buf("aT%d_%d" % (i, j)) for j in range(GB)] for i in range(2)]
    if moe:
        gate = kb.sb("gate", [128, NTT, 8], F32)
        gateb = [Buf("gate%d" % i) for i in range(NTT)]
        wr_sb = kb.sb("wr_sb", [128, 8, 8], F32); wrb = Buf("wr")
        h1Tf = [kb.sb("h1Tf0", [128, 8, 128], F32)] * 2
        h1Tfb = [Buf("h1Tf0")] * 2
        lg = kb.sb("lg", [128, 8], F32); lgb = Buf("lg")
        mx8 = kb.sb("mx8", [128, 8], F32); mx8b = Buf("mx8")
        rt = kb.sb("rt", [128, 4], F32); rtb = Buf("rt")
        ex8 = kb.sb("ex8", [128, 8], F32); ex8b = Buf("ex8")

    pg = [kb.ps("pg%d" % i, [128, 512]) for i in range(2)]; pgb = [Buf("pg%d" % i) for i in range(2)]
    pu = [kb.ps("pu%d" % i, [128, 512]) for i in range(2)]; pub = [Buf("pu%d" % i) for i in range(2)]
    pd = [kb.ps("pd%d" % i, [128, 512]) for i in range(2)]; pdb = [Buf("pd%d" % i) for i in range(2)]
    pt = [kb.ps("pt%d" % i, [128, 512]) for i in range(2)]; ptb = [Buf("pt%d" % i) for i in range(2)]

    kb.dma("sp", gt[:], bcast_rows(ln1_g), writes=[pb])
    kb.dma("sp", bt[:], bcast_rows(ln1_b), writes=[pb])
    if moe and 'nowr' not in DBG:
        kb.dma("sp", wr_sb[:], w_r.rearrange("(kt p) e -> p kt e", p=128), writes=[wrb])
    wo_sb = wbuf[:, 0:8192].rearrange("p (kt n) -> p kt n", kt=8)
    wob = Buf("wo")
    w_o_v = w_o.rearrange("(kt p) n -> p kt n", p=128)
    for i in range(4):
        stg.load(wo_sb[:, 2 * i:2 * i + 2, :], [wob], w_o_v[:, 2 * i:2 * i + 2, :], 2, 1024)
    oT_sb = wbuf[:, 8192:12288].rearrange("p (kt t) -> p kt t", kt=8)
    oTb = Buf("oTs")
    wslot_b = [Buf("wslot0"), Buf("wslot1")]

    if fusedm:
        kb.dma("sp", sel[:], sel_in, writes=[selb])
    else:
        oT_v = oT.rearrange("(kt p) t -> p kt t", p=128)
    xres_v = xres.rearrange("(tt p) d -> tt p d", p=128) if xres is not None else None
    y_v = y.rearrange("(tt p) d -> tt p d", p=128) if y is not None else None

    evac_i = 0
    for c in range(NCH):
        if not fusedm:
            for i in range(2):
                stg.load(oT_sb[:, 4 * i:4 * i + 4, :], [oTb], oT_v[:, 4 * i:4 * i + 4, c * 512:(c + 1) * 512], 4, 512)
        for tl in range(4):
            tt = c * 4 + tl
            r = tt % 2
            if fusedm:
                for rk in range(4):
                    kb.dma("sp", cand[:].rearrange("p (g f) -> p g f", g=4), o_cand_fn(rk, tt), writes=[candb])
                    if rk == 0:
                        kb.op("dve", lambda e: e.tensor_scalar(osel[:], cand[:], sel[:, 0:1], None, ALU.mult),
                              reads=[candb, selb], writes=[oselb])
                    else:
                        kb.op("dve", lambda e, rk=rk: e.scalar_tensor_tensor(
                            out=osel[:], in0=cand[:], scalar=sel[:, rk:rk + 1], in1=osel[:], op0=ALU.mult, op1=ALU.add),
                            reads=[candb, selb, oselb], writes=[oselb])
                for half in range(2):
                    for j in range(4):
                        kt = half * 4 + j
                        kb.op("pe", lambda e, half=half, kt=kt, j=j: e.transpose(
                            pt[half][:, j * 128:(j + 1) * 128], osel[:, kt * 128:(kt + 1) * 128], ident[:]),
                            reads=[oselb, identb], writes=[ptb[half]])
                    kb.op("act", lambda e, half=half: e.copy(
                        oT_t[:, half * 4:(half + 1) * 4, :], pt[half][:].rearrange("p (j t) -> p j t", j=4)),
                        reads=[ptb[half]], writes=[oT_tb])
            kb.dma("sp", xr[r][:], xres_v[tt] if xres_fn is None else xres_fn(tt), writes=[xrb[r]])
            for nh in range(2):
                pi = evac_i % 2
                evac_i += 1
                for kt in range(8):
                    if fusedm:
                        kb.op("pe", lambda e, pi=pi, kt=kt, nh=nh: e.matmul(
                            pd[pi][:], oT_t[:, kt, :],
                            wo_sb[:, kt, nh * 512:(nh + 1) * 512], start=(kt == 0), stop=(kt == 7)),
                            reads=[oT_tb, wob], writes=[pdb[pi]])
                    else:
                        kb.op("pe", lambda e, pi=pi, kt=kt, tl=tl, nh=nh: e.matmul(
                            pd[pi][:], oT_sb[:, kt, tl * 128:(tl + 1) * 128],
                            wo_sb[:, kt, nh * 512:(nh + 1) * 512], start=(kt == 0), stop=(kt == 7)),
                            reads=[oTb, wob], writes=[pdb[pi]])
                kb.op("dve", lambda e, pi=pi, r=r, nh=nh: e.scalar_tensor_tensor(
                    out=xr[r][:, nh * 512:(nh + 1) * 512], in0=xr[r][:, nh * 512:(nh + 1) * 512],
                    scalar=DN_ALPHA, in1=pd[pi][:], op0=ALU.mult, op1=ALU.add),
                    reads=[xrb[r], pdb[pi]], writes=[xrb[r]])
            layer_norm_tile(kb, xr[r][:], xrb[r], h1[r][:], h1b[r], gt, bt, pb, lntmp)
            kb.op("act", lambda e, tt=tt, r=r: e.mul(acc[:, tt, :], h1[r][:], DN_ALPHA),
                  reads=[h1b[r]], writes=[accb[tt]])
            for half in range(2):
                pi = half
                for j in range(4):
                    kt = half * 4 + j
                    kb.op("pe", lambda e, pi=pi, r=r, kt=kt, j=j: e.transpose(
                        pt[pi][:, j * 128:(j + 1) * 128], h1[r][:, kt * 128:(kt + 1) * 128], ident[:]),
                        reads=[h1b[r], identb], writes=[ptb[pi]])
                kb.op("act", lambda e, pi=pi, tt=tt, half=half: e.copy(
                    h1T[:, half * 4:(half + 1) * 4, tt * 128:(tt + 1) * 128],
                    pt[pi][:].rearrange("p (j t) -> p j t", j=4)),
                    reads=[ptb[pi]], writes=[h1Tb[tt]])
                if moe and 'noh1tf' not in DBG:
                    kb.op("act", lambda e, pi=pi, r=r, half=half: e.copy(
                        h1Tf[r][:, half * 4:(half + 1) * 4, :],
                        pt[pi][:].rearrange("p (j t) -> p j t", j=4)),
                        reads=[ptb[pi]], writes=[h1Tfb[r]])
            if moe and DBG_NOROUTER:
                kb.op("pool", lambda e, tt=tt: e.memset(gate[:, tt, :], 0.5), writes=[gateb[tt]])
            if moe and not DBG_NOROUTER:
                for kt in range(8):
                    kb.op("pe", lambda e, r=r, kt=kt: e.matmul(
                        pg[0][:, 0:8], h1Tf[r][:, kt, :], wr_sb[:, kt, :], start=(kt == 0), stop=(kt == 7)),
                        reads=[h1Tfb[r], wrb], writes=[pgb[0]])
                kb.op("dve", lambda e: e.tensor_copy(lg[:], pg[0][:, 0:8]), reads=[pgb[0]], writes=[lgb])
                kb.op("dve", lambda e: e.max(mx8[:], lg[:]), reads=[lgb], writes=[mx8b])
                kb.op("dve", lambda e: e.tensor_scalar(rt[:, 0:1], mx8[:, 0:1], -1.0, None, ALU.mult),
                      reads=[mx8b], writes=[rtb])
                kb.op("act", lambda e: e.activation(out=ex8[:], in_=lg[:], func=AF.Exp, bias=rt[:, 0:1], scale=1.0),
                      reads=[lgb, rtb], writes=[ex8b])
                kb.op("act", lambda e: e.activation(out=rt[:, 1:2], in_=mx8[:, 1:2], func=AF.Exp, bias=rt[:, 0:1], scale=1.0),
                      reads=[mx8b, rtb], writes=[rtb])
                kb.op("dve", lambda e: e.tensor_scalar(rt[:, 2:3], rt[:, 1:2], 1.0, None, ALU.add),
                      reads=[rtb], writes=[rtb])
                kb.op("dve", lambda e: e.reciprocal(rt[:, 2:3], rt[:, 2:3]), reads=[rtb], writes=[rtb])
                kb.op("dve", lambda e: e.tensor_scalar(ex8[:], ex8[:], rt[:, 2:3], None, ALU.mult),
                      reads=[ex8b, rtb], writes=[ex8b])
                kb.op("dve", lambda e, tt=tt: e.scalar_tensor_tensor(
                    out=gate[:, tt, :], in0=lg[:], scalar=mx8[:, 1:2], in1=ex8[:], op0=ALU.is_ge, op1=ALU.mult),
                    reads=[lgb, mx8b, ex8b], writes=[gateb[tt]])

    kb.dma("sp", gt[:], bcast_rows(ln2_g), writes=[pb])
    kb.dma("sp", bt[:], bcast_rows(ln2_b), writes=[pb])

    w_gu_v = w_gu.rearrange("e (kt p) n -> e p kt n", p=128)
    w_dn_v = w_dn.rearrange("e (hb p) n -> e p hb n", p=128)
    gi = 0
    gu_i = 0
    d_i = 0
    sil_i = 0
    for e_ in range(E):
        for g in range(NG):
            sl = gi % 2
            gi += 1
            base = sl * SLOT
            wg_sb = wbuf[:, base:base + 2048].rearrange("p (kt n) -> p kt n", kt=8)
            wu_sb = wbuf[:, base + 2048:base + 4096].rearrange("p (kt n) -> p kt n", kt=8)
            wd_sb = wbuf[:, base + 4096:base + 6144].rearrange("p (hb n) -> p hb n", hb=GB)
            h0 = g * HG
            wb = wslot_b[sl]
            extra = [wob, oTb] if gi <= 2 else []
            stg.load(wg_sb, [wb] + extra, w_gu_v[e_, :, :, h0:h0 + HG], 8, HG)
            stg.load(wu_sb, [wb], w_gu_v[e_, :, :, H + h0:H + h0 + HG], 8, HG)
            stg.load(wd_sb, [wb], w_dn_v[e_, :, g * GB:(g + 1) * GB, :], GB, 1024)
            for c in range(NCH):
                ab = c % 2
                for blk in range(GB):
                    pi = gu_i % 2
                    gu_i += 1
                    for kt in range(8):
                        kb.op("pe", lambda e, pi=pi, kt=kt, blk=blk, c=c, wg_sb=wg_sb: e.matmul(
                            pg[pi][:], wg_sb[:, kt, blk * 128:(blk + 1) * 128],
                            h1T[:, kt, c * 512:(c + 1) * 512], start=(kt == 0), stop=(kt == 7)),
                            reads=[wb] + h1Tb[c * 4:(c + 1) * 4], writes=[pgb[pi]])
                    for kt in range(8):
                        kb.op("pe", lambda e, pi=pi, kt=kt, blk=blk, c=c, wu_sb=wu_sb: e.matmul(
                            pu[pi][:], wu_sb[:, kt, blk * 128:(blk + 1) * 128],
                            h1T[:, kt, c * 512:(c + 1) * 512], start=(kt == 0), stop=(kt == 7)),
                            reads=[wb], writes=[pub[pi]])
                    si = sil_i % 2
                    sil_i += 1
                    kb.op("act", lambda e, pi=pi, si=si: e.activation(out=sil[si][:], in_=pg[pi][:], func=AF.Silu),
                          reads=[pgb[pi]], writes=[silb[si]])
                    kb.op("dve", lambda e, pi=pi, si=si, ab=ab, blk=blk: e.tensor_tensor(
                        aT[ab][:, blk, :], pu[pi][:], sil[si][:], ALU.mult),
                        reads=[pub[pi], silb[si]], writes=[aTb[ab][blk]])
                for tl in range(4):
                    tt = c * 4 + tl
                    for nh in range(2):
                        pi = d_i % 2
                        d_i += 1
                        for blk in range(GB):
                            kb.op("pe", lambda e, pi=pi, ab=ab, blk=blk, tl=tl, nh=nh, wd_sb=wd_sb: e.matmul(
                                pd[pi][:], aT[ab][:, blk, tl * 128:(tl + 1) * 128],
                                wd_sb[:, blk, nh * 512:(nh + 1) * 512], start=(blk == 0), stop=(blk == GB - 1)),
                                reads=[aTb[ab][blk], wb], writes=[pdb[pi]])
                        if moe and 'nostt' not in DBG:
                            kb.op("dve", lambda e, pi=pi, tt=tt, nh=nh, e_=e_: e.scalar_tensor_tensor(
                                out=acc[:, tt, nh * 512:(nh + 1) * 512], in0=pd[pi][:],
                                scalar=gate[:, tt, e_:e_ + 1], in1=acc[:, tt, nh * 512:(nh + 1) * 512],
                                op0=ALU.mult, op1=ALU.add),
                                reads=[pdb[pi], gateb[tt], accb[tt]], writes=[accb[tt]])
                        else:
                            kb.op("dve", lambda e, pi=pi, tt=tt, nh=nh: e.tensor_tensor(
                                acc[:, tt, nh * 512:(nh + 1) * 512], pd[pi][:],
                                acc[:, tt, nh * 512:(nh + 1) * 512], ALU.add),
                                reads=[pdb[pi], accb[tt]], writes=[accb[tt]])

    outs = []
    for tt in range(NTT):
        r = tt % 2
        layer_norm_tile(kb, acc[:, tt, :], accb[tt], xr[r][:], xrb[r], gt, bt, pb, lntmp)
        outs.append(kb.dma("sp", y_v[tt] if y_fn is None else y_fn(tt), xr[r][:], reads=[xrb[r]]))
        if gather_fn is not None and gather_fn(tt) is not None:
            gs_, gd_ = gather_fn(tt)
            outs.append(kb.collective(gs_, gd_, deps=outs[-2:]))
    kb.wait_all("sp", outs)
    tickets = kb.final_tickets()
    kb.emit()
    return tickets


def build_nsa_phase(nc, st=None, prev=(), pfx="", o_dst_fn=None, gather_fn=None):
    T = SEQ
    ONLY = _os.environ.get('NSA_DBG', '')
    NEG = -30000.0

    def din(name, shape):
        return nc.dram_tensor(pfx + name, list(shape), F32, kind="ExternalInput").ap()

    xT = din("xT", [1024, T])
    wall = din("wall", [1024, 780])
    w1 = din("w1", [2, 2048, 256])
    peT = din("peT", [128, 32])
    w2k = din("w2k", [256, 128])
    w2v = din("w2v", [256, 64])
    ov = din("ov", [512, 128])
    dslc = din("dslc", [128, 512])
    dcmp = din("dcmp", [128, 512])
    nslope = din("nslope", [128, 4])
    relslc = din("relslc", [128, 64])
    relcmp = din("relcmp", [128, 16])
    o_out = None if o_dst_fn is not None else nc.dram_tensor("o", [T, 256], F32, kind="ExternalOutput").ap()

    kb = KB(nc, st, prev, pfx)
    ident, identb = make_ident(kb)

    QT = [kb.sb("QT%d" % i, [128, T], BF16) for i in range(2)]
    KsT2 = kb.sb("KsT2", [128, T], BF16)
    KwT2 = kb.sb("KwT2", [128, T], BF16)
    Vs1 = kb.sb("Vs1", [128, 64, 65], BF16)
    Vw1 = kb.sb("Vw1", [128, 64, 65], BF16)
    gates = kb.sb("gates", [128, 64, 12], F32)
    KcT2 = kb.sb("KcT2", [128, 512], BF16)
    VcX = kb.sb("VcX", [128, 4, 193], BF16)
    nsl = kb.sb("nsl", [128, 4], F32)
    misc = kb.sb("misc", [128, 512], F32)
    cpe = kb.sb("cpe", [128, 4], F32)
    QTb = Buf("QT"); KsTb = Buf("KsT"); KwTb = Buf("KwT"); Vsb = Buf("Vs"); Vwb = Buf("Vw")
    gatesb = Buf("gates"); KcTb = Buf("KcT"); VcXb = Buf("VcX"); nslb = Buf("nsl"); miscb = Buf("misc")
    cpeb = Buf("cpe")

    OVLW = 20800
    ovl = kb.sb("ovl", [128, OVLW], F32)
    off = [0]

    def carve(nwords):
        a = off[0]
        off[0] += nwords
        assert off[0] <= OVLW, off[0]
        return ovl[:, a:a + nwords]

    bk = [kb.ps("bk%d" % i, [128, 512]) for i in range(8)]
    bkb = [Buf("bk%d" % i) for i in range(8)]

    w1_sb = carve(4096).bitcast(BF16).rearrange("p (l h) -> p l h", l=32)
    xbf = [carve(1024).bitcast(BF16).rearrange("p (kt t) -> p kt t", kt=8) for _ in range(2)]
    xbfb = [Buf("xbf0"), Buf("xbf1")]
    KVcT = carve(4096).bitcast(BF16)
    KVcTb = Buf("KVcT")
    wall_sb = carve(3120).bitcast(BF16).rearrange("p (kt n) -> p kt n", kt=8)
    wallb = Buf("wall")
    w1b = Buf("w1")
    stg_aps = [carve(2048) for _ in range(2)]
    stg = Stager(kb, 2, 2048, aps=stg_aps)
    u_t = [carve(512) for _ in range(4)]
    ub = [Buf("u%d" % i) for i in range(4)]
    hidT = carve(1024).bitcast(BF16).rearrange("p (s n) -> p s n", s=4)
    hidTb = Buf("hidT")
    peT_sb = carve(16).bitcast(BF16)
    w2k_sb = carve(128).bitcast(BF16).rearrange("p (hh n) -> p hh n", hh=2)
    w2v_sb = carve(64).bitcast(BF16).rearrange("p (hh n) -> p hh n", hh=2)
    smallb = Buf("small")

    kb.dma("sp", nsl[:], nslope, writes=[nslb])
    wall_v = wall.rearrange("(kt p) n -> p kt n", p=128)
    for i in range(4):
        stg.load(wall_sb[:, 2 * i:2 * i + 2, :], [wallb], wall_v[:, 2 * i:2 * i + 2, :], 2, 780)
    kb.op("pool", lambda e: e.memset(Vs1[:, :, 64:65], 1.0), writes=[Vsb])
    kb.op("pool", lambda e: e.memset(Vw1[:, :, 64:65], 1.0), writes=[Vwb])
    kb.op("pool", lambda e: e.memset(VcX[:, :, 64:65], 1.0), writes=[VcXb])
    kb.op("pool", lambda e: e.memset(KcT2[:], 0.0), writes=[KcTb])
    kb.op("pool", lambda e: e.memset(hidT, 0.0), writes=[hidTb])

    xT_v = xT.rearrange("(kt p) t -> p kt t", p=128)
    ev = 0
    for c in range(32):
        xs = c % 2
        tok = slice(c * 256, (c + 1) * 256)
        stg.load(xbf[xs], [xbfb[xs]], xT_v[:, :, tok], 8, 256)
        for oi, (col0, dst, dstb, scale) in enumerate((
                (0, QT[0], QTb, 0.125), (128, QT[1], QTb, 0.125), (256, KVcT, KVcTb, 1.0),
                (384, KsT2, KsTb, 1.0), (512, KwT2, KwTb, 1.0))):
            pi = ev % 2
            ev += 1
            for kt in range(8):
                kb.op("pe", lambda e, pi=pi, kt=kt, xs=xs, col0=col0: e.matmul(
                    bk[pi][:, 0:256], wall_sb[:, kt, col0:col0 + 128], xbf[xs][:, kt, :],
                    start=(kt == 0), stop=(kt == 7)), reads=[wallb, xbfb[xs]], writes=[bkb[pi]])
            if oi % 2 == 0:
                kb.op("act", lambda e, pi=pi, dst=dst, tok=tok, scale=scale: e.mul(dst[:, tok], bk[pi][:, 0:256], scale),
                      reads=[bkb[pi]], writes=[dstb])
            else:
                kb.op("dve", lambda e, pi=pi, dst=dst, tok=tok, scale=scale: e.tensor_scalar(
                    dst[:, tok], bk[pi][:, 0:256], scale, None, ALU.mult), reads=[bkb[pi]], writes=[dstb])
        for tl in range(2):
            tix = c * 2 + tl
            pi = 2 + tl
            for kt in range(8):
                kb.op("pe", lambda e, pi=pi, kt=kt, xs=xs, tl=tl: e.matmul(
                    bk[pi][:, 0:140], xbf[xs][:, kt, tl * 128:(tl + 1) * 128], wall_sb[:, kt, 640:780],
                    start=(kt == 0), stop=(kt == 7)), reads=[wallb, xbfb[xs]], writes=[bkb[pi]])
            kb.op("dve", lambda e, pi=pi, tix=tix: e.tensor_copy(Vs1[:, tix, 0:64], bk[pi][:, 0:64]),
                  reads=[bkb[pi]], writes=[Vsb])
            kb.op("dve", lambda e, pi=pi, tix=tix: e.tensor_copy(Vw1[:, tix, 0:64], bk[pi][:, 64:128]),
                  reads=[bkb[pi]], writes=[Vwb])
            kb.op("dve", lambda e, pi=pi, tix=tix: e.tensor_copy(gates[:, tix, :], bk[pi][:, 128:140]),
                  reads=[bkb[pi]], writes=[gatesb])
            kb.op("act", lambda e, tix=tix: e.activation(out=gates[:, tix, :], in_=gates[:, tix, :],
                                                         func=AF.Sigmoid), reads=[gatesb], writes=[gatesb])

    for i in range(4):
        si, sview, sbuf_ = stg.acquire()
        v3 = sview[:, 0:2048].rearrange("p (l h) -> p l h", l=8)
        for s in range(2):
            kb.dma("sp", v3[s * 64:(s + 1) * 64], w1[s].rearrange("(l d) h -> d l h", d=64)[:, 8 * i:8 * i + 8, :],
                   writes=[sbuf_])
        kb.op("pool", lambda e, i=i, v3=v3: e.tensor_copy(w1_sb[:, 8 * i:8 * i + 8, :], v3), reads=[sbuf_], writes=[w1b])
    kb.dma("sp", misc[:, 0:32], peT, writes=[miscb])
    kb.op("pool", lambda e: e.tensor_copy(peT_sb, misc[:, 0:32]), reads=[miscb], writes=[smallb])
    kb.dma("sp", misc[:, 0:256].rearrange("p (hh n) -> p hh n", hh=2), w2k.rearrange("(hh p) n -> p hh n", p=128),
           writes=[miscb])
    kb.op("pool", lambda e: e.tensor_copy(w2k_sb, misc[:, 0:256].rearrange("p (hh n) -> p hh n", hh=2)),
          reads=[miscb], writes=[smallb])
    kb.dma("sp", misc[:, 0:128].rearrange("p (hh n) -> p hh n", hh=2), w2v.rearrange("(hh p) n -> p hh n", p=128),
           writes=[miscb])
    kb.op("pool", lambda e: e.tensor_copy(w2v_sb, misc[:, 0:128].rearrange("p (hh n) -> p hh n", hh=2)),
          reads=[miscb], writes=[smallb])
    kb.dma("sp", misc[:, 0:512].rearrange("p (i j) -> p i j", i=4), ov.rearrange("(i p) j -> p i j", p=128),
           writes=[miscb])
    kb.op("pool", lambda e: e.tensor_copy(VcX[:, :, 65:193], misc[:, 0:512].rearrange("p (i j) -> p i j", i=4)),
          reads=[miscb], writes=[VcXb])

    for s in range(2):
        ps_ = slice(s * 64, (s + 1) * 64)
        for hh in range(2):
            idx = s * 2 + hh
            ph = 4 + hh
            for l in range(32):
                kb.op("pe", lambda e, ph=ph, l=l, hh=hh, ps_=ps_: e.matmul(
                    bk[ph][:, 0:511], w1_sb[ps_, l, hh * 128:(hh + 1) * 128], KVcT[ps_, l:l + 8161:16],
                    start=(l == 0), stop=(l == 31)), reads=[w1b, KVcTb], writes=[bkb[ph]])
            for l in range(32):
                kb.op("pe", lambda e, l=l, hh=hh, ps_=ps_: e.matmul(
                    bk[6][:, 0:1], w1_sb[ps_, l, hh * 128:(hh + 1) * 128], peT_sb[ps_, l:l + 1],
                    start=(l == 0), stop=(l == 31)), reads=[w1b, smallb], writes=[bkb[6]])
            kb.op("dve", lambda e, idx=idx: e.tensor_copy(cpe[:, idx:idx + 1], bk[6][:, 0:1]), reads=[bkb[6]], writes=[cpeb])
            u, u2, w_, sg = u_t
            kb.op("dve", lambda e, ph=ph, idx=idx, u=u: e.tensor_scalar(u[:, 0:511], bk[ph][:, 0:511], cpe[:, idx:idx + 1], None, ALU.add),
                  reads=[bkb[ph], cpeb], writes=[ub[0]])
            kb.op("dve", lambda e, u=u, u2=u2: e.tensor_tensor(u2[:, 0:511], u[:, 0:511], u[:, 0:511], ALU.mult),
                  reads=[ub[0]], writes=[ub[1]])
            kb.op("dve", lambda e, u2=u2: e.tensor_scalar(u2[:, 0:511], u2[:, 0:511], 0.044715, 1.0, ALU.mult, ALU.add),
                  reads=[ub[1]], writes=[ub[1]])
            kb.op("dve", lambda e, u=u, u2=u2, w_=w_: e.tensor_tensor(w_[:, 0:511], u2[:, 0:511], u[:, 0:511], ALU.mult),
                  reads=[ub[0], ub[1]], writes=[ub[2]])
            kb.op("act", lambda e, w_=w_, sg=sg: e.activation(out=sg[:, 0:511], in_=w_[:, 0:511], func=AF.Sigmoid,
                                                              scale=1.5957691216057308), reads=[ub[2]], writes=[ub[3]])
            kb.op("dve", lambda e, idx=idx, u=u, sg=sg: e.tensor_tensor(hidT[:, idx, 0:511], u[:, 0:511], sg[:, 0:511], ALU.mult),
                  reads=[ub[0], ub[3]], writes=[hidTb])
    for hh in range(2):
        kb.op("pe", lambda e, hh=hh: e.matmul(bk[7][:, 0:511], w2k_sb[:, hh, :], hidT[:, hh, 0:511],
                                              start=(hh == 0), stop=(hh == 1)), reads=[smallb, hidTb], writes=[bkb[7]])
    kb.op("act", lambda e: e.copy(KcT2[:, 0:511], bk[7][:, 0:511]), reads=[bkb[7]], writes=[KcTb])
    for i in range(4):
        for hh in range(2):
            kb.op("pe", lambda e, hh=hh, i=i: e.matmul(bk[6][:, i * 64:(i + 1) * 64], hidT[:, 2 + hh, i * 128:(i + 1) * 128],
                                                       w2v_sb[:, hh, :], start=(hh == 0), stop=(hh == 1)),
                  reads=[smallb, hidTb], writes=[bkb[6]])
    kb.op("dve", lambda e: e.tensor_copy(VcX[:, :, 0:64], bk[6][:, 0:256].rearrange("p (i d) -> p i d", i=4)),
          reads=[bkb[6]], writes=[VcXb])

    kb.barrier()
    off[0] = 0
    Ebig = carve(4096).bitcast(BF16)
    negselT = carve(4096).bitcast(BF16)
    Bs = [carve(512) for _ in range(4)]
    Bc = [carve(512) for _ in range(4)]
    cbs = carve(256).rearrange("p (h m) -> p h m", h=4)
    cbc = carve(64).rearrange("p (h m) -> p h m", h=4)
    tt_ = [carve(512) for _ in range(3)]
    ttb = [Buf("t%d" % i) for i in range(3)]
    scr = tt_[0]
    dtab = tt_[1]
    rtab = tt_[2]
    pT = [carve(256).bitcast(BF16) for _ in range(3)]
    pTb = [Buf("pT%d" % i) for i in range(3)]
    oacc = [carve(1024).rearrange("p (q f) -> p q f", q=4) for _ in range(2)]
    oaccb = [Buf("oacc0"), Buf("oacc1")]
    imp = carve(512).rearrange("p (q j) -> p q j", q=4)
    impb = Buf("imp")
    sc = carve(512).rearrange("p (q j) -> p q j", q=4)
    sc2 = carve(512).rearrange("p (q j) -> p q j", q=4)
    nsel = carve(512).rearrange("p (q j) -> p q j", q=4)
    scb = [Buf("sc%d" % i) for i in range(4)]
    sc2b = [Buf("sc2%d" % i) for i in range(4)]
    nselb = [Buf("nsel%d" % i) for i in range(4)]
    mx = carve(64).rearrange("p (q j) -> p q j", q=4)
    mxb = [Buf("mx%d" % i) for i in range(4)]
    rd = carve(8)
    rdb = Buf("rd")
    constb = Buf("const")
    Eb = Buf("Ebig")
    nsTb = Buf("negselT")

    kb.dma("sp", dtab, dslc, writes=[ttb[1]])
    for h in range(4):
        kb.op("dve", lambda e, h=h: e.tensor_scalar(Bs[h], dtab, nsl[:, h:h + 1], None, ALU.mult),
              reads=[ttb[1], nslb], writes=[constb])
    kb.dma("sp", dtab, dcmp, writes=[ttb[1]])
    for h in range(4):
        kb.op("dve", lambda e, h=h: e.tensor_scalar(Bc[h], dtab, nsl[:, h:h + 1], None, ALU.mult),
              reads=[ttb[1], nslb], writes=[constb])
    kb.dma("sp", rtab[:, 0:64], relslc, writes=[ttb[2]])
    for h in range(4):
        kb.op("dve", lambda e, h=h: e.tensor_scalar(cbs[:, h, :], rtab[:, 0:64], nsl[:, h:h + 1], None, ALU.mult),
              reads=[ttb[2], nslb], writes=[constb])
    kb.dma("sp", rtab[:, 0:16], relcmp, writes=[ttb[2]])
    for h in range(4):
        kb.op("dve", lambda e, h=h: e.tensor_scalar(cbc[:, h, :], rtab[:, 0:16], nsl[:, h:h + 1], None, ALU.mult),
              reads=[ttb[2], nslb], writes=[constb])
    scrb = ttb[0]
    for i in range(16):
        k0 = i * 512
        kb.op("pool", lambda e: e.memset(scr, 1.0), writes=[scrb])
        kb.op("pool", lambda e, k0=k0: e.affine_select(out=scr, in_=scr, pattern=[[1, 512]], compare_op=ALU.is_ge,
                                                       fill=kb.freg[0.0], base=k0, channel_multiplier=-64),
              reads=[scrb], writes=[scrb])
        kb.op("pool", lambda e, k0=k0: e.affine_select(out=scr, in_=scr, pattern=[[-1, 512]], compare_op=ALU.is_ge,
                                                       fill=kb.freg[0.0], base=63 - k0, channel_multiplier=64),
              reads=[scrb], writes=[scrb])
        kb.op("pool", lambda e, k0=k0: e.tensor_copy(Ebig[:, k0:k0 + 512], scr), reads=[scrb], writes=[Eb])

    o_v = o_out.rearrange("(c q p) f -> c p q f", p=128, q=4) if o_out is not None else None
    cnt = {"s": 0, "t": 0, "p": 0}
    outs = []

    def unit(h, c, KT, ksl, cb_ap, Bt, mask, acc_bank_views, Vrhs, first, last, sel):
        hp = slice(64 * (h % 2), 64 * (h % 2) + 64)
        q_ap = QT[h // 2][hp, c * 512:(c + 1) * 512]
        si = cnt["s"] % 2; cnt["s"] += 1
        ti = cnt["t"] % 3; cnt["t"] += 1
        kb.op("pe", lambda e: e.matmul(bk[si][:], KT[hp, ksl], q_ap, start=True, stop=(sel is None)),
              reads=[QTb, KsTb, KwTb, KcTb], writes=[bkb[si]])
        if sel is not None:
            kb.op("pe", lambda e: e.matmul(bk[si][:], Ebig[:, ksl], negselT[:, c * 512:(c + 1) * 512],
                                           start=False, stop=True), reads=[Eb, nsTb], writes=[bkb[si]])
        kb.op("dve", lambda e: e.scalar_tensor_tensor(out=tt_[ti], in0=bk[si][:], scalar=cb_ap, in1=Bt,
                                                      op0=ALU.add, op1=ALU.add),
              reads=[bkb[si], constb], writes=[ttb[ti]])
        if mask is not None:
            pat, base, cm = mask
            kb.op("pool", lambda e: e.affine_select(out=tt_[ti], in_=tt_[ti], pattern=pat, compare_op=ALU.is_ge,
                                                    fill=kb.freg[NEG], base=base, channel_multiplier=cm),
                  reads=[ttb[ti]], writes=[ttb[ti]])
        kb.op("act", lambda e: e.activation(out=pT[ti], in_=tt_[ti], func=AF.Exp), reads=[ttb[ti]], writes=[pTb[ti]])
        for qs in range(4):
            view, vb = acc_bank_views[qs]
            kb.op("pe", lambda e, qs=qs, view=view: e.matmul(view, pT[ti][:, qs * 128:(qs + 1) * 128], Vrhs,
                                                             start=first, stop=last),
                  reads=[pTb[ti], Vsb, Vwb, VcXb], writes=[vb])

    for c in range(16):
        oa = oacc[c % 2]
        oab = oaccb[c % 2]
        for h in range(4):
            views = [(bk[2 + qs][:, 0:193], bkb[2 + qs]) for qs in range(4)]
            ni = c // 4 + 1
            for i in range(ni):
                mask = None
                if i >= c // 4 - 1:
                    mask = ([[1, 512]], 512 * c - 2048 * i - 31, -16)
                unit(h, c, KcT2, slice(i * 128, (i + 1) * 128), cbc[:, h, c - 4 * i:c - 4 * i + 1], Bc[h], mask,
                     views, VcX[:, i, :], i == 0, i == ni - 1, None)
            for qs in range(4):
                kb.op("dve", lambda e, qs=qs: e.tensor_scalar(rd[:, qs:qs + 1], bk[2 + qs][:, 64:65], 1e-30, None, ALU.max),
                      reads=[bkb[2 + qs]], writes=[rdb])
            kb.op("dve", lambda e: e.reciprocal(rd[:, 0:4], rd[:, 0:4]), reads=[rdb], writes=[rdb])
            for qs in range(4):
                if h == 0:
                    kb.op("dve", lambda e, qs=qs: e.tensor_scalar(
                        imp[:, qs, :], bk[2 + qs][:, 65:193], rd[:, qs:qs + 1], None, ALU.mult),
                        reads=[bkb[2 + qs], rdb], writes=[impb])
                else:
                    kb.op("dve", lambda e, qs=qs: e.scalar_tensor_tensor(
                        out=imp[:, qs, :], in0=bk[2 + qs][:, 65:193], scalar=rd[:, qs:qs + 1], in1=imp[:, qs, :],
                        op0=ALU.mult, op1=ALU.add), reads=[bkb[2 + qs], rdb, impb], writes=[impb])
            kb.op("dve", lambda e, h=h, c=c: e.tensor_tensor(
                rd[:, 4:8], rd[:, 0:4], gates[:, 4 * c:4 * c + 4, h * 3 + 0], ALU.mult),
                reads=[rdb, gatesb], writes=[rdb])
            for qs in range(4):
                if ONLY in ('slc', 'win'):
                    kb.op("dve", lambda e, qs=qs, h=h, oa=oa: e.tensor_scalar(
                        oa[:, qs, h * 64:(h + 1) * 64], bk[2 + qs][:, 0:64], 0.0, None, ALU.mult),
                        reads=[bkb[2 + qs], rdb], writes=[oab])
                else:
                    kb.op("dve", lambda e, qs=qs, h=h, oa=oa: e.tensor_scalar(
                        oa[:, qs, h * 64:(h + 1) * 64], bk[2 + qs][:, 0:64], rd[:, 4 + qs:5 + qs], None, ALU.mult),
                        reads=[bkb[2 + qs], rdb], writes=[oab])
        for qs in range(4):
            t0 = 512 * c + 128 * qs
            kb.op("pool", lambda e, qs=qs, t0=t0: e.affine_select(
                out=sc[:, qs, :], in_=imp[:, qs, :], pattern=[[-64, 128]], compare_op=ALU.is_ge, fill=kb.freg[-1e9],
                base=t0 - 128, channel_multiplier=1), reads=[impb], writes=[scb[qs]])
            kb.op("pool", lambda e, qs=qs: e.memset(sc[:, qs, 0:1], -1e9), writes=[scb[qs]])
            kb.op("dve", lambda e, qs=qs: e.max(mx[:, qs, 0:8], sc[:, qs, :]), reads=[scb[qs]], writes=[mxb[qs]])
            kb.op("dve", lambda e, qs=qs: e.match_replace(sc2[:, qs, :], mx[:, qs, 0:8], sc[:, qs, :], -2e9),
                  reads=[scb[qs], mxb[qs]], writes=[sc2b[qs]])
            kb.op("dve", lambda e, qs=qs: e.max(mx[:, qs, 8:16], sc2[:, qs, :]), reads=[sc2b[qs]], writes=[mxb[qs]])
            kb.op("dve", lambda e, qs=qs: e.tensor_scalar(nsel[:, qs, :], sc[:, qs, :], mx[:, qs, 12:13], NEG,
                                                          ALU.is_lt, ALU.mult),
                  reads=[scb[qs], mxb[qs]], writes=[nselb[qs]])
            kb.op("pool", lambda e, qs=qs, t0=t0: e.affine_select(
                out=nsel[:, qs, :], in_=nsel[:, qs, :], pattern=[[-64, 128]], compare_op=ALU.is_ge, fill=kb.freg[0.0],
                base=t0 - 128, channel_multiplier=1), reads=[nselb[qs]], writes=[nselb[qs]])
            kb.op("pool", lambda e, qs=qs: e.memset(nsel[:, qs, 0:1], 0.0), writes=[nselb[qs]])
            kb.op("pe", lambda e, qs=qs: e.transpose(bk[6][:, qs * 128:(qs + 1) * 128], nsel[:, qs, :], ident[:]),
                  reads=[nselb[qs], identb], writes=[bkb[6]])
        kb.op("act", lambda e, c=c: e.copy(negselT[:, c * 512:(c + 1) * 512], bk[6][:]), reads=[bkb[6]], writes=[nsTb])
        for br, KT, V1 in ((2, KwT2, Vw1), (1, KsT2, Vs1)):
            for h in range(4):
                views = [(bk[2 + qs][:, 0:65], bkb[2 + qs]) for qs in range(4)]
                j0 = max(0, 4 * c - 4) if br == 2 else 0
                j1 = 4 * c + 3
                for j in range(j0, j1 + 1):
                    rel = 512 * c - 128 * j
                    if j >= 4 * c:
                        mask = ([[1, 512]], rel, -1)
                    elif br == 2:
                        mask = ([[-1, 512]], 511 - rel, 1)
                    else:
                        mask = None
                    m = 4 * c - j + 3
                    unit(h, c, KT, slice(j * 128, (j + 1) * 128), cbs[:, h, m:m + 1], Bs[h], mask, views,
                         V1[:, j, :], j == j0, j == j1, True if br == 1 else None)
                for qs in range(4):
                    kb.op("dve", lambda e, qs=qs: e.tensor_scalar(rd[:, qs:qs + 1], bk[2 + qs][:, 64:65], 1e-30, None, ALU.max),
                          reads=[bkb[2 + qs]], writes=[rdb])
                kb.op("dve", lambda e: e.reciprocal(rd[:, 0:4], rd[:, 0:4]), reads=[rdb], writes=[rdb])
                kb.op("dve", lambda e, h=h, c=c, br=br: e.tensor_tensor(
                    rd[:, 0:4], rd[:, 0:4], gates[:, 4 * c:4 * c + 4, h * 3 + br], ALU.mult),
                    reads=[rdb, gatesb], writes=[rdb])
                for qs in range(4):
                    if ONLY and ONLY != ('win' if br == 2 else 'slc'):
                        kb.op("dve", lambda e, qs=qs: e.tensor_copy(rd[:, 4 + qs:5 + qs], bk[2 + qs][:, 64:65]),
                              reads=[bkb[2 + qs]], writes=[rdb])
                        continue
                    kb.op("dve", lambda e, qs=qs, h=h, oa=oa: e.scalar_tensor_tensor(
                        out=oa[:, qs, h * 64:(h + 1) * 64], in0=bk[2 + qs][:, 0:64], scalar=rd[:, qs:qs + 1],
                        in1=oa[:, qs, h * 64:(h + 1) * 64], op0=ALU.mult, op1=ALU.add),
                        reads=[bkb[2 + qs], rdb, oab], writes=[oab])
        outs.append(kb.dma("sp", o_v[c] if o_dst_fn is None else o_dst_fn(c), oa, reads=[oab]))
        if gather_fn is not None and gather_fn(c) is not None:
            gs_, gd_ = gather_fn(c)
            outs.append(kb.collective(gs_, gd_, deps=outs[-2:]))
    kb.wait_all("sp", outs)
    tickets = kb.final_tickets()
    kb.emit()
    return tickets


def build_gla_phase(nc, st=None, prev=(), pfx="", x_src_fn=None, o_dst_fn=None, gather_fn=None):
    T = SEQ
    QSCALE = 128.0 ** -0.5

    def din(name, shape):
        return nc.dram_tensor(pfx + name, list(shape), F32, kind="ExternalInput").ap()

    xT = din("xT", [1024, T]) if x_src_fn is None else None
    wall = din("wall", [1024, 784])
    wg2 = din("wg2", [16, 128])
    bg2 = din("bg2", [1, 128])
    hng = din("hng", [256])
    lblk = din("lblk", [128, 128])
    ublk = din("ublk", [128, 128])
    o_out = None if o_dst_fn is not None else nc.dram_tensor("o", [T, 256], F32, kind="ExternalOutput").ap()

    kb = KB(nc, st, prev, pfx)
    if x_src_fn is not None:
        ident, identb = make_ident(kb)
        xtok = [kb.sb("xtok%d" % i, [128, D_MODEL], F32) for i in range(2)]
        xtokb = [Buf("xtok0"), Buf("xtok1")]
    one1 = kb.sb("one1", [128, 1], F32)
    epsr = kb.sb("epsr", [128, 1], F32)
    cb = Buf("consts")
    kb.op("pool", lambda e: e.memset(one1[:], 1.0), writes=[cb])
    kb.op("pool", lambda e: e.memset(epsr[:], RMS_EPS), writes=[cb])

    wall_sb = kb.sb("wall_sb", [128, 8, 784], BF16); wallb = Buf("wall")
    stg = Stager(kb, 2, 2048)
    xbf = [kb.sb("xbf%d" % i, [128, 8, 256], BF16) for i in range(2)]
    xbfb = [Buf("xbf0"), Buf("xbf1")]
    L01 = kb.sb("L01", [128, 128], F32)
    LS = kb.sb("LS", [128, 128], F32)
    US = kb.sb("US", [128, 128], F32)
    hn = kb.sb("hn", [128, 256], F32)
    wg2_sb = kb.sb("wg2_sb", [16, 128], BF16)
    bg2_sb = kb.sb("bg2_sb", [1, 128], BF16)
    ones_bf = kb.sb("ones_bf", [1, 128], BF16)
    misc = kb.sb("misc", [128, 128], F32); miscb = Buf("misc")

    qT_sb = kb.sb("qT_sb", [128, 256], F32); qTb = Buf("qT")
    kT_sb = kb.sb("kT_sb", [128, 256], F32); kTb = Buf("kT")
    alT_sb = kb.sb("alT_sb", [16, 256], BF16); alTb = Buf("alT")
    v_bf = kb.sb("v_bf", [128, 256], BF16); vb = Buf("v")
    k_tok = kb.sb("k_tok", [128, 128], F32); ktb = Buf("ktok")
    gs = kb.sb("gs", [128, 256], F32); gsb = Buf("gs")
    e1 = kb.sb("e1", [128, 128], F32); e1b = Buf("e1")
    la = kb.sb("la", [128, 128], F32); lab = Buf("la")
    bT_sb = kb.sb("bT_sb", [128, 2, 64], F32); bTb = Buf("bT")
    bd = kb.sb("bd", [128, 2, 64], F32); bdb = Buf("bd")
    eg = kb.sb("eg", [128, 128], F32); egb = Buf("eg")
    ieg = kb.sb("ieg", [128, 128], F32); iegb = Buf("ieg")
    eb = kb.sb("eb", [128, 128], F32); ebb = Buf("eb")
    erb = kb.sb("erb", [128, 128], F32); erbb = Buf("erb")
    qgT = kb.sb("qgT", [128, 128], BF16); qgb = Buf("qg")
    kgT = kb.sb("kgT", [128, 128], BF16); kgb = Buf("kg")
    qbP = [kb.sb("qbP%d" % i, [128, 128], BF16) for i in range(2)]; qbPb = [Buf("qbP0"), Buf("qbP1")]
    kbt = kb.sb("kbt", [128, 128], BF16); kbtb = Buf("kbt")
    AT = kb.sb("AT", [128, 128], BF16); ATb = Buf("AT")
    dec = kb.sb("dec", [128, 2], F32); decb = Buf("dec")
    S32 = kb.sb("S32", [128, 256], F32); S32b = Buf("S32")
    Sbf = [kb.sb("Sbf%d" % i, [128, 256], BF16) for i in range(3)]; Sbfb = [Buf("Sbf%d" % i) for i in range(3)]
    st = kb.sb("st", [128, 6], F32); stb = Buf("st")
    mv = kb.sb("mv", [128, 2], F32); mvb = Buf("mv")
    rs = kb.sb("rs", [128, 2], F32); rsb = Buf("rs")
    ot = [kb.sb("ot%d" % i, [128, 256], F32) for i in range(2)]; otb = [Buf("ot0"), Buf("ot1")]

    bk = [kb.ps("bk%d" % i, [128, 512]) for i in range(8)]
    bkb = [Buf("bk%d" % i) for i in range(8)]

    wall_v = wall.rearrange("(kt p) n -> p kt n", p=128)
    for i in range(4):
        stg.load(wall_sb[:, 2 * i:2 * i + 2, :], [wallb], wall_v[:, 2 * i:2 * i + 2, :], 2, 784)
    kb.dma("sp", L01[:], lblk, writes=[cb])
    kb.op("dve", lambda e: e.tensor_scalar(LS[:], L01[:], -1.0 / 16.0, None, ALU.mult), reads=[cb], writes=[cb])
    kb.dma("sp", misc[:], ublk, writes=[miscb])
    kb.op("dve", lambda e: e.tensor_scalar(US[:], misc[:], -1.0 / 16.0, None, ALU.mult), reads=[miscb], writes=[cb])
    kb.dma("sp", hn[:], bcast_rows(hng), writes=[cb])
    kb.dma("sp", misc[0:16, :], wg2, writes=[miscb])
    kb.op("dve", lambda e: e.tensor_copy(wg2_sb[:], misc[0:16, :]), reads=[miscb], writes=[cb])
    kb.dma("sp", misc[0:1, :], bg2, writes=[miscb])
    kb.op("dve", lambda e: e.tensor_copy(bg2_sb[:], misc[0:1, :]), reads=[miscb], writes=[cb])
    kb.op("pool", lambda e: e.memset(ones_bf[:], 1.0), writes=[cb])
    kb.op("pool", lambda e: e.memset(qbP[0][:], 0.0), writes=[qbPb[0]])
    kb.op("pool", lambda e: e.memset(qbP[1][:], 0.0), writes=[qbPb[1]])
    kb.op("pool", lambda e: e.memset(S32[:], 0.0), writes=[S32b])

    xT_v = xT.rearrange("(kt p) t -> p kt t", p=128) if x_src_fn is None else None
    o_v = o_out.rearrange("(m p) f -> m p f", p=128) if o_out is not None else None
    outs = []
    s_i = 0
    have_S = False
    for c in range(32):
        xs = c % 2
        if x_src_fn is None:
            stg.load(xbf[xs][:], [xbfb[xs]], xT_v[:, :, c * 256:(c + 1) * 256], 8, 256)
        else:
            for tl in range(2):
                xi = (c * 2 + tl) % 2
                kb.dma("sp", xtok[xi][:], x_src_fn(c * 2 + tl), writes=[xtokb[xi]])
                for half in range(2):
                    for j in range(4):
                        kt = half * 4 + j
                        kb.op("pe", lambda e, half=half, j=j, kt=kt, xi=xi: e.transpose(
                            bk[half][:, j * 128:(j + 1) * 128], xtok[xi][:, kt * 128:(kt + 1) * 128], ident[:]),
                            reads=[xtokb[xi], identb], writes=[bkb[half]])
                    kb.op("act", lambda e, half=half, xs=xs, tl=tl: e.copy(
                        xbf[xs][:, half * 4:(half + 1) * 4, tl * 128:(tl + 1) * 128],
                        bk[half][:].rearrange("p (j t) -> p j t", j=4)),
                        reads=[bkb[half]], writes=[xbfb[xs]])
        for kt in range(8):
            kb.op("pe", lambda e, kt=kt, xs=xs: e.matmul(bk[0][:, 0:256], wall_sb[:, kt, 0:128], xbf[xs][:, kt, :],
                                                         start=(kt == 0), stop=(kt == 7)),
                  reads=[wallb, xbfb[xs]], writes=[bkb[0]])
        kb.op("dve", lambda e: e.tensor_scalar(qT_sb[:], bk[0][:, 0:256], QSCALE, None, ALU.mult),
              reads=[bkb[0]], writes=[qTb])
        for kt in range(8):
            kb.op("pe", lambda e, kt=kt, xs=xs: e.matmul(bk[1][:, 0:256], wall_sb[:, kt, 128:256], xbf[xs][:, kt, :],
                                                         start=(kt == 0), stop=(kt == 7)),
                  reads=[wallb, xbfb[xs]], writes=[bkb[1]])
        kb.op("act", lambda e: e.copy(kT_sb[:], bk[1][:, 0:256]), reads=[bkb[1]], writes=[kTb])
        for kt in range(8):
            kb.op("pe", lambda e, kt=kt, xs=xs: e.matmul(bk[2][0:16, 0:256], wall_sb[:, kt, 768:784], xbf[xs][:, kt, :],
                                                         start=(kt == 0), stop=(kt == 7)),
                  reads=[wallb, xbfb[xs]], writes=[bkb[2]])
        kb.op("act", lambda e: e.copy(alT_sb[:], bk[2][0:16, 0:256]), reads=[bkb[2]], writes=[alTb])
        for tl in range(2):
            m = c * 2 + tl
            tk = slice(tl * 128, (tl + 1) * 128)
            for kt in range(8):
                kb.op("pe", lambda e, kt=kt, xs=xs, tk=tk: e.matmul(bk[3][:, 0:256], xbf[xs][:, kt, tk], wall_sb[:, kt, 256:512],
                                                                    start=(kt == 0), stop=(kt == 7)),
                      reads=[wallb, xbfb[xs]], writes=[bkb[3]])
            for kt in range(8):
                kb.op("pe", lambda e, kt=kt, xs=xs, tk=tk: e.matmul(bk[3][:, 256:384], xbf[xs][:, kt, tk], wall_sb[:, kt, 128:256],
                                                                    start=(kt == 0), stop=(kt == 7)),
                      reads=[wallb, xbfb[xs]], writes=[bkb[3]])
            kb.op("dve", lambda e: e.tensor_copy(v_bf[:], bk[3][:, 0:256]), reads=[bkb[3]], writes=[vb])
            kb.op("dve", lambda e: e.tensor_copy(k_tok[:], bk[3][:, 256:384]), reads=[bkb[3]], writes=[ktb])
            for kt in range(8):
                kb.op("pe", lambda e, kt=kt, xs=xs, tk=tk: e.matmul(bk[4][:, 0:256], xbf[xs][:, kt, tk], wall_sb[:, kt, 512:768],
                                                                    start=(kt == 0), stop=(kt == 7)),
                      reads=[wallb, xbfb[xs]], writes=[bkb[4]])
            kb.op("act", lambda e: e.activation(out=gs[:], in_=bk[4][:, 0:256], func=AF.Silu), reads=[bkb[4]], writes=[gsb])
            kb.op("pool", lambda e: e.tensor_tensor(gs[:], gs[:], hn[:], ALU.mult), reads=[gsb, cb], writes=[gsb])
            kb.op("pe", lambda e, tk=tk: e.matmul(bk[2][:, 256:384], alT_sb[:, tk], wg2_sb[:], start=True, stop=False),
                  reads=[alTb, cb], writes=[bkb[2]])
            kb.op("pe", lambda e: e.matmul(bk[2][:, 256:384], ones_bf[:], bg2_sb[:], start=False, stop=True),
                  reads=[cb], writes=[bkb[2]])
            kb.op("act", lambda e: e.activation(out=e1[:], in_=bk[2][:, 256:384], func=AF.Exp, scale=-1.0),
                  reads=[bkb[2]], writes=[e1b])
            kb.op("act", lambda e: e.activation(out=la[:], in_=e1[:], func=AF.Ln, bias=one1[:], scale=1.0),
                  reads=[e1b, cb], writes=[lab])
            kb.op("pe", lambda e: e.matmul(bk[5][:, 0:128], la[:], LS[:], start=True, stop=True),
                  reads=[lab, cb], writes=[bkb[5]])
            kb.op("pe", lambda e: e.matmul(bk[5][:, 128:256], US[:], la[:], start=True, stop=True),
                  reads=[lab, cb], writes=[bkb[5]])
            kb.op("act", lambda e: e.copy(bT_sb[:], bk[5][:, 0:128].rearrange("p (c t) -> p c t", c=2)),
                  reads=[bkb[5]], writes=[bTb])
            kb.op("act", lambda e: e.activation(out=erb[:], in_=bk[5][:, 128:256], func=AF.Exp), reads=[bkb[5]], writes=[erbb])
            for cc in range(2):
                kb.op("dve", lambda e, cc=cc: e.tensor_scalar(bd[:, cc, :], bT_sb[:, cc, :], bT_sb[:, cc, 32:33], None,
                                                              ALU.subtract), reads=[bTb], writes=[bdb])
            kb.op("act", lambda e: e.activation(out=eg[:], in_=bd[:].rearrange("p c t -> p (c t)"), func=AF.Exp),
                  reads=[bdb], writes=[egb])
            kb.op("act", lambda e: e.activation(out=eb[:], in_=bT_sb[:].rearrange("p c t -> p (c t)"), func=AF.Exp),
                  reads=[bTb], writes=[ebb])
            kb.op("act", lambda e: e.activation(out=dec[:], in_=bT_sb[:, :, 63], func=AF.Exp), reads=[bTb], writes=[decb])
            kb.op("dve", lambda e: e.reciprocal(ieg[:], eg[:]), reads=[egb], writes=[iegb])
            kb.op("dve", lambda e, tk=tk: e.tensor_tensor(qgT[:], qT_sb[:, tk], eg[:], ALU.mult), reads=[qTb, egb], writes=[qgb])
            kb.op("dve", lambda e, tk=tk: e.tensor_tensor(kgT[:], kT_sb[:, tk], ieg[:], ALU.mult), reads=[kTb, iegb], writes=[kgb])
            kb.op("dve", lambda e, tk=tk: e.tensor_tensor(qbP[0][:, 0:64], qT_sb[:, tk.start:tk.start + 64], eb[:, 0:64], ALU.mult),
                  reads=[qTb, ebb], writes=[qbPb[0]])
            kb.op("dve", lambda e, tk=tk: e.tensor_tensor(qbP[1][:, 64:128], qT_sb[:, tk.start + 64:tk.start + 128], eb[:, 64:128], ALU.mult),
                  reads=[qTb, ebb], writes=[qbPb[1]])
            kb.op("dve", lambda e: e.tensor_tensor(kbt[:], k_tok[:], erb[:], ALU.mult), reads=[ktb, erbb], writes=[kbtb])
            kb.op("pe", lambda e: e.matmul(bk[6][:, 0:128], kgT[:], qgT[:], start=True, stop=True),
                  reads=[kgb, qgb], writes=[bkb[6]])
            kb.op("dve", lambda e: e.tensor_tensor(AT[:], bk[6][:, 0:128], L01[:], ALU.mult), reads=[bkb[6], cb], writes=[ATb])
            s_prev = s_i
            terms = [(AT, ATb, v_bf, vb)]
            if have_S:
                terms.append((qbP[0], qbPb[0], Sbf[s_prev % 3], Sbfb[s_prev % 3]))
            for cc in range(2):
                pr = slice(cc * 64, (cc + 1) * 64)
                kb.op("pe", lambda e, pr=pr: e.matmul(bk[6][:, 128:384], kbt[pr, :], v_bf[pr, :], start=True, stop=True),
                      reads=[kbtb, vb], writes=[bkb[6]])
                kb.op("dve", lambda e, cc=cc: e.scalar_tensor_tensor(out=S32[:], in0=S32[:], scalar=dec[:, cc:cc + 1],
                                                                     in1=bk[6][:, 128:384], op0=ALU.mult, op1=ALU.add),
                      reads=[S32b, decb, bkb[6]], writes=[S32b])
                s_i += 1
                kb.op("act", lambda e, si=s_i: e.copy(Sbf[si % 3][:], S32[:]), reads=[S32b], writes=[Sbfb[s_i % 3]])
                if cc == 0:
                    terms.append((qbP[1], qbPb[1], Sbf[s_i % 3], Sbfb[s_i % 3]))
            have_S = True
            for ti, (l_, lb_, r_, rb_) in enumerate(terms):
                kb.op("pe", lambda e, l_=l_, r_=r_, ti=ti, n=len(terms): e.matmul(
                    bk[7][:, 0:256], l_[:], r_[:], start=(ti == 0), stop=(ti == n - 1)),
                    reads=[lb_, rb_], writes=[bkb[7]])
            kb.op("dve", lambda e: e.bn_stats(st[:], bk[7][:, 0:256]), reads=[bkb[7]], writes=[stb])
            kb.op("dve", lambda e: e.bn_aggr(mv[:], st[:]), reads=[stb], writes=[mvb])
            kb.op("dve", lambda e: e.tensor_tensor(rs[:, 0:1], mv[:, 0:1], mv[:, 0:1], ALU.mult), reads=[mvb], writes=[rsb])
            kb.op("dve", lambda e: e.tensor_tensor(rs[:, 0:1], rs[:, 0:1], mv[:, 1:2], ALU.add), reads=[mvb, rsb], writes=[rsb])
            kb.op("act", lambda e: e.activation(out=rs[:, 1:2], in_=rs[:, 0:1], func=AF.Sqrt, bias=epsr[:], scale=1.0),
                  reads=[rsb, cb], writes=[rsb])
            kb.op("dve", lambda e: e.reciprocal(rs[:, 1:2], rs[:, 1:2]), reads=[rsb], writes=[rsb])
            oi = m % 2
            kb.op("dve", lambda e, oi=oi: e.scalar_tensor_tensor(out=ot[oi][:], in0=bk[7][:, 0:256], scalar=rs[:, 1:2],
                                                                 in1=gs[:], op0=ALU.mult, op1=ALU.mult),
                  reads=[bkb[7], rsb, gsb], writes=[otb[oi]])
            outs.append(kb.dma("sp", o_v[m] if o_dst_fn is None else o_dst_fn(m), ot[oi][:], reads=[otb[oi]]))
            if gather_fn is not None and gather_fn(m) is not None:
                gs_, gd_ = gather_fn(m)
                outs.append(kb.collective(gs_, gd_, deps=outs[-8:]))
    kb.wait_all("sp", outs)
    tickets = kb.final_tickets()
    kb.emit()
    return tickets


def build_fused(nc):
    def internal(name, shape):
        return nc.dram_tensor(name, list(shape), F32, kind="Internal").ap()

    st = SemState(nc)
    oA = [internal("i_oA%d" % k, [1024, 256]) for k in range(8)]
    gA = [internal("i_gA%d" % k, [4 * 1024, 256]) for k in range(8)]
    x1 = [internal("i_x1%d" % k, [256, D_MODEL]) for k in range(8)]
    gX = [internal("i_gX%d" % k, [4 * 256, D_MODEL]) for k in range(8)]
    oC = [internal("i_oC%d" % k, [1024, 256]) for k in range(8)]
    gC = [internal("i_gC%d" % k, [4 * 1024, 256]) for k in range(8)]
    y = nc.dram_tensor("y", [NT, D_MODEL], F32, kind="ExternalOutput").ap()

    def mixer_cand(g_list):
        def fn(rk, tt):
            t0 = rk * NT + tt * 128
            k, i = t0 // 1024, t0 % 1024
            return g_list[k].rearrange("(g t) f -> t g f", g=4)[i:i + 128]
        return fn

    t = build_nsa_phase(
        nc, st, (), "a_",
        o_dst_fn=lambda c: oA[c // 2][(c % 2) * 512:(c % 2) * 512 + 512, :].rearrange("(q p) f -> p q f", p=128),
        gather_fn=lambda c: (oA[c // 2], gA[c // 2]) if c % 2 == 1 else None)
    t = build_ffn_phase(
        nc, False, 1, st, t, "b_", o_cand_fn=mixer_cand(gA),
        y_fn=lambda tt: x1[tt // 2][(tt % 2) * 128:(tt % 2) * 128 + 128, :],
        gather_fn=lambda tt: (x1[tt // 2], gX[tt // 2]) if tt % 2 == 1 else None)

    def x_src_fn(m):
        r, k, i = m // 16, (m % 16) // 2, (m % 2) * 128
        return gX[k][r * 256 + i:r * 256 + i + 128, :]

    t = build_gla_phase(
        nc, st, t, "c_", x_src_fn=x_src_fn,
        o_dst_fn=lambda m: oC[m // 8][(m % 8) * 128:(m % 8) * 128 + 128, :],
        gather_fn=lambda m: (oC[m // 8], gC[m // 8]) if m % 8 == 7 else None)
    t = build_ffn_phase(
        nc, True, 8, st, t, "d_", o_cand_fn=mixer_cand(gC),
        xres_fn=lambda tt: x1[tt // 2][(tt % 2) * 128:(tt % 2) * 128 + 128, :],
        y_fn=lambda tt: y[tt * 128:(tt + 1) * 128, :])
    st.es.close()
    return nc


_PROG_CACHE = {}


def _get_prog(key, builder):
    if key not in _PROG_CACHE:
        nc = bass.Bass("TRN2", target_bir_lowering=False)
        _PROG_CACHE[key] = builder(nc)
    return _PROG_CACHE[key]


def run_ffn_phase(moe, oT_list, xres_list, w_o, ln1_g, ln1_b, ln2_g, ln2_b, w_gu, w_dn, w_r=None):
    E = w_gu.shape[0]
    nc = _get_prog("ffn_%s_%d" % (moe, E), lambda nc: build_ffn_phase(nc, moe, E))
    in_maps = []
    for c in range(NCORES):
        m = {"oT": oT_list[c], "xres": xres_list[c], "w_o": w_o, "ln1_g": ln1_g, "ln1_b": ln1_b,
             "ln2_g": ln2_g, "ln2_b": ln2_b, "w_gu": w_gu, "w_dn": w_dn}
        if moe:
            m["w_r"] = w_r
        in_maps.append(m)
    res = run_bass_kernel_spmd(nc, in_maps, core_ids=list(range(NCORES)))
    return [r["y"] for r in res.results]


def _nsa_consts():
    kl = np.arange(128, dtype=np.float32)[:, None]
    ql = np.arange(512, dtype=np.float32)[None, :]
    dslc = np.ascontiguousarray(np.broadcast_to(ql - kl, (128, 512)).astype(np.float32))
    dcmp = np.ascontiguousarray(np.broadcast_to(ql - 16.0 * kl, (128, 512)).astype(np.float32))
    relslc = np.ascontiguousarray(np.broadcast_to((128.0 * np.arange(64) - 384.0)[None, :], (128, 64)).astype(np.float32))
    relcmp = np.ascontiguousarray(np.broadcast_to((512.0 * np.arange(16) - 31.0)[None, :], (128, 16)).astype(np.float32))
    n_cmp = (SEQ - 32) // 16 + 1
    cs = np.arange(n_cmp) * 16
    ce = cs + 31
    bs = np.arange(SEQ // 64) * 64
    be = bs + 63
    ov = np.zeros((512, 128), np.float32)
    ov[:n_cmp] = ((cs[:, None] <= be[None]) & (ce[:, None] >= bs[None])).astype(np.float32)
    return dslc, dcmp, relslc, relcmp, ov


def run_nsa_phase(x, w_in, pe_k, pe_v, wk1, wk2, wv1, wv2):
    nc = _get_prog("nsa", build_nsa_phase)
    dslc, dcmp, relslc, relcmp, ov = _nsa_consts()
    slopes = (2.0 ** (-8.0 * (np.arange(16, dtype=np.float32) + 1.0) / 16)).astype(np.float32)
    peT = np.ascontiguousarray(np.concatenate([pe_k.T, pe_v.T], axis=0))
    w1 = np.ascontiguousarray(np.stack([wk1, wv1]))
    w2k = np.ascontiguousarray(np.concatenate([wk2, wk2], axis=1))
    xTs = [np.ascontiguousarray(x[b].T) for b in range(BATCH)]
    in_maps = []
    for core in range(NCORES):
        b, g = core // 4, core % 4
        kvcol = lambda i: w_in[:, 1024 + i * 256 + g * 64:1024 + i * 256 + (g + 1) * 64]
        wall = np.concatenate([
            w_in[:, g * 256:(g + 1) * 256], kvcol(0), kvcol(1), kvcol(2), kvcol(2), kvcol(4), kvcol(4),
            kvcol(3), kvcol(5), w_in[:, 2560 + g * 12:2560 + (g + 1) * 12]], axis=1)
        nsl = np.ascontiguousarray(np.broadcast_to(-slopes[g * 4:(g + 1) * 4][None, :], (128, 4)).astype(np.float32))
        in_maps.append({"xT": xTs[b], "wall": np.ascontiguousarray(wall), "w1": w1, "peT": peT, "w2k": w2k,
                        "w2v": wv2, "ov": ov, "dslc": dslc, "dcmp": dcmp, "nslope": nsl, "relslc": relslc,
                        "relcmp": relcmp})
    res = run_bass_kernel_spmd(nc, in_maps, core_ids=list(range(NCORES)))
    o = np.empty((BATCH, SEQ, 1024), np.float32)
    for core in range(NCORES):
        b, g = core // 4, core % 4
        o[b, :, g * 256:(g + 1) * 256] = res.results[core]["o"]
    return o


def _gla_consts():
    t = np.arange(128)
    same = (t[:, None] // 64) == (t[None, :] // 64)
    lblk = (same & (t[:, None] <= t[None, :])).astype(np.float32)
    ublk = (same & (t[:, None] > t[None, :])).astype(np.float32)
    return lblk, ublk


def run_gla_phase(x, w_in, w_gate2, b_gate2, head_norm_g):
    nc = _get_prog("gla", build_gla_phase)
    lblk, ublk = _gla_consts()
    xTs = [np.ascontiguousarray(x[b].T) for b in range(BATCH)]
    in_maps = []
    for core in range(NCORES):
        b, h = core // 4, core % 4
        wall = np.concatenate([
            w_in[:, h * 128:(h + 1) * 128], w_in[:, 512 + h * 128:512 + (h + 1) * 128],
            w_in[:, 1024 + h * 256:1024 + (h + 1) * 256], w_in[:, 2048 + h * 256:2048 + (h + 1) * 256],
            w_in[:, 3072:3088]], axis=1)
        in_maps.append({"xT": xTs[b], "wall": np.ascontiguousarray(wall),
                        "wg2": np.ascontiguousarray(w_gate2[:, h * 128:(h + 1) * 128]),
                        "bg2": np.ascontiguousarray(b_gate2[None, h * 128:(h + 1) * 128]),
                        "hng": np.ascontiguousarray(head_norm_g[h * 256:(h + 1) * 256]),
                        "lblk": lblk, "ublk": ublk})
    res = run_bass_kernel_spmd(nc, in_maps, core_ids=list(range(NCORES)))
    o = np.empty((BATCH, SEQ, 1024), np.float32)
    for core in range(NCORES):
        b, h = core // 4, core % 4
        o[b, :, h * 256:(h + 1) * 256] = res.results[core]["o"]
    return o


def kernel(**inputs):
    g = lambda k: np.ascontiguousarray(np.asarray(inputs[k], dtype=np.float32))
    x = g("x")
    NTOK = BATCH * SEQ
    nc = _get_prog("fused", build_fused)
    xf = x.reshape(NTOK, D_MODEL)
    xTs = [np.ascontiguousarray(x[b].T) for b in range(BATCH)]
    w_in0 = g("l0_w_in")
    dslc, dcmp, relslc, relcmp, ov = _nsa_consts()
    slopes = (2.0 ** (-8.0 * (np.arange(16, dtype=np.float32) + 1.0) / 16)).astype(np.float32)
    peT = np.ascontiguousarray(np.concatenate([g("l0_cmp_pe_k").T, g("l0_cmp_pe_v").T], axis=0))
    w1 = np.ascontiguousarray(np.stack([g("l0_cmp_wk1"), g("l0_cmp_wv1")]))
    wk2 = g("l0_cmp_wk2")
    w2k = np.ascontiguousarray(np.concatenate([wk2, wk2], axis=1))
    w2v = g("l0_cmp_wv2")
    w_in1 = g("l1_w_in")
    wg2 = g("l1_w_gate2"); bg2 = g("l1_b_gate2"); hng = g("l1_head_norm_g")
    lblk, ublk = _gla_consts()
    shared = {
        "a_w1": w1, "a_peT": peT, "a_w2k": w2k, "a_w2v": w2v, "a_ov": ov, "a_dslc": dslc, "a_dcmp": dcmp,
        "a_relslc": relslc, "a_relcmp": relcmp,
        "b_w_o": g("l0_w_o"), "b_ln1_g": g("l0_ln1_g"), "b_ln1_b": g("l0_ln1_b"), "b_ln2_g": g("l0_ln2_g"),
        "b_ln2_b": g("l0_ln2_b"), "b_w_gu": g("l0_ffn_w_gu")[None], "b_w_dn": g("l0_ffn_w_down")[None],
        "c_lblk": lblk, "c_ublk": ublk,
        "d_w_o": g("l1_w_o"), "d_ln1_g": g("l1_ln1_g"), "d_ln1_b": g("l1_ln1_b"), "d_ln2_g": g("l1_ln2_g"),
        "d_ln2_b": g("l1_ln2_b"), "d_w_gu": g("l1_moe_w_gu"), "d_w_dn": g("l1_moe_w_down"), "d_w_r": g("l1_router"),
    }
    in_maps = []
    for core in range(NCORES):
        b, r = core // 4, core % 4
        kvcol = lambda i: w_in0[:, 1024 + i * 256 + r * 64:1024 + i * 256 + (r + 1) * 64]
        wall_a = np.concatenate([
            w_in0[:, r * 256:(r + 1) * 256], kvcol(0), kvcol(1), kvcol(2), kvcol(2), kvcol(4), kvcol(4),
            kvcol(3), kvcol(5), w_in0[:, 2560 + r * 12:2560 + (r + 1) * 12]], axis=1)
        nsl = np.ascontiguousarray(np.broadcast_to(-slopes[r * 4:(r + 1) * 4][None, :], (128, 4)).astype(np.float32))
        wall_c = np.concatenate([
            w_in1[:, r * 128:(r + 1) * 128], w_in1[:, 512 + r * 128:512 + (r + 1) * 128],
            w_in1[:, 1024 + r * 256:1024 + (r + 1) * 256], w_in1[:, 2048 + r * 256:2048 + (r + 1) * 256],
            w_in1[:, 3072:3088]], axis=1)
        sel4 = np.zeros((128, 4), np.float32)
        sel4[:, r] = 1.0
        m = dict(shared)
        m.update({
            "a_xT": xTs[b], "a_wall": np.ascontiguousarray(wall_a), "a_nslope": nsl,
            "b_sel4": sel4, "b_xres": np.ascontiguousarray(xf[core * NT:(core + 1) * NT]),
            "c_wall": np.ascontiguousarray(wall_c), "c_wg2": np.ascontiguousarray(wg2[:, r * 128:(r + 1) * 128]),
            "c_bg2": np.ascontiguousarray(bg2[None, r * 128:(r + 1) * 128]),
            "c_hng": np.ascontiguousarray(hng[r * 256:(r + 1) * 256]),
            "d_sel4": sel4,
        })
        in_maps.append(m)
    res = run_bass_kernel_spmd(nc, in_maps, core_ids=list(range(NCORES)))
    return np.concatenate([r_["y"] for r_ in res.results], axis=0).reshape(BATCH, SEQ, D_MODEL).astype(np.float32)
```

```python
import math
from contextlib import ExitStack

import numpy as np
import concourse.bass as bass
import concourse.mybir as mybir
from concourse.bass_utils import run_bass_kernel_spmd

F32 = mybir.dt.float32
BF16 = mybir.dt.bfloat16
AF = mybir.ActivationFunctionType
ALU = mybir.AluOpType
AX = mybir.AxisListType

D_MODEL = 1024
BATCH = 2
SEQ = 8192
DN_ALPHA = 4 ** 0.25
LN_EPS = 1e-5
RMS_EPS = 1e-6
NCORES = 8
NT = 2048


class Buf:
    __slots__ = ("name", "last_write", "readers")

    def __init__(self, name):
        self.name = name
        self.last_write = None
        self.readers = {}


class SemState:
    def __init__(self, nc):
        self.es = ExitStack()
        self.sem = {}
        self.cnt = {}
        self.dsem = {}
        self.dsem_rr = {}
        for e in KB.ENGS:
            self.sem[e] = self.es.enter_context(nc.semaphore("s_" + e))
            self.cnt[e] = 0
        self.sem["cc"] = self.es.enter_context(nc.semaphore("s_cc"))
        self.cnt["cc"] = 0
        for q, n in (("sp", 16), ("pool", 4), ("act", 2)):
            lst = []
            for i in range(n):
                key = "d_%s_%d" % (q, i)
                self.sem[key] = self.es.enter_context(nc.semaphore(key))
                self.cnt[key] = 0
                lst.append(key)
            self.dsem[q] = lst
            self.dsem_rr[q] = 0


class KB:
    ENGS = ("pe", "act", "dve", "pool", "sp")

    def __init__(self, nc, st=None, prev=(), pfx=""):
        self.nc = nc
        self.pfx = pfx
        self.es = ExitStack()
        self.own_st = st is None
        if st is None:
            st = SemState(nc)
        self.st = st
        self.sem = st.sem
        self.cnt = st.cnt
        self.dsem = st.dsem
        self.dsem_rr = st.dsem_rr
        self.start_val = dict(st.cnt)
        self.waited = {}
        self.prog = {}
        for e in self.ENGS:
            self.waited[e] = {}
            self.prog[e] = []
        for e in self.ENGS:
            waits = self._need(e, list(prev))
            if waits:
                self.prog[e].append((waits, None, None))
        self.n_ins = 0

    def final_tickets(self):
        return [(k, v) for k, v in self.cnt.items() if v > 0]

    def sb(self, name, shape, dt):
        n = 1
        for d in shape[1:]:
            n *= d
        self.sb_bytes = getattr(self, "sb_bytes", 0) + n * (2 if dt == BF16 else 4)
        assert self.sb_bytes <= 178000, ("SBUF budget exceeded", name, self.sb_bytes)
        return self.es.enter_context(self.nc.sbuf_tensor(self.pfx + name, list(shape), dt))

    def ps(self, name, shape, dt=F32):
        return self.es.enter_context(self.nc.psum_tensor(self.pfx + name, list(shape), dt))

    def _need(self, eng, tickets):
        waits = []
        w = self.waited[eng]
        for t in tickets:
            if t is None:
                continue
            key, val = t
            if w.get(key, 0) < val:
                w[key] = val
                waits.append((key, val))
        return waits

    def _deps(self, eng, reads, writes, extra):
        tickets = list(extra)
        for b in reads:
            t = b.last_write
            if t is not None and not (t[0] == eng and eng == "pe"):
                tickets.append(t)
        for b in writes:
            t = b.last_write
            if t is not None and not (t[0] == eng and eng == "pe"):
                tickets.append(t)
            for k, v in b.readers.items():
                if not (k == eng and eng == "pe"):
                    tickets.append((k, v))
        return tickets

    def op(self, eng, fn, reads=(), writes=(), deps=()):
        waits = self._need(eng, self._deps(eng, reads, writes, deps))
        self.cnt[eng] += 1
        t = (eng, self.cnt[eng])
        self.prog[eng].append((waits, fn, (eng, 1)))
        for b in reads:
            b.readers[t[0]] = max(b.readers.get(t[0], 0), t[1])
        for b in writes:
            b.last_write = t
            b.readers = {}
        self.n_ins += 1
        return t

    def dma(self, q, out, in_, reads=(), writes=(), deps=()):
        lst = self.dsem[q]
        key = lst[self.dsem_rr[q] % len(lst)]
        self.dsem_rr[q] += 1
        tickets = self._deps("dma", reads, writes, deps)
        if self.cnt[key] > 0:
            tickets.append((key, self.cnt[key]))
        waits = self._need(q, tickets)
        self.cnt[key] += 16
        t = (key, self.cnt[key])

        def fn(e, out=out, in_=in_):
            return e.dma_start(out=out, in_=in_)

        self.prog[q].append((waits, fn, (key, 16)))
        for b in reads:
            b.readers[t[0]] = max(b.readers.get(t[0], 0), t[1])
        for b in writes:
            b.last_write = t
            b.readers = {}
        self.n_ins += 1
        return t

    def barrier(self):
        tickets = [(e, self.cnt[e]) for e in self.ENGS if self.cnt[e] > 0]
        for q in self.dsem:
            for key in self.dsem[q]:
                if self.cnt[key] > 0:
                    tickets.append((key, self.cnt[key]))
        for e in self.ENGS:
            waits = self._need(e, tickets)
            self.prog[e].append((waits, None, None))

    def collective(self, src, dst, deps=()):
        waits = self._need("pool", list(deps))
        self.cnt["cc"] += 1
        t = ("cc", self.cnt["cc"])

        def fn(e, src=src, dst=dst):
            return e.collective_compute("AllGather", ALU.bypass, replica_groups=[[0, 1, 2, 3], [4, 5, 6, 7]],
                                        ins=[src], outs=[dst])

        self.prog["pool"].append((waits, fn, ("cc", 1)))
        return t

    def wait_all(self, eng, tickets):
        waits = self._need(eng, tickets)
        self.prog[eng].append((waits, None, None))

    def check(self):
        val = dict(self.start_val)
        pc = {e: 0 for e in self.ENGS}
        progress = True
        while progress:
            progress = False
            for e in self.ENGS:
                lst = self.prog[e]
                while pc[e] < len(lst):
                    waits, fn, inc = lst[pc[e]]
                    if any(val[k] < v for k, v in waits):
                        break
                    if inc is not None:
                        val[inc[0]] += inc[1]
                    pc[e] += 1
                    progress = True
        for e in self.ENGS:
            if pc[e] < len(self.prog[e]):
                waits = self.prog[e][pc[e]][0]
                raise RuntimeError("deadlock: engine %s stuck at %d/%d waiting %s (vals %s)" % (
                    e, pc[e], len(self.prog[e]), waits, {k: val[k] for k, _ in waits}))

    def emit(self):
        self.check()
        nc = self.nc
        prog = self.prog
        sem = self.sem

        def run(e, lst):
            for waits, fn, inc in lst:
                for key, val in waits:
                    e.wait_ge(sem[key], val)
                if fn is not None:
                    ins = fn(e)
                    ins.then_inc(sem[inc[0]], inc[1])

        with nc.Block() as block:
            @block.tensor
            def _(e):
                run(e, prog["pe"])

            @block.scalar
            def _(e):
                run(e, prog["act"])

            @block.vector
            def _(e):
                run(e, prog["dve"])

            @block.gpsimd
            def _(e):
                self.freg = {v: e.to_reg(v) for v in (0.0, -30000.0, -1e9)}
                run(e, prog["pool"])

            @block.sync
            def _(e):
                run(e, prog["sp"])
        self.es.close()
        if self.own_st:
            self.st.es.close()


def bcast_rows(ap1d, nparts=128):
    n = ap1d.shape[0]
    return bass.AP(ap1d.tensor, ap1d.offset, [[0, nparts], [1, n]])


def make_ident(kb, name="ident"):
    ident = kb.sb(name, [128, 128], F32)
    b = Buf(name)
    kb.op("pool", lambda e: e.memset(ident[:], 1.0), writes=[b])
    kb.op("pool", lambda e: e.affine_select(out=ident[:], in_=ident[:], pattern=[[-1, 128]],
                                            compare_op=ALU.is_equal, fill=0.0, base=0,
                                            channel_multiplier=1), reads=[b], writes=[b])
    return ident, b


import os as _os
DBG_NOROUTER = 'norouter' in _os.environ.get('MOE_DBG', '')
DBG_H = 'bigh' in _os.environ.get('MOE_DBG', '')
DBG = _os.environ.get('MOE_DBG', '')


class Stager:
    def __init__(self, kb, n, elems, name="stg", aps=None):
        self.kb = kb
        if aps is None:
            aps = [kb.sb("%s%d" % (name, i), [128, elems], F32)[:] for i in range(n)]
        self.t = aps
        self.b = [Buf("%s%d" % (name, i)) for i in range(n)]
        self.i = 0

    def acquire(self):
        i = self.i % len(self.t)
        self.i += 1
        return i, self.t[i], self.b[i]

    def load(self, dst_ap, dst_bufs, src_ap, a, b, eng="pool", dst_reads=()):
        kb = self.kb
        i = self.i % len(self.t)
        self.i += 1
        view = self.t[i][:, 0:a * b].rearrange("p (a b) -> p a b", a=a)
        kb.dma("sp", view, src_ap, writes=[self.b[i]])
        if eng == "act":
            return kb.op("act", lambda e: e.copy(dst_ap, view), reads=[self.b[i]], writes=list(dst_bufs))
        return kb.op(eng, lambda e: e.tensor_copy(dst_ap, view), reads=[self.b[i]], writes=list(dst_bufs))


def layer_norm_tile(kb, h, hb, out, outb, gt, bt, pb, tmp):
    st, stb, mv, mvb, rs, rsb = tmp
    kb.op("dve", lambda e: e.bn_stats(st[:, 0:6], h[:, 0:512]), reads=[hb], writes=[stb])
    kb.op("dve", lambda e: e.bn_stats(st[:, 6:12], h[:, 512:1024]), reads=[hb, stb], writes=[stb])
    kb.op("dve", lambda e: e.bn_aggr(mv[:], st[:]), reads=[stb], writes=[mvb])
    kb.op("act", lambda e: e.activation(out=rs[:], in_=mv[:, 1:2], func=AF.Sqrt, bias=kb.eps_ln[:], scale=1.0),
          reads=[mvb], writes=[rsb])
    kb.op("dve", lambda e: e.reciprocal(rs[:], rs[:]), reads=[rsb], writes=[rsb])
    kb.op("dve", lambda e: e.tensor_scalar(out, h, mv[:, 0:1], rs[:, 0:1], ALU.subtract, ALU.mult),
          reads=[hb, mvb, rsb], writes=[outb])
    kb.op("pool", lambda e: e.tensor_tensor(out, out, gt[:], ALU.mult), reads=[outb, pb], writes=[outb])
    kb.op("pool", lambda e: e.tensor_tensor(out, out, bt[:], ALU.add), reads=[outb, pb], writes=[outb])


def build_ffn_phase(nc, moe, E=None, st=None, prev=(), pfx="", o_cand_fn=None, xres_fn=None, y_fn=None, gather_fn=None):
    if E is None:
        E = 8 if moe else 1
    H = 3584 if (moe or DBG_H) else 2816
    GB = 2
    NG = (H // 128) // GB
    HG = GB * 128
    NTT = NT // 128
    NCH = NT // 512

    def din(name, shape):
        return nc.dram_tensor(pfx + name, list(shape), F32, kind="ExternalInput").ap()

    fusedm = o_cand_fn is not None
    oT = din("oT", [D_MODEL, NT]) if not fusedm else None
    sel_in = din("sel4", [128, 4]) if fusedm else None
    xres = din("xres", [NT, D_MODEL]) if xres_fn is None else None
    w_o = din("w_o", [D_MODEL, D_MODEL])
    ln1_g = din("ln1_g", [D_MODEL]); ln1_b = din("ln1_b", [D_MODEL])
    ln2_g = din("ln2_g", [D_MODEL]); ln2_b = din("ln2_b", [D_MODEL])
    w_gu = din("w_gu", [E, D_MODEL, 2 * H])
    w_dn = din("w_dn", [E, H, D_MODEL])
    if moe:
        w_r = din("w_r", [D_MODEL, 8])
    y = nc.dram_tensor("y", [NT, D_MODEL], F32, kind="ExternalOutput").ap() if y_fn is None else None

    kb = KB(nc, st, prev, pfx)
    ident, identb = make_ident(kb)
    kb.eps_ln = kb.sb("eps_ln", [128, 1], F32)
    epsb = Buf("eps")
    kb.op("pool", lambda e: e.memset(kb.eps_ln[:], LN_EPS), writes=[epsb])

    acc = kb.sb("acc", [128, NTT, D_MODEL], F32)
    accb = [Buf("acc%d" % i) for i in range(NTT)]
    h1T = kb.sb("h1T", [128, 8, NT], BF16)
    h1Tb = [Buf("h1T%d" % i) for i in range(NTT)]
    stg = Stager(kb, 2, 2048)
    SLOT = 3 * 2048
    wbuf = kb.sb("wbuf", [128, 2 * SLOT], BF16)
    gt = kb.sb("gt", [128, D_MODEL], F32); bt = kb.sb("bt", [128, D_MODEL], F32)
    pb = Buf("lnparams")
    if fusedm:
        xr = [kb.sb("xr0", [128, D_MODEL], F32)] * 2
        xrb = [Buf("xr0")] * 2
        cand = kb.sb("cand", [128, D_MODEL], F32); candb = Buf("cand")
        osel = kb.sb("osel", [128, D_MODEL], F32); oselb = Buf("osel")
        oT_t = kb.sb("oT_t", [128, 8, 128], BF16); oT_tb = Buf("oT_t")
        sel = kb.sb("sel", [128, 4], F32); selb = Buf("sel")
    else:
        xr = [kb.sb("xr%d" % i, [128, D_MODEL], F32) for i in range(2)]
        xrb = [Buf("xr%d" % i) for i in range(2)]
    h1 = [kb.sb("h1_0", [128, D_MODEL], F32)] * 2
    h1b = [Buf("h1_0")] * 2
    st = kb.sb("st", [128, 12], F32); stb = Buf("st")
    mv = kb.sb("mv", [128, 2], F32); mvb = Buf("mv")
    rs = kb.sb("rs", [128, 1], F32); rsb = Buf("rs")
    lntmp = (st, stb, mv, mvb, rs, rsb)
    sil = [kb.sb("sil%d" % i, [128, 512], BF16) for i in range(2)]
    silb = [Buf("sil%d" % i) for i in range(2)]
    aT = [kb.sb("aT%d" % i, [128, GB, 512], BF16) for i in range(2)]
    aTb = [[Buf("aT%d_%d" % (i, j)) for j in range(GB)] for i in range(2)]
    if moe:
        gate = kb.sb("gate", [128, NTT, 8], F32)
        gateb = [Buf("gate%d" % i) for i in range(NTT)]
        wr_sb = kb.sb("wr_sb", [128, 8, 8], F32); wrb = Buf("wr")
        h1Tf = [kb.sb("h1Tf0", [128, 8, 128], F32)] * 2
        h1Tfb = [Buf("h1Tf0")] * 2
        lg = kb.sb("lg", [128, 8], F32); lgb = Buf("lg")
        mx8 = kb.sb("mx8", [128, 8], F32); mx8b = Buf("mx8")
        rt = kb.sb("rt", [128, 4], F32); rtb = Buf("rt")
        ex8 = kb.sb("ex8", [128, 8], F32); ex8b = Buf("ex8")

    pg = [kb.ps("pg%d" % i, [128, 512]) for i in range(2)]; pgb = [Buf("pg%d" % i) for i in range(2)]
    pu = [kb.ps("pu%d" % i, [128, 512]) for i in range(2)]; pub = [Buf("pu%d" % i) for i in range(2)]
    pd = [kb.ps("pd%d" % i, [128, 512]) for i in range(2)]; pdb = [Buf("pd%d" % i) for i in range(2)]
    pt = [kb.ps("pt%d" % i, [128, 512]) for i in range(2)]; ptb = [Buf("pt%d" % i) for i in range(2)]

    kb.dma("sp", gt[:], bcast_rows(ln1_g), writes=[pb])
    kb.dma("sp", bt[:], bcast_rows(ln1_b), writes=[pb])
    if moe and 'nowr' not in DBG:
        kb.dma("sp", wr_sb[:], w_r.rearrange("(kt p) e -> p kt e", p=128), writes=[wrb])
    wo_sb = wbuf[:, 0:8192].rearrange("p (kt n) -> p kt n", kt=8)
    wob = Buf("wo")
    w_o_v = w_o.rearrange("(kt p) n -> p kt n", p=128)
    for i in range(4):
        stg.load(wo_sb[:, 2 * i:2 * i + 2, :], [wob], w_o_v[:, 2 * i:2 * i + 2, :], 2, 1024)
    oT_sb = wbuf[:, 8192:12288].rearrange("p (kt t) -> p kt t", kt=8)
    oTb = Buf("oTs")
    wslot_b = [Buf("wslot0"), Buf("wslot1")]

    if fusedm:
        kb.dma("sp", sel[:], sel_in, writes=[selb])
    else:
        oT_v = oT.rearrange("(kt p) t -> p kt t", p=128)
    xres_v = xres.rearrange("(tt p) d -> tt p d", p=128) if xres is not None else None
    y_v = y.rearrange("(tt p) d -> tt p d", p=128) if y is not None else None

    evac_i = 0
    for c in range(NCH):
        if not fusedm:
            for i in range(2):
                stg.load(oT_sb[:, 4 * i:4 * i + 4, :], [oTb], oT_v[:, 4 * i:4 * i + 4, c * 512:(c + 1) * 512], 4, 512)
        for tl in range(4):
            tt = c * 4 + tl
            r = tt % 2
            if fusedm:
                for rk in range(4):
                    kb.dma("sp", cand[:].rearrange("p (g f) -> p g f", g=4), o_cand_fn(rk, tt), writes=[candb])
                    if rk == 0:
                        kb.op("dve", lambda e: e.tensor_scalar(osel[:], cand[:], sel[:, 0:1], None, ALU.mult),
                              reads=[candb, selb], writes=[oselb])
                    else:
                        kb.op("dve", lambda e, rk=rk: e.scalar_tensor_tensor(
                            out=osel[:], in0=cand[:], scalar=sel[:, rk:rk + 1], in1=osel[:], op0=ALU.mult, op1=ALU.add),
                            reads=[candb, selb, oselb], writes=[oselb])
                for half in range(2):
                    for j in range(4):
                        kt = half * 4 + j
                        kb.op("pe", lambda e, half=half, kt=kt, j=j: e.transpose(
                            pt[half][:, j * 128:(j + 1) * 128], osel[:, kt * 128:(kt + 1) * 128], ident[:]),
                            reads=[oselb, identb], writes=[ptb[half]])
                    kb.op("act", lambda e, half=half: e.copy(
                        oT_t[:, half * 4:(half + 1) * 4, :], pt[half][:].rearrange("p (j t) -> p j t", j=4)),
                        reads=[ptb[half]], writes=[oT_tb])
            kb.dma("sp", xr[r][:], xres_v[tt] if xres_fn is None else xres_fn(tt), writes=[xrb[r]])
            for nh in range(2):
                pi = evac_i % 2
                evac_i += 1
                for kt in range(8):
                    if fusedm:
                        kb.op("pe", lambda e, pi=pi, kt=kt, nh=nh: e.matmul(
                            pd[pi][:], oT_t[:, kt, :],
                            wo_sb[:, kt, nh * 512:(nh + 1) * 512], start=(kt == 0), stop=(kt == 7)),
                            reads=[oT_tb, wob], writes=[pdb[pi]])
                    else:
                        kb.op("pe", lambda e, pi=pi, kt=kt, tl=tl, nh=nh: e.matmul(
                            pd[pi][:], oT_sb[:, kt, tl * 128:(tl + 1) * 128],
                            wo_sb[:, kt, nh * 512:(nh + 1) * 512], start=(kt == 0), stop=(kt == 7)),
                            reads=[oTb, wob], writes=[pdb[pi]])
                kb.op("dve", lambda e, pi=pi, r=r, nh=nh: e.scalar_tensor_tensor(
                    out=xr[r][:, nh * 512:(nh + 1) * 512], in0=xr[r][:, nh * 512:(nh + 1) * 512],
                    scalar=DN_ALPHA, in1=pd[pi][:], op0=ALU.mult, op1=ALU.add),
                    reads=[xrb[r], pdb[pi]], writes=[xrb[r]])
            layer_norm_tile(kb, xr[r][:], xrb[r], h1[r][:], h1b[r], gt, bt, pb, lntmp)
            kb.op("act", lambda e, tt=tt, r=r: e.mul(acc[:, tt, :], h1[r][:], DN_ALPHA),
                  reads=[h1b[r]], writes=[accb[tt]])
            for half in range(2):
                pi = half
                for j in range(4):
                    kt = half * 4 + j
                    kb.op("pe", lambda e, pi=pi, r=r, kt=kt, j=j: e.transpose(
                        pt[pi][:, j * 128:(j + 1) * 128], h1[r][:, kt * 128:(kt + 1) * 128], ident[:]),
                        reads=[h1b[r], identb], writes=[ptb[pi]])
                kb.op("act", lambda e, pi=pi, tt=tt, half=half: e.copy(
                    h1T[:, half * 4:(half + 1) * 4, tt * 128:(tt + 1) * 128],
                    pt[pi][:].rearrange("p (j t) -> p j t", j=4)),
                    reads=[ptb[pi]], writes=[h1Tb[tt]])
                if moe and 'noh1tf' not in DBG:
                    kb.op("act", lambda e, pi=pi, r=r, half=half: e.copy(
                        h1Tf[r][:, half * 4:(half + 1) * 4, :],
                        pt[pi][:].rearrange("p (j t) -> p j t", j=4)),
                        reads=[ptb[pi]], writes=[h1Tfb[r]])
            if moe and DBG_NOROUTER:
                kb.op("pool", lambda e, tt=tt: e.memset(gate[:, tt, :], 0.5), writes=[gateb[tt]])
            if moe and not DBG_NOROUTER:
                for kt in range(8):
                    kb.op("pe", lambda e, r=r, kt=kt: e.matmul(
                        pg[0][:, 0:8], h1Tf[r][:, kt, :], wr_sb[:, kt, :], start=(kt == 0), stop=(kt == 7)),
                        reads=[h1Tfb[r], wrb], writes=[pgb[0]])
                kb.op("dve", lambda e: e.tensor_copy(lg[:], pg[0][:, 0:8]), reads=[pgb[0]], writes=[lgb])
                kb.op("dve", lambda e: e.max(mx8[:], lg[:]), reads=[lgb], writes=[mx8b])
                kb.op("dve", lambda e: e.tensor_scalar(rt[:, 0:1], mx8[:, 0:1], -1.0, None, ALU.mult),
                      reads=[mx8b], writes=[rtb])
                kb.op("act", lambda e: e.activation(out=ex8[:], in_=lg[:], func=AF.Exp, bias=rt[:, 0:1], scale=1.0),
                      reads=[lgb, rtb], writes=[ex8b])
                kb.op("act", lambda e: e.activation(out=rt[:, 1:2], in_=mx8[:, 1:2], func=AF.Exp, bias=rt[:, 0:1], scale=1.0),
                      reads=[mx8b, rtb], writes=[rtb])
                kb.op("dve", lambda e: e.tensor_scalar(rt[:, 2:3], rt[:, 1:2], 1.0, None, ALU.add),
                      reads=[rtb], writes=[rtb])
                kb.op("dve", lambda e: e.reciprocal(rt[:, 2:3], rt[:, 2:3]), reads=[rtb], writes=[rtb])
                kb.op("dve", lambda e: e.tensor_scalar(ex8[:], ex8[:], rt[:, 2:3], None, ALU.mult),
                      reads=[ex8b, rtb], writes=[ex8b])
                kb.op("dve", lambda e, tt=tt: e.scalar_tensor_tensor(
                    out=gate[:, tt, :], in0=lg[:], scalar=mx8[:, 1:2], in1=ex8[:], op0=ALU.is_ge, op1=ALU.mult),
                    reads=[lgb, mx8b, ex8b], writes=[gateb[tt]])

    kb.dma("sp", gt[:], bcast_rows(ln2_g), writes=[pb])
    kb.dma("sp", bt[:], bcast_rows(ln2_b), writes=[pb])

    w_gu_v = w_gu.rearrange("e (kt p) n -> e p kt n", p=128)
    w_dn_v = w_dn.rearrange("e (hb p) n -> e p hb n", p=128)
    gi = 0
    gu_i = 0
    d_i = 0
    sil_i = 0
    for e_ in range(E):
        for g in range(NG):
            sl = gi % 2
            gi += 1
            base = sl * SLOT
            wg_sb = wbuf[:, base:base + 2048].rearrange("p (kt n) -> p kt n", kt=8)
            wu_sb = wbuf[:, base + 2048:base + 4096].rearrange("p (kt n) -> p kt n", kt=8)
            wd_sb = wbuf[:, base + 4096:base + 6144].rearrange("p (hb n) -> p hb n", hb=GB)
            h0 = g * HG
            wb = wslot_b[sl]
            extra = [wob, oTb] if gi <= 2 else []
            stg.load(wg_sb, [wb] + extra, w_gu_v[e_, :, :, h0:h0 + HG], 8, HG)
            stg.load(wu_sb, [wb], w_gu_v[e_, :, :, H + h0:H + h0 + HG], 8, HG)
            stg.load(wd_sb, [wb], w_dn_v[e_, :, g * GB:(g + 1) * GB, :], GB, 1024)
            for c in range(NCH):
                ab = c % 2
                for blk in range(GB):
                    pi = gu_i % 2
                    gu_i += 1
                    for kt in range(8):
                        kb.op("pe", lambda e, pi=pi, kt=kt, blk=blk, c=c, wg_sb=wg_sb: e.matmul(
                            pg[pi][:], wg_sb[:, kt, blk * 128:(blk + 1) * 128],
                            h1T[:, kt, c * 512:(c + 1) * 512], start=(kt == 0), stop=(kt == 7)),
                            reads=[wb] + h1Tb[c * 4:(c + 1) * 4], writes=[pgb[pi]])
                    for kt in range(8):
                        kb.op("pe", lambda e, pi=pi, kt=kt, blk=blk, c=c, wu_sb=wu_sb: e.matmul(
                            pu[pi][:], wu_sb[:, kt, blk * 128:(blk + 1) * 128],
                            h1T[:, kt, c * 512:(c + 1) * 512], start=(kt == 0), stop=(kt == 7)),
                            reads=[wb], writes=[pub[pi]])
                    si = sil_i % 2
                    sil_i += 1
                    kb.op("act", lambda e, pi=pi, si=si: e.activation(out=sil[si][:], in_=pg[pi][:], func=AF.Silu),
                          reads=[pgb[pi]], writes=[silb[si]])
                    kb.op("dve", lambda e, pi=pi, si=si, ab=ab, blk=blk: e.tensor_tensor(
                        aT[ab][:, blk, :], pu[pi][:], sil[si][:], ALU.mult),
                        reads=[pub[pi], silb[si]], writes=[aTb[ab][blk]])
                for tl in range(4):
                    tt = c * 4 + tl
                    for nh in range(2):
                        pi = d_i % 2
                        d_i += 1
                        for blk in range(GB):
                            kb.op("pe", lambda e, pi=pi, ab=ab, blk=blk, tl=tl, nh=nh, wd_sb=wd_sb: e.matmul(
                                pd[pi][:], aT[ab][:, blk, tl * 128:(tl + 1) * 128],
                                wd_sb[:, blk, nh * 512:(nh + 1) * 512], start=(blk == 0), stop=(blk == GB - 1)),
                                reads=[aTb[ab][blk], wb], writes=[pdb[pi]])
                        if moe and 'nostt' not in DBG:
                            kb.op("dve", lambda e, pi=pi, tt=tt, nh=nh, e_=e_: e.scalar_tensor_tensor(
                                out=acc[:, tt, nh * 512:(nh + 1) * 512], in0=pd[pi][:],
                                scalar=gate[:, tt, e_:e_ + 1], in1=acc[:, tt, nh * 512:(nh + 1) * 512],
                                op0=ALU.mult, op1=ALU.add),
                                reads=[pdb[pi], gateb[tt], accb[tt]], writes=[accb[tt]])
                        else:
                            kb.op("dve", lambda e, pi=pi, tt=tt, nh=nh: e.tensor_tensor(
                                acc[:, tt, nh * 512:(nh + 1) * 512], pd[pi][:],
                                acc[:, tt, nh * 512:(nh + 1) * 512], ALU.add),
                                reads=[pdb[pi], accb[tt]], writes=[accb[tt]])

    outs = []
    for tt in range(NTT):
        r = tt % 2
        layer_norm_tile(kb, acc[:, tt, :], accb[tt], xr[r][:], xrb[r], gt, bt, pb, lntmp)
        outs.append(kb.dma("sp", y_v[tt] if y_fn is None else y_fn(tt), xr[r][:], reads=[xrb[r]]))
        if gather_fn is not None and gather_fn(tt) is not None:
            gs_, gd_ = gather_fn(tt)
            outs.append(kb.collective(gs_, gd_, deps=outs[-2:]))
    kb.wait_all("sp", outs)
    tickets = kb.final_tickets()
    kb.emit()
    return tickets


def build_nsa_phase(nc, st=None, prev=(), pfx="", o_dst_fn=None, gather_fn=None):
    T = SEQ
    ONLY = _os.environ.get('NSA_DBG', '')
    NEG = -30000.0

    def din(name, shape):
        return nc.dram_tensor(pfx + name, list(shape), F32, kind="ExternalInput").ap()

    xT = din("xT", [1024, T])
    wall = din("wall", [1024, 780])
    w1 = din("w1", [2, 2048, 256])
    peT = din("peT", [128, 32])
    w2k = din("w2k", [256, 128])
    w2v = din("w2v", [256, 64])
    ov = din("ov", [512, 128])
    dslc = din("dslc", [128, 512])
    dcmp = din("dcmp", [128, 512])
    nslope = din("nslope", [128, 4])
    relslc = din("relslc", [128, 64])
    relcmp = din("relcmp", [128, 16])
    o_out = None if o_dst_fn is not None else nc.dram_tensor("o", [T, 256], F32, kind="ExternalOutput").ap()

    kb = KB(nc, st, prev, pfx)
    ident, identb = make_ident(kb)

    QT = [kb.sb("QT%d" % i, [128, T], BF16) for i in range(2)]
    KsT2 = kb.sb("KsT2", [128, T], BF16)
    KwT2 = kb.sb("KwT2", [128, T], BF16)
    Vs1 = kb.sb("Vs1", [128, 64, 65], BF16)
    Vw1 = kb.sb("Vw1", [128, 64, 65], BF16)
    gates = kb.sb("gates", [128, 64, 12], F32)
    KcT2 = kb.sb("KcT2", [128, 512], BF16)
    VcX = kb.sb("VcX", [128, 4, 193], BF16)
    nsl = kb.sb("nsl", [128, 4], F32)
    misc = kb.sb("misc", [128, 512], F32)
    cpe = kb.sb("cpe", [128, 4], F32)
    QTb = Buf("QT"); KsTb = Buf("KsT"); KwTb = Buf("KwT"); Vsb = Buf("Vs"); Vwb = Buf("Vw")
    gatesb = Buf("gates"); KcTb = Buf("KcT"); VcXb = Buf("VcX"); nslb = Buf("nsl"); miscb = Buf("misc")
    cpeb = Buf("cpe")

    OVLW = 20800
    ovl = kb.sb("ovl", [128, OVLW], F32)
    off = [0]

    def carve(nwords):
        a = off[0]
        off[0] += nwords
        assert off[0] <= OVLW, off[0]
        return ovl[:, a:a + nwords]

    bk = [kb.ps("bk%d" % i, [128, 512]) for i in range(8)]
    bkb = [Buf("bk%d" % i) for i in range(8)]

    w1_sb = carve(4096).bitcast(BF16).rearrange("p (l h) -> p l h", l=32)
    xbf = [carve(1024).bitcast(BF16).rearrange("p (kt t) -> p kt t", kt=8) for _ in range(2)]
    xbfb = [Buf("xbf0"), Buf("xbf1")]
    KVcT = carve(4096).bitcast(BF16)
    KVcTb = Buf("KVcT")
    wall_sb = carve(3120).bitcast(BF16).rearrange("p (kt n) -> p kt n", kt=8)
    wallb = Buf("wall")
    w1b = Buf("w1")
    stg_aps = [carve(2048) for _ in range(2)]
    stg = Stager(kb, 2, 2048, aps=stg_aps)
    u_t = [carve(512) for _ in range(4)]
    ub = [Buf("u%d" % i) for i in range(4)]
    hidT = carve(1024).bitcast(BF16).rearrange("p (s n) -> p s n", s=4)
    hidTb = Buf("hidT")
    peT_sb = carve(16).bitcast(BF16)
    w2k_sb = carve(128).bitcast(BF16).rearrange("p (hh n) -> p hh n", hh=2)
    w2v_sb = carve(64).bitcast(BF16).rearrange("p (hh n) -> p hh n", hh=2)
    smallb = Buf("small")

    kb.dma("sp", nsl[:], nslope, writes=[nslb])
    wall_v = wall.rearrange("(kt p) n -> p kt n", p=128)
    for i in range(4):
        stg.load(wall_sb[:, 2 * i:2 * i + 2, :], [wallb], wall_v[:, 2 * i:2 * i + 2, :], 2, 780)
    kb.op("pool", lambda e: e.memset(Vs1[:, :, 64:65], 1.0), writes=[Vsb])
    kb.op("pool", lambda e: e.memset(Vw1[:, :, 64:65], 1.0), writes=[Vwb])
    kb.op("pool", lambda e: e.memset(VcX[:, :, 64:65], 1.0), writes=[VcXb])
    kb.op("pool", lambda e: e.memset(KcT2[:], 0.0), writes=[KcTb])
    kb.op("pool", lambda e: e.memset(hidT, 0.0), writes=[hidTb])

    xT_v = xT.rearrange("(kt p) t -> p kt t", p=128)
    ev = 0
    for c in range(32):
        xs = c % 2
        tok = slice(c * 256, (c + 1) * 256)
        stg.load(xbf[xs], [xbfb[xs]], xT_v[:, :, tok], 8, 256)
        for oi, (col0, dst, dstb, scale) in enumerate((
                (0, QT[0], QTb, 0.125), (128, QT[1], QTb, 0.125), (256, KVcT, KVcTb, 1.0),
                (384, KsT2, KsTb, 1.0), (512, KwT2, KwTb, 1.0))):
            pi = ev % 2
            ev += 1
            for kt in range(8):
                kb.op("pe", lambda e, pi=pi, kt=kt, xs=xs, col0=col0: e.matmul(
                    bk[pi][:, 0:256], wall_sb[:, kt, col0:col0 + 128], xbf[xs][:, kt, :],
                    start=(kt == 0), stop=(kt == 7)), reads=[wallb, xbfb[xs]], writes=[bkb[pi]])
            if oi % 2 == 0:
                kb.op("act", lambda e, pi=pi, dst=dst, tok=tok, scale=scale: e.mul(dst[:, tok], bk[pi][:, 0:256], scale),
                      reads=[bkb[pi]], writes=[dstb])
            else:
                kb.op("dve", lambda e, pi=pi, dst=dst, tok=tok, scale=scale: e.tensor_scalar(
                    dst[:, tok], bk[pi][:, 0:256], scale, None, ALU.mult), reads=[bkb[pi]], writes=[dstb])
        for tl in range(2):
            tix = c * 2 + tl
            pi = 2 + tl
            for kt in range(8):
                kb.op("pe", lambda e, pi=pi, kt=kt, xs=xs, tl=tl: e.matmul(
                    bk[pi][:, 0:140], xbf[xs][:, kt, tl * 128:(tl + 1) * 128], wall_sb[:, kt, 640:780],
                    start=(kt == 0), stop=(kt == 7)), reads=[wallb, xbfb[xs]], writes=[bkb[pi]])
            kb.op("dve", lambda e, pi=pi, tix=tix: e.tensor_copy(Vs1[:, tix, 0:64], bk[pi][:, 0:64]),
                  reads=[bkb[pi]], writes=[Vsb])
            kb.op("dve", lambda e, pi=pi, tix=tix: e.tensor_copy(Vw1[:, tix, 0:64], bk[pi][:, 64:128]),
                  reads=[bkb[pi]], writes=[Vwb])
            kb.op("dve", lambda e, pi=pi, tix=tix: e.tensor_copy(gates[:, tix, :], bk[pi][:, 128:140]),
                  reads=[bkb[pi]], writes=[gatesb])
            kb.op("act", lambda e, tix=tix: e.activation(out=gates[:, tix, :], in_=gates[:, tix, :],
                                                         func=AF.Sigmoid), reads=[gatesb], writes=[gatesb])

    for i in range(4):
        si, sview, sbuf_ = stg.acquire()
        v3 = sview[:, 0:2048].rearrange("p (l h) -> p l h", l=8)
        for s in range(2):
            kb.dma("sp", v3[s * 64:(s + 1) * 64], w1[s].rearrange("(l d) h -> d l h", d=64)[:, 8 * i:8 * i + 8, :],
                   writes=[sbuf_])
        kb.op("pool", lambda e, i=i, v3=v3: e.tensor_copy(w1_sb[:, 8 * i:8 * i + 8, :], v3), reads=[sbuf_], writes=[w1b])
    kb.dma("sp", misc[:, 0:32], peT, writes=[miscb])
    kb.op("pool", lambda e: e.tensor_copy(peT_sb, misc[:, 0:32]), reads=[miscb], writes=[smallb])
    kb.dma("sp", misc[:, 0:256].rearrange("p (hh n) -> p hh n", hh=2), w2k.rearrange("(hh p) n -> p hh n", p=128),
           writes=[miscb])
    kb.op("pool", lambda e: e.tensor_copy(w2k_sb, misc[:, 0:256].rearrange("p (hh n) -> p hh n", hh=2)),
          reads=[miscb], writes=[smallb])
    kb.dma("sp", misc[:, 0:128].rearrange("p (hh n) -> p hh n", hh=2), w2v.rearrange("(hh p) n -> p hh n", p=128),
           writes=[miscb])
    kb.op("pool", lambda e: e.tensor_copy(w2v_sb, misc[:, 0:128].rearrange("p (hh n) -> p hh n", hh=2)),
          reads=[miscb], writes=[smallb])
    kb.dma("sp", misc[:, 0:512].rearrange("p (i j) -> p i j", i=4), ov.rearrange("(i p) j -> p i j", p=128),
           writes=[miscb])
    kb.op("pool", lambda e: e.tensor_copy(VcX[:, :, 65:193], misc[:, 0:512].rearrange("p (i j) -> p i j", i=4)),
          reads=[miscb], writes=[VcXb])

    for s in range(2):
        ps_ = slice(s * 64, (s + 1) * 64)
        for hh in range(2):
            idx = s * 2 + hh
            ph = 4 + hh
            for l in range(32):
                kb.op("pe", lambda e, ph=ph, l=l, hh=hh, ps_=ps_: e.matmul(
                    bk[ph][:, 0:511], w1_sb[ps_, l, hh * 128:(hh + 1) * 128], KVcT[ps_, l:l + 8161:16],
                    start=(l == 0), stop=(l == 31)), reads=[w1b, KVcTb], writes=[bkb[ph]])
            for l in range(32):
                kb.op("pe", lambda e, l=l, hh=hh, ps_=ps_: e.matmul(
                    bk[6][:, 0:1], w1_sb[ps_, l, hh * 128:(hh + 1) * 128], peT_sb[ps_, l:l + 1],
                    start=(l == 0), stop=(l == 31)), reads=[w1b, smallb], writes=[bkb[6]])
            kb.op("dve", lambda e, idx=idx: e.tensor_copy(cpe[:, idx:idx + 1], bk[6][:, 0:1]), reads=[bkb[6]], writes=[cpeb])
            u, u2, w_, sg = u_t
            kb.op("dve", lambda e, ph=ph, idx=idx, u=u: e.tensor_scalar(u[:, 0:511], bk[ph][:, 0:511], cpe[:, idx:idx + 1], None, ALU.add),
                  reads=[bkb[ph], cpeb], writes=[ub[0]])
            kb.op("dve", lambda e, u=u, u2=u2: e.tensor_tensor(u2[:, 0:511], u[:, 0:511], u[:, 0:511], ALU.mult),
                  reads=[ub[0]], writes=[ub[1]])
            kb.op("dve", lambda e, u2=u2: e.tensor_scalar(u2[:, 0:511], u2[:, 0:511], 0.044715, 1.0, ALU.mult, ALU.add),
                  reads=[ub[1]], writes=[ub[1]])
            kb.op("dve", lambda e, u=u, u2=u2, w_=w_: e.tensor_tensor(w_[:, 0:511], u2[:, 0:511], u[:, 0:511], ALU.mult),
                  reads=[ub[0], ub[1]], writes=[ub[2]])
            kb.op("act", lambda e, w_=w_, sg=sg: e.activation(out=sg[:, 0:511], in_=w_[:, 0:511], func=AF.Sigmoid,
                                                              scale=1.5957691216057308), reads=[ub[2]], writes=[ub[3]])
            kb.op("dve", lambda e, idx=idx, u=u, sg=sg: e.tensor_tensor(hidT[:, idx, 0:511], u[:, 0:511], sg[:, 0:511], ALU.mult),
                  reads=[ub[0], ub[3]], writes=[hidTb])
    for hh in range(2):
        kb.op("pe", lambda e, hh=hh: e.matmul(bk[7][:, 0:511], w2k_sb[:, hh, :], hidT[:, hh, 0:511],
                                              start=(hh == 0), stop=(hh == 1)), reads=[smallb, hidTb], writes=[bkb[7]])
    kb.op("act", lambda e: e.copy(KcT2[:, 0:511], bk[7][:, 0:511]), reads=[bkb[7]], writes=[KcTb])
    for i in range(4):
        for hh in range(2):
            kb.op("pe", lambda e, hh=hh, i=i: e.matmul(bk[6][:, i * 64:(i + 1) * 64], hidT[:, 2 + hh, i * 128:(i + 1) * 128],
                                                       w2v_sb[:, hh, :], start=(hh == 0), stop=(hh == 1)),
                  reads=[smallb, hidTb], writes=[bkb[6]])
    kb.op("dve", lambda e: e.tensor_copy(VcX[:, :, 0:64], bk[6][:, 0:256].rearrange("p (i d) -> p i d", i=4)),
          reads=[bkb[6]], writes=[VcXb])

    kb.barrier()
    off[0] = 0
    Ebig = carve(4096).bitcast(BF16)
    negselT = carve(4096).bitcast(BF16)
    Bs = [carve(512) for _ in range(4)]
    Bc = [carve(512) for _ in range(4)]
    cbs = carve(256).rearrange("p (h m) -> p h m", h=4)
    cbc = carve(64).rearrange("p (h m) -> p h m", h=4)
    tt_ = [carve(512) for _ in range(3)]
    ttb = [Buf("t%d" % i) for i in range(3)]
    scr = tt_[0]
    dtab = tt_[1]
    rtab = tt_[2]
    pT = [carve(256).bitcast(BF16) for _ in range(3)]
    pTb = [Buf("pT%d" % i) for i in range(3)]
    oacc = [carve(1024).rearrange("p (q f) -> p q f", q=4) for _ in range(2)]
    oaccb = [Buf("oacc0"), Buf("oacc1")]
    imp = carve(512).rearrange("p (q j) -> p q j", q=4)
    impb = Buf("imp")
    sc = carve(512).rearrange("p (q j) -> p q j", q=4)
    sc2 = carve(512).rearrange("p (q j) -> p q j", q=4)
    nsel = carve(512).rearrange("p (q j) -> p q j", q=4)
    scb = [Buf("sc%d" % i) for i in range(4)]
    sc2b = [Buf("sc2%d" % i) for i in range(4)]
    nselb = [Buf("nsel%d" % i) for i in range(4)]
    mx = carve(64).rearrange("p (q j) -> p q j", q=4)
    mxb = [Buf("mx%d" % i) for i in range(4)]
    rd = carve(8)
    rdb = Buf("rd")
    constb = Buf("const")
    Eb = Buf("Ebig")
    nsTb = Buf("negselT")

    kb.dma("sp", dtab, dslc, writes=[ttb[1]])
    for h in range(4):
        kb.op("dve", lambda e, h=h: e.tensor_scalar(Bs[h], dtab, nsl[:, h:h + 1], None, ALU.mult),
              reads=[ttb[1], nslb], writes=[constb])
    kb.dma("sp", dtab, dcmp, writes=[ttb[1]])
    for h in range(4):
        kb.op("dve", lambda e, h=h: e.tensor_scalar(Bc[h], dtab, nsl[:, h:h + 1], None, ALU.mult),
              reads=[ttb[1], nslb], writes=[constb])
    kb.dma("sp", rtab[:, 0:64], relslc, writes=[ttb[2]])
    for h in range(4):
        kb.op("dve", lambda e, h=h: e.tensor_scalar(cbs[:, h, :], rtab[:, 0:64], nsl[:, h:h + 1], None, ALU.mult),
              reads=[ttb[2], nslb], writes=[constb])
    kb.dma("sp", rtab[:, 0:16], relcmp, writes=[ttb[2]])
    for h in range(4):
        kb.op("dve", lambda e, h=h: e.tensor_scalar(cbc[:, h, :], rtab[:, 0:16], nsl[:, h:h + 1], None, ALU.mult),
              reads=[ttb[2], nslb], writes=[constb])
    scrb = ttb[0]
    for i in range(16):
        k0 = i * 512
        kb.op("pool", lambda e: e.memset(scr, 1.0), writes=[scrb])
        kb.op("pool", lambda e, k0=k0: e.affine_select(out=scr, in_=scr, pattern=[[1, 512]], compare_op=ALU.is_ge,
                                                       fill=kb.freg[0.0], base=k0, channel_multiplier=-64),
              reads=[scrb], writes=[scrb])
        kb.op("pool", lambda e, k0=k0: e.affine_select(out=scr, in_=scr, pattern=[[-1, 512]], compare_op=ALU.is_ge,
                                                       fill=kb.freg[0.0], base=63 - k0, channel_multiplier=64),
              reads=[scrb], writes=[scrb])
        kb.op("pool", lambda e, k0=k0: e.tensor_copy(Ebig[:, k0:k0 + 512], scr), reads=[scrb], writes=[Eb])

    o_v = o_out.rearrange("(c q p) f -> c p q f", p=128, q=4) if o_out is not None else None
    cnt = {"s": 0, "t": 0, "p": 0}
    outs = []

    SBANK = (0, 1, 7)
    pend = []
    DEPTH = 2

    def pop_one():
        back, post = pend.pop(0)
        back()
        if post is not None:
            post()

    def push(back, post=None):
        pend.append((back, post))
        while len(pend) > DEPTH:
            pop_one()

    def flush():
        while pend:
            pop_one()

    def unit(h, c, KT, ksl, cb_ap, Bt, mask, acc_bank_views, Vrhs, first, last, sel, qs_min=0):
        hp = slice(64 * (h % 2), 64 * (h % 2) + 64)
        q_ap = QT[h // 2][hp, c * 512:(c + 1) * 512]
        si = SBANK[cnt["s"] % 3]; cnt["s"] += 1
        ti = cnt["t"] % 3; cnt["t"] += 1
        kb.op("pe", lambda e: e.matmul(bk[si][:], KT[hp, ksl], q_ap, start=True, stop=(sel is None)),
              reads=[QTb, KsTb, KwTb, KcTb], writes=[bkb[si]])
        if sel is not None:
            kb.op("pe", lambda e: e.matmul(bk[si][:], Ebig[:, ksl], negselT[:, c * 512:(c + 1) * 512],
                                           start=False, stop=True), reads=[Eb, nsTb], writes=[bkb[si]])
        kb.op("dve", lambda e: e.scalar_tensor_tensor(out=tt_[ti], in0=bk[si][:], scalar=cb_ap, in1=Bt,
                                                      op0=ALU.add, op1=ALU.add),
              reads=[bkb[si], constb], writes=[ttb[ti]])
        if mask is not None:
            pat, base, cm = mask
            kb.op("pool", lambda e: e.affine_select(out=tt_[ti], in_=tt_[ti], pattern=pat, compare_op=ALU.is_ge,
                                                    fill=kb.freg[NEG], base=base, channel_multiplier=cm),
                  reads=[ttb[ti]], writes=[ttb[ti]])
        kb.op("act", lambda e: e.activation(out=pT[ti], in_=tt_[ti], func=AF.Exp), reads=[ttb[ti]], writes=[pTb[ti]])

        def back():
            for qs in range(qs_min, 4):
                view, vb = acc_bank_views[qs]
                kb.op("pe", lambda e, qs=qs, view=view: e.matmul(view, pT[ti][:, qs * 128:(qs + 1) * 128], Vrhs,
                                                                 start=first, stop=last),
                      reads=[pTb[ti], Vsb, Vwb, VcXb], writes=[vb])
        return back

    def post_cmp(h, c, oa, oab):
        for qs in range(4):
            kb.op("dve", lambda e, qs=qs: e.tensor_scalar(rd[:, qs:qs + 1], bk[2 + qs][:, 64:65], 1e-30, None, ALU.max),
                  reads=[bkb[2 + qs]], writes=[rdb])
        kb.op("dve", lambda e: e.reciprocal(rd[:, 0:4], rd[:, 0:4]), reads=[rdb], writes=[rdb])
        for qs in range(4):
            if h == 0:
                kb.op("dve", lambda e, qs=qs: e.tensor_scalar(
                    imp[:, qs, :], bk[2 + qs][:, 65:193], rd[:, qs:qs + 1], None, ALU.mult),
                    reads=[bkb[2 + qs], rdb], writes=[impb])
            else:
                kb.op("dve", lambda e, qs=qs: e.scalar_tensor_tensor(
                    out=imp[:, qs, :], in0=bk[2 + qs][:, 65:193], scalar=rd[:, qs:qs + 1], in1=imp[:, qs, :],
                    op0=ALU.mult, op1=ALU.add), reads=[bkb[2 + qs], rdb, impb], writes=[impb])
        kb.op("dve", lambda e: e.tensor_tensor(
            rd[:, 4:8], rd[:, 0:4], gates[:, 4 * c:4 * c + 4, h * 3 + 0], ALU.mult),
            reads=[rdb, gatesb], writes=[rdb])
        for qs in range(4):
            if ONLY in ('slc', 'win'):
                kb.op("dve", lambda e, qs=qs: e.tensor_scalar(
                    oa[:, qs, h * 64:(h + 1) * 64], bk[2 + qs][:, 0:64], 0.0, None, ALU.mult),
                    reads=[bkb[2 + qs], rdb], writes=[oab])
            else:
                kb.op("dve", lambda e, qs=qs: e.tensor_scalar(
                    oa[:, qs, h * 64:(h + 1) * 64], bk[2 + qs][:, 0:64], rd[:, 4 + qs:5 + qs], None, ALU.mult),
                    reads=[bkb[2 + qs], rdb], writes=[oab])

    def post_sw(h, c, br, oa, oab):
        for qs in range(4):
            kb.op("dve", lambda e, qs=qs: e.tensor_scalar(rd[:, qs:qs + 1], bk[2 + qs][:, 64:65], 1e-30, None, ALU.max),
                  reads=[bkb[2 + qs]], writes=[rdb])
        kb.op("dve", lambda e: e.reciprocal(rd[:, 0:4], rd[:, 0:4]), reads=[rdb], writes=[rdb])
        kb.op("dve", lambda e: e.tensor_tensor(
            rd[:, 0:4], rd[:, 0:4], gates[:, 4 * c:4 * c + 4, h * 3 + br], ALU.mult),
            reads=[rdb, gatesb], writes=[rdb])
        for qs in range(4):
            if ONLY and ONLY != ('win' if br == 2 else 'slc'):
                kb.op("dve", lambda e, qs=qs: e.tensor_copy(rd[:, 4 + qs:5 + qs], bk[2 + qs][:, 64:65]),
                      reads=[bkb[2 + qs]], writes=[rdb])
                continue
            kb.op("dve", lambda e, qs=qs: e.scalar_tensor_tensor(
                out=oa[:, qs, h * 64:(h + 1) * 64], in0=bk[2 + qs][:, 0:64], scalar=rd[:, qs:qs + 1],
                in1=oa[:, qs, h * 64:(h + 1) * 64], op0=ALU.mult, op1=ALU.add),
                reads=[bkb[2 + qs], rdb, oab], writes=[oab])

    def selection(c):
        for qs in range(4):
            t0 = 512 * c + 128 * qs
            kb.op("pool", lambda e, qs=qs, t0=t0: e.affine_select(
                out=sc[:, qs, :], in_=imp[:, qs, :], pattern=[[-64, 128]], compare_op=ALU.is_ge, fill=kb.freg[-1e9],
                base=t0 - 128, channel_multiplier=1), reads=[impb], writes=[scb[qs]])
            kb.op("pool", lambda e, qs=qs: e.memset(sc[:, qs, 0:1], -1e9), writes=[scb[qs]])
            kb.op("dve", lambda e, qs=qs: e.max(mx[:, qs, 0:8], sc[:, qs, :]), reads=[scb[qs]], writes=[mxb[qs]])
            kb.op("dve", lambda e, qs=qs: e.match_replace(sc2[:, qs, :], mx[:, qs, 0:8], sc[:, qs, :], -2e9),
                  reads=[scb[qs], mxb[qs]], writes=[sc2b[qs]])
            kb.op("dve", lambda e, qs=qs: e.max(mx[:, qs, 8:16], sc2[:, qs, :]), reads=[sc2b[qs]], writes=[mxb[qs]])
            kb.op("dve", lambda e, qs=qs: e.tensor_scalar(nsel[:, qs, :], sc[:, qs, :], mx[:, qs, 12:13], NEG,
                                                          ALU.is_lt, ALU.mult),
                  reads=[scb[qs], mxb[qs]], writes=[nselb[qs]])
            kb.op("pool", lambda e, qs=qs, t0=t0: e.affine_select(
                out=nsel[:, qs, :], in_=nsel[:, qs, :], pattern=[[-64, 128]], compare_op=ALU.is_ge, fill=kb.freg[0.0],
                base=t0 - 128, channel_multiplier=1), reads=[nselb[qs]], writes=[nselb[qs]])
            kb.op("pool", lambda e, qs=qs: e.memset(nsel[:, qs, 0:1], 0.0), writes=[nselb[qs]])
            kb.op("pe", lambda e, qs=qs: e.transpose(bk[6][:, qs * 128:(qs + 1) * 128], nsel[:, qs, :], ident[:]),
                  reads=[nselb[qs], identb], writes=[bkb[6]])
        kb.op("act", lambda e: e.copy(negselT[:, c * 512:(c + 1) * 512], bk[6][:]), reads=[bkb[6]], writes=[nsTb])

    for c in range(16):
        oa = oacc[c % 2]
        oab = oaccb[c % 2]
        for h in range(4):
            views = [(bk[2 + qs][:, 0:193], bkb[2 + qs]) for qs in range(4)]
            ni = c // 4 + 1
            for i in range(ni):
                mask = None
                if i >= c // 4 - 1:
                    mask = ([[1, 512]], 512 * c - 2048 * i - 31, -16)
                bk_ = unit(h, c, KcT2, slice(i * 128, (i + 1) * 128), cbc[:, h, c - 4 * i:c - 4 * i + 1], Bc[h], mask,
                           views, VcX[:, i, :], i == 0, i == ni - 1, None)
                push(bk_, (lambda h=h, c=c, oa=oa, oab=oab: post_cmp(h, c, oa, oab)) if i == ni - 1 else None)
        first_win = True
        for br, KT, V1 in ((2, KwT2, Vw1), (1, KsT2, Vs1)):
            if br == 1:
                pass
            for h in range(4):
                views = [(bk[2 + qs][:, 0:65], bkb[2 + qs]) for qs in range(4)]
                j0 = max(0, 4 * c - 4) if br == 2 else 0
                j1 = 4 * c + 3
                for j in range(j0, j1 + 1):
                    rel = 512 * c - 128 * j
                    qs_min = 0
                    if j >= 4 * c:
                        mask = ([[1, 512]], rel, -1)
                        qs_min = j - 4 * c
                    elif br == 2:
                        mask = ([[-1, 512]], 511 - rel, 1)
                    else:
                        mask = None
                    m = 4 * c - j + 3
                    if first_win:
                        flush()
                        selection(c)
                        first_win = False
                    bk_ = unit(h, c, KT, slice(j * 128, (j + 1) * 128), cbs[:, h, m:m + 1], Bs[h], mask, views,
                               V1[:, j, :], j == j0, j == j1, True if br == 1 else None, qs_min)
                    push(bk_, (lambda h=h, c=c, br=br, oa=oa, oab=oab: post_sw(h, c, br, oa, oab)) if j == j1 else None)
        flush()
        outs.append(kb.dma("sp", o_v[c] if o_dst_fn is None else o_dst_fn(c), oa, reads=[oab]))
        if gather_fn is not None and gather_fn(c) is not None:
            gs_, gd_ = gather_fn(c)
            outs.append(kb.collective(gs_, gd_, deps=outs[-2:]))
    kb.wait_all("sp", outs)
    tickets = kb.final_tickets()
    kb.emit()
    return tickets


def build_gla_phase(nc, st=None, prev=(), pfx="", x_src_fn=None, o_dst_fn=None, gather_fn=None):
    T = SEQ
    QSCALE = 128.0 ** -0.5

    def din(name, shape):
        return nc.dram_tensor(pfx + name, list(shape), F32, kind="ExternalInput").ap()

    xT = din("xT", [1024, T]) if x_src_fn is None else None
    wall = din("wall", [1024, 784])
    wg2 = din("wg2", [16, 128])
    bg2 = din("bg2", [1, 128])
    hng = din("hng", [256])
    lblk = din("lblk", [128, 128])
    ublk = din("ublk", [128, 128])
    o_out = None if o_dst_fn is not None else nc.dram_tensor("o", [T, 256], F32, kind="ExternalOutput").ap()

    kb = KB(nc, st, prev, pfx)
    if x_src_fn is not None:
        ident, identb = make_ident(kb)
        xtok = [kb.sb("xtok%d" % i, [128, D_MODEL], F32) for i in range(2)]
        xtokb = [Buf("xtok0"), Buf("xtok1")]
    one1 = kb.sb("one1", [128, 1], F32)
    epsr = kb.sb("epsr", [128, 1], F32)
    cb = Buf("consts")
    kb.op("pool", lambda e: e.memset(one1[:], 1.0), writes=[cb])
    kb.op("pool", lambda e: e.memset(epsr[:], RMS_EPS), writes=[cb])

    wall_sb = kb.sb("wall_sb", [128, 8, 784], BF16); wallb = Buf("wall")
    stg = Stager(kb, 2, 2048)
    xbf = [kb.sb("xbf%d" % i, [128, 8, 256], BF16) for i in range(2)]
    xbfb = [Buf("xbf0"), Buf("xbf1")]
    L01 = kb.sb("L01", [128, 128], F32)
    LS = kb.sb("LS", [128, 128], F32)
    US = kb.sb("US", [128, 128], F32)
    hn = kb.sb("hn", [128, 256], F32)
    wg2_sb = kb.sb("wg2_sb", [16, 128], BF16)
    bg2_sb = kb.sb("bg2_sb", [1, 128], BF16)
    ones_bf = kb.sb("ones_bf", [1, 128], BF16)
    misc = kb.sb("misc", [128, 128], F32); miscb = Buf("misc")

    qT_sb = kb.sb("qT_sb", [128, 256], F32); qTb = Buf("qT")
    kT_sb = kb.sb("kT_sb", [128, 256], F32); kTb = Buf("kT")
    alT_sb = kb.sb("alT_sb", [16, 256], BF16); alTb = Buf("alT")
    v_bf = kb.sb("v_bf", [128, 256], BF16); vb = Buf("v")
    k_tok = kb.sb("k_tok", [128, 128], F32); ktb = Buf("ktok")
    gs = kb.sb("gs", [128, 256], F32); gsb = Buf("gs")
    e1 = kb.sb("e1", [128, 128], F32); e1b = Buf("e1")
    la = kb.sb("la", [128, 128], F32); lab = Buf("la")
    bT_sb = kb.sb("bT_sb", [128, 2, 64], F32); bTb = Buf("bT")
    bd = kb.sb("bd", [128, 2, 64], F32); bdb = Buf("bd")
    eg = kb.sb("eg", [128, 128], F32); egb = Buf("eg")
    ieg = kb.sb("ieg", [128, 128], F32); iegb = Buf("ieg")
    eb = kb.sb("eb", [128, 128], F32); ebb = Buf("eb")
    erb = kb.sb("erb", [128, 128], F32); erbb = Buf("erb")
    qgT = kb.sb("qgT", [128, 128], BF16); qgb = Buf("qg")
    kgT = kb.sb("kgT", [128, 128], BF16); kgb = Buf("kg")
    qbP = [kb.sb("qbP%d" % i, [128, 128], BF16) for i in range(2)]; qbPb = [Buf("qbP0"), Buf("qbP1")]
    kbt = kb.sb("kbt", [128, 128], BF16); kbtb = Buf("kbt")
    AT = kb.sb("AT", [128, 128], BF16); ATb = Buf("AT")
    dec = kb.sb("dec", [128, 2], F32); decb = Buf("dec")
    S32 = kb.sb("S32", [128, 256], F32); S32b = Buf("S32")
    Sbf = [kb.sb("Sbf%d" % i, [128, 256], BF16) for i in range(3)]; Sbfb = [Buf("Sbf%d" % i) for i in range(3)]
    st = kb.sb("st", [128, 6], F32); stb = Buf("st")
    mv = kb.sb("mv", [128, 2], F32); mvb = Buf("mv")
    rs = kb.sb("rs", [128, 2], F32); rsb = Buf("rs")
    ot = [kb.sb("ot%d" % i, [128, 256], F32) for i in range(2)]; otb = [Buf("ot0"), Buf("ot1")]

    bk = [kb.ps("bk%d" % i, [128, 512]) for i in range(8)]
    bkb = [Buf("bk%d" % i) for i in range(8)]

    wall_v = wall.rearrange("(kt p) n -> p kt n", p=128)
    for i in range(4):
        stg.load(wall_sb[:, 2 * i:2 * i + 2, :], [wallb], wall_v[:, 2 * i:2 * i + 2, :], 2, 784)
    kb.dma("sp", L01[:], lblk, writes=[cb])
    kb.op("dve", lambda e: e.tensor_scalar(LS[:], L01[:], -1.0 / 16.0, None, ALU.mult), reads=[cb], writes=[cb])
    kb.dma("sp", misc[:], ublk, writes=[miscb])
    kb.op("dve", lambda e: e.tensor_scalar(US[:], misc[:], -1.0 / 16.0, None, ALU.mult), reads=[miscb], writes=[cb])
    kb.dma("sp", hn[:], bcast_rows(hng), writes=[cb])
    kb.dma("sp", misc[0:16, :], wg2, writes=[miscb])
    kb.op("dve", lambda e: e.tensor_copy(wg2_sb[:], misc[0:16, :]), reads=[miscb], writes=[cb])
    kb.dma("sp", misc[0:1, :], bg2, writes=[miscb])
    kb.op("dve", lambda e: e.tensor_copy(bg2_sb[:], misc[0:1, :]), reads=[miscb], writes=[cb])
    kb.op("pool", lambda e: e.memset(ones_bf[:], 1.0), writes=[cb])
    kb.op("pool", lambda e: e.memset(qbP[0][:], 0.0), writes=[qbPb[0]])
    kb.op("pool", lambda e: e.memset(qbP[1][:], 0.0), writes=[qbPb[1]])
    kb.op("pool", lambda e: e.memset(S32[:], 0.0), writes=[S32b])

    xT_v = xT.rearrange("(kt p) t -> p kt t", p=128) if x_src_fn is None else None
    o_v = o_out.rearrange("(m p) f -> m p f", p=128) if o_out is not None else None
    outs = []
    s_i = 0
    have_S = False
    for c in range(32):
        xs = c % 2
        if x_src_fn is None:
            stg.load(xbf[xs][:], [xbfb[xs]], xT_v[:, :, c * 256:(c + 1) * 256], 8, 256)
        else:
            for tl in range(2):
                xi = (c * 2 + tl) % 2
                kb.dma("sp", xtok[xi][:], x_src_fn(c * 2 + tl), writes=[xtokb[xi]])
                for half in range(2):
                    for j in range(4):
                        kt = half * 4 + j
                        kb.op("pe", lambda e, half=half, j=j, kt=kt, xi=xi: e.transpose(
                            bk[half][:, j * 128:(j + 1) * 128], xtok[xi][:, kt * 128:(kt + 1) * 128], ident[:]),
                            reads=[xtokb[xi], identb], writes=[bkb[half]])
                    kb.op("act", lambda e, half=half, xs=xs, tl=tl: e.copy(
                        xbf[xs][:, half * 4:(half + 1) * 4, tl * 128:(tl + 1) * 128],
                        bk[half][:].rearrange("p (j t) -> p j t", j=4)),
                        reads=[bkb[half]], writes=[xbfb[xs]])
        for kt in range(8):
            kb.op("pe", lambda e, kt=kt, xs=xs: e.matmul(bk[0][:, 0:256], wall_sb[:, kt, 0:128], xbf[xs][:, kt, :],
                                                         start=(kt == 0), stop=(kt == 7)),
                  reads=[wallb, xbfb[xs]], writes=[bkb[0]])
        kb.op("dve", lambda e: e.tensor_scalar(qT_sb[:], bk[0][:, 0:256], QSCALE, None, ALU.mult),
              reads=[bkb[0]], writes=[qTb])
        for kt in range(8):
            kb.op("pe", lambda e, kt=kt, xs=xs: e.matmul(bk[1][:, 0:256], wall_sb[:, kt, 128:256], xbf[xs][:, kt, :],
                                                         start=(kt == 0), stop=(kt == 7)),
                  reads=[wallb, xbfb[xs]], writes=[bkb[1]])
        kb.op("act", lambda e: e.copy(kT_sb[:], bk[1][:, 0:256]), reads=[bkb[1]], writes=[kTb])
        for kt in range(8):
            kb.op("pe", lambda e, kt=kt, xs=xs: e.matmul(bk[2][0:16, 0:256], wall_sb[:, kt, 768:784], xbf[xs][:, kt, :],
                                                         start=(kt == 0), stop=(kt == 7)),
                  reads=[wallb, xbfb[xs]], writes=[bkb[2]])
        kb.op("act", lambda e: e.copy(alT_sb[:], bk[2][0:16, 0:256]), reads=[bkb[2]], writes=[alTb])
        for tl in range(2):
            m = c * 2 + tl
            tk = slice(tl * 128, (tl + 1) * 128)
            for kt in range(8):
                kb.op("pe", lambda e, kt=kt, xs=xs, tk=tk: e.matmul(bk[3][:, 0:256], xbf[xs][:, kt, tk], wall_sb[:, kt, 256:512],
                                                                    start=(kt == 0), stop=(kt == 7)),
                      reads=[wallb, xbfb[xs]], writes=[bkb[3]])
            for kt in range(8):
                kb.op("pe", lambda e, kt=kt, xs=xs, tk=tk: e.matmul(bk[3][:, 256:384], xbf[xs][:, kt, tk], wall_sb[:, kt, 128:256],
                                                                    start=(kt == 0), stop=(kt == 7)),
                      reads=[wallb, xbfb[xs]], writes=[bkb[3]])
            kb.op("dve", lambda e: e.tensor_copy(v_bf[:], bk[3][:, 0:256]), reads=[bkb[3]], writes=[vb])
            kb.op("dve", lambda e: e.tensor_copy(k_tok[:], bk[3][:, 256:384]), reads=[bkb[3]], writes=[ktb])
            for kt in range(8):
                kb.op("pe", lambda e, kt=kt, xs=xs, tk=tk: e.matmul(bk[4][:, 0:256], xbf[xs][:, kt, tk], wall_sb[:, kt, 512:768],
                                                                    start=(kt == 0), stop=(kt == 7)),
                      reads=[wallb, xbfb[xs]], writes=[bkb[4]])
            kb.op("act", lambda e: e.activation(out=gs[:], in_=bk[4][:, 0:256], func=AF.Silu), reads=[bkb[4]], writes=[gsb])
            kb.op("pool", lambda e: e.tensor_tensor(gs[:], gs[:], hn[:], ALU.mult), reads=[gsb, cb], writes=[gsb])
            kb.op("pe", lambda e, tk=tk: e.matmul(bk[2][:, 256:384], alT_sb[:, tk], wg2_sb[:], start=True, stop=False),
                  reads=[alTb, cb], writes=[bkb[2]])
            kb.op("pe", lambda e: e.matmul(bk[2][:, 256:384], ones_bf[:], bg2_sb[:], start=False, stop=True),
                  reads=[cb], writes=[bkb[2]])
            kb.op("act", lambda e: e.activation(out=e1[:], in_=bk[2][:, 256:384], func=AF.Exp, scale=-1.0),
                  reads=[bkb[2]], writes=[e1b])
            kb.op("act", lambda e: e.activation(out=la[:], in_=e1[:], func=AF.Ln, bias=one1[:], scale=1.0),
                  reads=[e1b, cb], writes=[lab])
            kb.op("pe", lambda e: e.matmul(bk[5][:, 0:128], la[:], LS[:], start=True, stop=True),
                  reads=[lab, cb], writes=[bkb[5]])
            kb.op("pe", lambda e: e.matmul(bk[5][:, 128:256], US[:], la[:], start=True, stop=True),
                  reads=[lab, cb], writes=[bkb[5]])
            kb.op("act", lambda e: e.copy(bT_sb[:], bk[5][:, 0:128].rearrange("p (c t) -> p c t", c=2)),
                  reads=[bkb[5]], writes=[bTb])
            kb.op("act", lambda e: e.activation(out=erb[:], in_=bk[5][:, 128:256], func=AF.Exp), reads=[bkb[5]], writes=[erbb])
            for cc in range(2):
                kb.op("dve", lambda e, cc=cc: e.tensor_scalar(bd[:, cc, :], bT_sb[:, cc, :], bT_sb[:, cc, 32:33], None,
                                                              ALU.subtract), reads=[bTb], writes=[bdb])
            kb.op("act", lambda e: e.activation(out=eg[:], in_=bd[:].rearrange("p c t -> p (c t)"), func=AF.Exp),
                  reads=[bdb], writes=[egb])
            kb.op("act", lambda e: e.activation(out=eb[:], in_=bT_sb[:].rearrange("p c t -> p (c t)"), func=AF.Exp),
                  reads=[bTb], writes=[ebb])
            kb.op("act", lambda e: e.activation(out=dec[:], in_=bT_sb[:, :, 63], func=AF.Exp), reads=[bTb], writes=[decb])
            kb.op("dve", lambda e: e.reciprocal(ieg[:], eg[:]), reads=[egb], writes=[iegb])
            kb.op("dve", lambda e, tk=tk: e.tensor_tensor(qgT[:], qT_sb[:, tk], eg[:], ALU.mult), reads=[qTb, egb], writes=[qgb])
            kb.op("dve", lambda e, tk=tk: e.tensor_tensor(kgT[:], kT_sb[:, tk], ieg[:], ALU.mult), reads=[kTb, iegb], writes=[kgb])
            kb.op("dve", lambda e, tk=tk: e.tensor_tensor(qbP[0][:, 0:64], qT_sb[:, tk.start:tk.start + 64], eb[:, 0:64], ALU.mult),
                  reads=[qTb, ebb], writes=[qbPb[0]])
            kb.op("dve", lambda e, tk=tk: e.tensor_tensor(qbP[1][:, 64:128], qT_sb[:, tk.start + 64:tk.start + 128], eb[:, 64:128], ALU.mult),
                  reads=[qTb, ebb], writes=[qbPb[1]])
            kb.op("dve", lambda e: e.tensor_tensor(kbt[:], k_tok[:], erb[:], ALU.mult), reads=[ktb, erbb], writes=[kbtb])
            kb.op("pe", lambda e: e.matmul(bk[6][:, 0:128], kgT[:], qgT[:], start=True, stop=True),
                  reads=[kgb, qgb], writes=[bkb[6]])
            kb.op("dve", lambda e: e.tensor_tensor(AT[:], bk[6][:, 0:128], L01[:], ALU.mult), reads=[bkb[6], cb], writes=[ATb])
            s_prev = s_i
            terms = [(AT, ATb, v_bf, vb)]
            if have_S:
                terms.append((qbP[0], qbPb[0], Sbf[s_prev % 3], Sbfb[s_prev % 3]))
            for cc in range(2):
                pr = slice(cc * 64, (cc + 1) * 64)
                kb.op("pe", lambda e, pr=pr: e.matmul(bk[6][:, 128:384], kbt[pr, :], v_bf[pr, :], start=True, stop=True),
                      reads=[kbtb, vb], writes=[bkb[6]])
                kb.op("dve", lambda e, cc=cc: e.scalar_tensor_tensor(out=S32[:], in0=S32[:], scalar=dec[:, cc:cc + 1],
                                                                     in1=bk[6][:, 128:384], op0=ALU.mult, op1=ALU.add),
                      reads=[S32b, decb, bkb[6]], writes=[S32b])
                s_i += 1
                kb.op("act", lambda e, si=s_i: e.copy(Sbf[si % 3][:], S32[:]), reads=[S32b], writes=[Sbfb[s_i % 3]])
                if cc == 0:
                    terms.append((qbP[1], qbPb[1], Sbf[s_i % 3], Sbfb[s_i % 3]))
            have_S = True
            for ti, (l_, lb_, r_, rb_) in enumerate(terms):
                kb.op("pe", lambda e, l_=l_, r_=r_, ti=ti, n=len(terms): e.matmul(
                    bk[7][:, 0:256], l_[:], r_[:], start=(ti == 0), stop=(ti == n - 1)),
                    reads=[lb_, rb_], writes=[bkb[7]])
            kb.op("dve", lambda e: e.bn_stats(st[:], bk[7][:, 0:256]), reads=[bkb[7]], writes=[stb])
            kb.op("dve", lambda e: e.bn_aggr(mv[:], st[:]), reads=[stb], writes=[mvb])
            kb.op("dve", lambda e: e.tensor_tensor(rs[:, 0:1], mv[:, 0:1], mv[:, 0:1], ALU.mult), reads=[mvb], writes=[rsb])
            kb.op("dve", lambda e: e.tensor_tensor(rs[:, 0:1], rs[:, 0:1], mv[:, 1:2], ALU.add), reads=[mvb, rsb], writes=[rsb])
            kb.op("act", lambda e: e.activation(out=rs[:, 1:2], in_=rs[:, 0:1], func=AF.Sqrt, bias=epsr[:], scale=1.0),
                  reads=[rsb, cb], writes=[rsb])
            kb.op("dve", lambda e: e.reciprocal(rs[:, 1:2], rs[:, 1:2]), reads=[rsb], writes=[rsb])
            oi = m % 2
            kb.op("dve", lambda e, oi=oi: e.scalar_tensor_tensor(out=ot[oi][:], in0=bk[7][:, 0:256], scalar=rs[:, 1:2],
                                                                 in1=gs[:], op0=ALU.mult, op1=ALU.mult),
                  reads=[bkb[7], rsb, gsb], writes=[otb[oi]])
            outs.append(kb.dma("sp", o_v[m] if o_dst_fn is None else o_dst_fn(m), ot[oi][:], reads=[otb[oi]]))
            if gather_fn is not None and gather_fn(m) is not None:
                gs_, gd_ = gather_fn(m)
                outs.append(kb.collective(gs_, gd_, deps=outs[-8:]))
    kb.wait_all("sp", outs)
    tickets = kb.final_tickets()
    kb.emit()
    return tickets


def build_fused(nc):
    def internal(name, shape):
        return nc.dram_tensor(name, list(shape), F32, kind="Internal").ap()

    st = SemState(nc)
    oA = [internal("i_oA%d" % k, [1024, 256]) for k in range(8)]
    gA = [internal("i_gA%d" % k, [4 * 1024, 256]) for k in range(8)]
    x1 = [internal("i_x1%d" % k, [256, D_MODEL]) for k in range(8)]
    gX = [internal("i_gX%d" % k, [4 * 256, D_MODEL]) for k in range(8)]
    oC = [internal("i_oC%d" % k, [1024, 256]) for k in range(8)]
    gC = [internal("i_gC%d" % k, [4 * 1024, 256]) for k in range(8)]
    y = nc.dram_tensor("y", [NT, D_MODEL], F32, kind="ExternalOutput").ap()

    def mixer_cand(g_list):
        def fn(rk, tt):
            t0 = rk * NT + tt * 128
            k, i = t0 // 1024, t0 % 1024
            return g_list[k].rearrange("(g t) f -> t g f", g=4)[i:i + 128]
        return fn

    t = build_nsa_phase(
        nc, st, (), "a_",
        o_dst_fn=lambda c: oA[c // 2][(c % 2) * 512:(c % 2) * 512 + 512, :].rearrange("(q p) f -> p q f", p=128),
        gather_fn=lambda c: (oA[c // 2], gA[c // 2]) if c % 2 == 1 else None)
    t = build_ffn_phase(
        nc, False, 1, st, t, "b_", o_cand_fn=mixer_cand(gA),
        y_fn=lambda tt: x1[tt // 2][(tt % 2) * 128:(tt % 2) * 128 + 128, :],
        gather_fn=lambda tt: (x1[tt // 2], gX[tt // 2]) if tt % 2 == 1 else None)

    def x_src_fn(m):
        r, k, i = m // 16, (m % 16) // 2, (m % 2) * 128
        return gX[k][r * 256 + i:r * 256 + i + 128, :]

    t = build_gla_phase(
        nc, st, t, "c_", x_src_fn=x_src_fn,
        o_dst_fn=lambda m: oC[m // 8][(m % 8) * 128:(m % 8) * 128 + 128, :],
        gather_fn=lambda m: (oC[m // 8], gC[m // 8]) if m % 8 == 7 else None)
    t = build_ffn_phase(
        nc, True, 8, st, t, "d_", o_cand_fn=mixer_cand(gC),
        xres_fn=lambda tt: x1[tt // 2][(tt % 2) * 128:(tt % 2) * 128 + 128, :],
        y_fn=lambda tt: y[tt * 128:(tt + 1) * 128, :])
    st.es.close()
    return nc


_PROG_CACHE = {}


def _get_prog(key, builder):
    if key not in _PROG_CACHE:
        nc = bass.Bass("TRN2", target_bir_lowering=False)
        builder(nc)
        _PROG_CACHE[key] = nc
    return _PROG_CACHE[key]


def run_ffn_phase(moe, oT_list, xres_list, w_o, ln1_g, ln1_b, ln2_g, ln2_b, w_gu, w_dn, w_r=None):
    E = w_gu.shape[0]
    nc = _get_prog("ffn_%s_%d" % (moe, E), lambda nc: build_ffn_phase(nc, moe, E))
    in_maps = []
    for c in range(NCORES):
        m = {"oT": oT_list[c], "xres": xres_list[c], "w_o": w_o, "ln1_g": ln1_g, "ln1_b": ln1_b,
             "ln2_g": ln2_g, "ln2_b": ln2_b, "w_gu": w_gu, "w_dn": w_dn}
        if moe:
            m["w_r"] = w_r
        in_maps.append(m)
    res = run_bass_kernel_spmd(nc, in_maps, core_ids=list(range(NCORES)))
    return [r["y"] for r in res.results]


def _nsa_consts():
    kl = np.arange(128, dtype=np.float32)[:, None]
    ql = np.arange(512, dtype=np.float32)[None, :]
    dslc = np.ascontiguousarray(np.broadcast_to(ql - kl, (128, 512)).astype(np.float32))
    dcmp = np.ascontiguousarray(np.broadcast_to(ql - 16.0 * kl, (128, 512)).astype(np.float32))
    relslc = np.ascontiguousarray(np.broadcast_to((128.0 * np.arange(64) - 384.0)[None, :], (128, 64)).astype(np.float32))
    relcmp = np.ascontiguousarray(np.broadcast_to((512.0 * np.arange(16) - 31.0)[None, :], (128, 16)).astype(np.float32))
    n_cmp = (SEQ - 32) // 16 + 1
    cs = np.arange(n_cmp) * 16
    ce = cs + 31
    bs = np.arange(SEQ // 64) * 64
    be = bs + 63
    ov = np.zeros((512, 128), np.float32)
    ov[:n_cmp] = ((cs[:, None] <= be[None]) & (ce[:, None] >= bs[None])).astype(np.float32)
    return dslc, dcmp, relslc, relcmp, ov


def run_nsa_phase(x, w_in, pe_k, pe_v, wk1, wk2, wv1, wv2):
    nc = _get_prog("nsa", build_nsa_phase)
    dslc, dcmp, relslc, relcmp, ov = _nsa_consts()
    slopes = (2.0 ** (-8.0 * (np.arange(16, dtype=np.float32) + 1.0) / 16)).astype(np.float32)
    peT = np.ascontiguousarray(np.concatenate([pe_k.T, pe_v.T], axis=0))
    w1 = np.ascontiguousarray(np.stack([wk1, wv1]))
    w2k = np.ascontiguousarray(np.concatenate([wk2, wk2], axis=1))
    xTs = [np.ascontiguousarray(x[b].T) for b in range(BATCH)]
    in_maps = []
    for core in range(NCORES):
        b, g = core // 4, core % 4
        kvcol = lambda i: w_in[:, 1024 + i * 256 + g * 64:1024 + i * 256 + (g + 1) * 64]
        wall = np.concatenate([
            w_in[:, g * 256:(g + 1) * 256], kvcol(0), kvcol(1), kvcol(2), kvcol(2), kvcol(4), kvcol(4),
            kvcol(3), kvcol(5), w_in[:, 2560 + g * 12:2560 + (g + 1) * 12]], axis=1)
        nsl = np.ascontiguousarray(np.broadcast_to(-slopes[g * 4:(g + 1) * 4][None, :], (128, 4)).astype(np.float32))
        in_maps.append({"xT": xTs[b], "wall": np.ascontiguousarray(wall), "w1": w1, "peT": peT, "w2k": w2k,
                        "w2v": wv2, "ov": ov, "dslc": dslc, "dcmp": dcmp, "nslope": nsl, "relslc": relslc,
                        "relcmp": relcmp})
    res = run_bass_kernel_spmd(nc, in_maps, core_ids=list(range(NCORES)))
    o = np.empty((BATCH, SEQ, 1024), np.float32)
    for core in range(NCORES):
        b, g = core // 4, core % 4
        o[b, :, g * 256:(g + 1) * 256] = res.results[core]["o"]
    return o


def _gla_consts():
    t = np.arange(128)
    same = (t[:, None] // 64) == (t[None, :] // 64)
    lblk = (same & (t[:, None] <= t[None, :])).astype(np.float32)
    ublk = (same & (t[:, None] > t[None, :])).astype(np.float32)
    return lblk, ublk


def run_gla_phase(x, w_in, w_gate2, b_gate2, head_norm_g):
    nc = _get_prog("gla", build_gla_phase)
    lblk, ublk = _gla_consts()
    xTs = [np.ascontiguousarray(x[b].T) for b in range(BATCH)]
    in_maps = []
    for core in range(NCORES):
        b, h = core // 4, core % 4
        wall = np.concatenate([
            w_in[:, h * 128:(h + 1) * 128], w_in[:, 512 + h * 128:512 + (h + 1) * 128],
            w_in[:, 1024 + h * 256:1024 + (h + 1) * 256], w_in[:, 2048 + h * 256:2048 + (h + 1) * 256],
            w_in[:, 3072:3088]], axis=1)
        in_maps.append({"xT": xTs[b], "wall": np.ascontiguousarray(wall),
                        "wg2": np.ascontiguousarray(w_gate2[:, h * 128:(h + 1) * 128]),
                        "bg2": np.ascontiguousarray(b_gate2[None, h * 128:(h + 1) * 128]),
                        "hng": np.ascontiguousarray(head_norm_g[h * 256:(h + 1) * 256]),
                        "lblk": lblk, "ublk": ublk})
    res = run_bass_kernel_spmd(nc, in_maps, core_ids=list(range(NCORES)))
    o = np.empty((BATCH, SEQ, 1024), np.float32)
    for core in range(NCORES):
        b, h = core // 4, core % 4
        o[b, :, h * 256:(h + 1) * 256] = res.results[core]["o"]
    return o


def kernel(**inputs):
    g = lambda k: np.ascontiguousarray(np.asarray(inputs[k], dtype=np.float32))
    x = g("x")
    NTOK = BATCH * SEQ
    nc = _get_prog("fused", build_fused)
    xf = x.reshape(NTOK, D_MODEL)
    xTs = [np.ascontiguousarray(x[b].T) for b in range(BATCH)]
    w_in0 = g("l0_w_in")
    dslc, dcmp, relslc, relcmp, ov = _nsa_consts()
    slopes = (2.0 ** (-8.0 * (np.arange(16, dtype=np.float32) + 1.0) / 16)).astype(np.float32)
    peT = np.ascontiguousarray(np.concatenate([g("l0_cmp_pe_k").T, g("l0_cmp_pe_v").T], axis=0))
    w1 = np.ascontiguousarray(np.stack([g("l0_cmp_wk1"), g("l0_cmp_wv1")]))
    wk2 = g("l0_cmp_wk2")
    w2k = np.ascontiguousarray(np.concatenate([wk2, wk2], axis=1))
    w2v = g("l0_cmp_wv2")
    w_in1 = g("l1_w_in")
    wg2 = g("l1_w_gate2"); bg2 = g("l1_b_gate2"); hng = g("l1_head_norm_g")
    lblk, ublk = _gla_consts()
    shared = {
        "a_w1": w1, "a_peT": peT, "a_w2k": w2k, "a_w2v": w2v, "a_ov": ov, "a_dslc": dslc, "a_dcmp": dcmp,
        "a_relslc": relslc, "a_relcmp": relcmp,
        "b_w_o": g("l0_w_o"), "b_ln1_g": g("l0_ln1_g"), "b_ln1_b": g("l0_ln1_b"), "b_ln2_g": g("l0_ln2_g"),
        "b_ln2_b": g("l0_ln2_b"), "b_w_gu": g("l0_ffn_w_gu")[None], "b_w_dn": g("l0_ffn_w_down")[None],
        "c_lblk": lblk, "c_ublk": ublk,
        "d_w_o": g("l1_w_o"), "d_ln1_g": g("l1_ln1_g"), "d_ln1_b": g("l1_ln1_b"), "d_ln2_g": g("l1_ln2_g"),
        "d_ln2_b": g("l1_ln2_b"), "d_w_gu": g("l1_moe_w_gu"), "d_w_dn": g("l1_moe_w_down"), "d_w_r": g("l1_router"),
    }
    in_maps = []
    for core in range(NCORES):
        b, r = core // 4, core % 4
        kvcol = lambda i: w_in0[:, 1024 + i * 256 + r * 64:1024 + i * 256 + (r + 1) * 64]
        wall_a = np.concatenate([
            w_in0[:, r * 256:(r + 1) * 256], kvcol(0), kvcol(1), kvcol(2), kvcol(2), kvcol(4), kvcol(4),
            kvcol(3), kvcol(5), w_in0[:, 2560 + r * 12:2560 + (r + 1) * 12]], axis=1)
        nsl = np.ascontiguousarray(np.broadcast_to(-slopes[r * 4:(r + 1) * 4][None, :], (128, 4)).astype(np.float32))
        wall_c = np.concatenate([
            w_in1[:, r * 128:(r + 1) * 128], w_in1[:, 512 + r * 128:512 + (r + 1) * 128],
            w_in1[:, 1024 + r * 256:1024 + (r + 1) * 256], w_in1[:, 2048 + r * 256:2048 + (r + 1) * 256],
            w_in1[:, 3072:3088]], axis=1)
        sel4 = np.zeros((128, 4), np.float32)
        sel4[:, r] = 1.0
        m = dict(shared)
        m.update({
            "a_xT": xTs[b], "a_wall": np.ascontiguousarray(wall_a), "a_nslope": nsl,
            "b_sel4": sel4, "b_xres": np.ascontiguousarray(xf[core * NT:(core + 1) * NT]),
            "c_wall": np.ascontiguousarray(wall_c), "c_wg2": np.ascontiguousarray(wg2[:, r * 128:(r + 1) * 128]),
            "c_bg2": np.ascontiguousarray(bg2[None, r * 128:(r + 1) * 128]),
            "c_hng": np.ascontiguousarray(hng[r * 256:(r + 1) * 256]),
            "d_sel4": sel4,
        })
        in_maps.append(m)
    res = run_bass_kernel_spmd(nc, in_maps, core_ids=list(range(NCORES)))
    return np.concatenate([r_["y"] for r_ in res.results], axis=0).reshape(BATCH, SEQ, D_MODEL).astype(np.float32)
```

```python
import math
from contextlib import ExitStack

import numpy as np
import concourse.bass as bass
import concourse.mybir as mybir
from concourse.bass_utils import run_bass_kernel_spmd

F32 = mybir.dt.float32
BF16 = mybir.dt.bfloat16
AF = mybir.ActivationFunctionType
ALU = mybir.AluOpType
AX = mybir.AxisListType

D_MODEL = 1024
BATCH = 2
SEQ = 8192
DN_ALPHA = 4 ** 0.25
LN_EPS = 1e-5
RMS_EPS = 1e-6
NCORES = 8
NT = 2048


class Buf:
    __slots__ = ("name", "last_write", "readers")

    def __init__(self, name):
        self.name = name
        self.last_write = None
        self.readers = {}


class SemState:
    def __init__(self, nc):
        self.es = ExitStack()
        self.sem = {}
        self.cnt = {}
        self.dsem = {}
        self.dsem_rr = {}
        for e in KB.ENGS:
            self.sem[e] = self.es.enter_context(nc.semaphore("s_" + e))
            self.cnt[e] = 0
        self.sem["cc"] = self.es.enter_context(nc.semaphore("s_cc"))
        self.cnt["cc"] = 0
        for q, n in (("sp", 16), ("pool", 4), ("act", 2)):
            lst = []
            for i in range(n):
                key = "d_%s_%d" % (q, i)
                self.sem[key] = self.es.enter_context(nc.semaphore(key))
                self.cnt[key] = 0
                lst.append(key)
            self.dsem[q] = lst
            self.dsem_rr[q] = 0


class KB:
    ENGS = ("pe", "act", "dve", "pool", "sp")

    def __init__(self, nc, st=None, prev=(), pfx=""):
        self.nc = nc
        self.pfx = pfx
        self.es = ExitStack()
        self.own_st = st is None
        if st is None:
            st = SemState(nc)
        self.st = st
        self.sem = st.sem
        self.cnt = st.cnt
        self.dsem = st.dsem
        self.dsem_rr = st.dsem_rr
        self.start_val = dict(st.cnt)
        self.waited = {}
        self.prog = {}
        for e in self.ENGS:
            self.waited[e] = {}
            self.prog[e] = []
        for e in self.ENGS:
            waits = self._need(e, list(prev))
            if waits:
                self.prog[e].append((waits, None, None))
        self.n_ins = 0

    def final_tickets(self):
        return [(k, v) for k, v in self.cnt.items() if v > 0]

    def sb(self, name, shape, dt):
        n = 1
        for d in shape[1:]:
            n *= d
        self.sb_bytes = getattr(self, "sb_bytes", 0) + n * (2 if dt == BF16 else 4)
        assert self.sb_bytes <= 178000, ("SBUF budget exceeded", name, self.sb_bytes)
        return self.es.enter_context(self.nc.sbuf_tensor(self.pfx + name, list(shape), dt))

    def ps(self, name, shape, dt=F32):
        return self.es.enter_context(self.nc.psum_tensor(self.pfx + name, list(shape), dt))

    def _need(self, eng, tickets):
        waits = []
        w = self.waited[eng]
        for t in tickets:
            if t is None:
                continue
            key, val = t
            if w.get(key, 0) < val:
                w[key] = val
                waits.append((key, val))
        return waits

    def _deps(self, eng, reads, writes, extra):
        tickets = list(extra)
        for b in reads:
            t = b.last_write
            if t is not None and not (t[0] == eng and eng == "pe"):
                tickets.append(t)
        for b in writes:
            t = b.last_write
            if t is not None and not (t[0] == eng and eng == "pe"):
                tickets.append(t)
            for k, v in b.readers.items():
                if not (k == eng and eng == "pe"):
                    tickets.append((k, v))
        return tickets

    def op(self, eng, fn, reads=(), writes=(), deps=()):
        waits = self._need(eng, self._deps(eng, reads, writes, deps))
        self.cnt[eng] += 1
        t = (eng, self.cnt[eng])
        self.prog[eng].append((waits, fn, (eng, 1)))
        for b in reads:
            b.readers[t[0]] = max(b.readers.get(t[0], 0), t[1])
        for b in writes:
            b.last_write = t
            b.readers = {}
        self.n_ins += 1
        return t

    def dma(self, q, out, in_, reads=(), writes=(), deps=()):
        lst = self.dsem[q]
        key = lst[self.dsem_rr[q] % len(lst)]
        self.dsem_rr[q] += 1
        tickets = self._deps("dma", reads, writes, deps)
        if self.cnt[key] > 0:
            tickets.append((key, self.cnt[key]))
        waits = self._need(q, tickets)
        self.cnt[key] += 16
        t = (key, self.cnt[key])

        def fn(e, out=out, in_=in_):
            return e.dma_start(out=out, in_=in_)

        self.prog[q].append((waits, fn, (key, 16)))
        for b in reads:
            b.readers[t[0]] = max(b.readers.get(t[0], 0), t[1])
        for b in writes:
            b.last_write = t
            b.readers = {}
        self.n_ins += 1
        return t

    def barrier(self):
        tickets = [(e, self.cnt[e]) for e in self.ENGS if self.cnt[e] > 0]
        for q in self.dsem:
            for key in self.dsem[q]:
                if self.cnt[key] > 0:
                    tickets.append((key, self.cnt[key]))
        for e in self.ENGS:
            waits = self._need(e, tickets)
            self.prog[e].append((waits, None, None))

    def collective(self, src, dst, deps=()):
        waits = self._need("pool", list(deps))
        self.cnt["cc"] += 1
        t = ("cc", self.cnt["cc"])

        def fn(e, src=src, dst=dst):
            return e.collective_compute("AllGather", ALU.bypass, replica_groups=[[0, 1, 2, 3], [4, 5, 6, 7]],
                                        ins=[src], outs=[dst])

        self.prog["pool"].append((waits, fn, ("cc", 1)))
        return t

    def wait_all(self, eng, tickets):
        waits = self._need(eng, tickets)
        self.prog[eng].append((waits, None, None))

    def check(self):
        val = dict(self.start_val)
        pc = {e: 0 for e in self.ENGS}
        progress = True
        while progress:
            progress = False
            for e in self.ENGS:
                lst = self.prog[e]
                while pc[e] < len(lst):
                    waits, fn, inc = lst[pc[e]]
                    if any(val[k] < v for k, v in waits):
                        break
                    if inc is not None:
                        val[inc[0]] += inc[1]
                    pc[e] += 1
                    progress = True
        for e in self.ENGS:
            if pc[e] < len(self.prog[e]):
                waits = self.prog[e][pc[e]][0]
                raise RuntimeError("deadlock: engine %s stuck at %d/%d waiting %s (vals %s)" % (
                    e, pc[e], len(self.prog[e]), waits, {k: val[k] for k, _ in waits}))

    def emit(self):
        self.check()
        nc = self.nc
        prog = self.prog
        sem = self.sem

        def run(e, lst):
            for waits, fn, inc in lst:
                for key, val in waits:
                    e.wait_ge(sem[key], val)
                if fn is not None:
                    ins = fn(e)
                    ins.then_inc(sem[inc[0]], inc[1])

        with nc.Block() as block:
            @block.tensor
            def _(e):
                run(e, prog["pe"])

            @block.scalar
            def _(e):
                run(e, prog["act"])

            @block.vector
            def _(e):
                run(e, prog["dve"])

            @block.gpsimd
            def _(e):
                self.freg = {v: e.to_reg(v) for v in (0.0, -30000.0, -1e9)}
                run(e, prog["pool"])

            @block.sync
            def _(e):
                run(e, prog["sp"])
        self.es.close()
        if self.own_st:
            self.st.es.close()


def bcast_rows(ap1d, nparts=128):
    n = ap1d.shape[0]
    return bass.AP(ap1d.tensor, ap1d.offset, [[0, nparts], [1, n]])


def make_ident(kb, name="ident"):
    ident = kb.sb(name, [128, 128], F32)
    b = Buf(name)
    kb.op("pool", lambda e: e.memset(ident[:], 1.0), writes=[b])
    kb.op("pool", lambda e: e.affine_select(out=ident[:], in_=ident[:], pattern=[[-1, 128]],
                                            compare_op=ALU.is_equal, fill=0.0, base=0,
                                            channel_multiplier=1), reads=[b], writes=[b])
    return ident, b


import os as _os
DBG_NOROUTER = 'norouter' in _os.environ.get('MOE_DBG', '')
DBG_H = 'bigh' in _os.environ.get('MOE_DBG', '')
DBG = _os.environ.get('MOE_DBG', '')


class Stager:
    def __init__(self, kb, n, elems, name="stg", aps=None):
        self.kb = kb
        if aps is None:
            aps = [kb.sb("%s%d" % (name, i), [128, elems], F32)[:] for i in range(n)]
        self.t = aps
        self.b = [Buf("%s%d" % (name, i)) for i in range(n)]
        self.i = 0

    def acquire(self):
        i = self.i % len(self.t)
        self.i += 1
        return i, self.t[i], self.b[i]

    def load(self, dst_ap, dst_bufs, src_ap, a, b, eng="pool", dst_reads=()):
        kb = self.kb
        i = self.i % len(self.t)
        self.i += 1
        view = self.t[i][:, 0:a * b].rearrange("p (a b) -> p a b", a=a)
        kb.dma("sp", view, src_ap, writes=[self.b[i]])
        if eng == "act":
            return kb.op("act", lambda e: e.copy(dst_ap, view), reads=[self.b[i]], writes=list(dst_bufs))
        return kb.op(eng, lambda e: e.tensor_copy(dst_ap, view), reads=[self.b[i]], writes=list(dst_bufs))


def layer_norm_tile(kb, h, hb, out, outb, gt, bt, pb, tmp):
    st, stb, mv, mvb, rs, rsb = tmp
    kb.op("dve", lambda e: e.bn_stats(st[:, 0:6], h[:, 0:512]), reads=[hb], writes=[stb])
    kb.op("dve", lambda e: e.bn_stats(st[:, 6:12], h[:, 512:1024]), reads=[hb, stb], writes=[stb])
    kb.op("dve", lambda e: e.bn_aggr(mv[:], st[:]), reads=[stb], writes=[mvb])
    kb.op("act", lambda e: e.activation(out=rs[:], in_=mv[:, 1:2], func=AF.Sqrt, bias=kb.eps_ln[:], scale=1.0),
          reads=[mvb], writes=[rsb])
    kb.op("dve", lambda e: e.reciprocal(rs[:], rs[:]), reads=[rsb], writes=[rsb])
    kb.op("dve", lambda e: e.tensor_scalar(out, h, mv[:, 0:1], rs[:, 0:1], ALU.subtract, ALU.mult),
          reads=[hb, mvb, rsb], writes=[outb])
    kb.op("pool", lambda e: e.tensor_tensor(out, out, gt[:], ALU.mult), reads=[outb, pb], writes=[outb])
    kb.op("pool", lambda e: e.tensor_tensor(out, out, bt[:], ALU.add), reads=[outb, pb], writes=[outb])


def build_ffn_phase(nc, moe, E=None, st=None, prev=(), pfx="", o_cand_fn=None, xres_fn=None, y_fn=None, gather_fn=None):
    if E is None:
        E = 8 if moe else 1
    H = 3584 if (moe or DBG_H) else 2816
    GB = 2
    NG = (H // 128) // GB
    HG = GB * 128
    NTT = NT // 128
    NCH = NT // 512

    def din(name, shape):
        return nc.dram_tensor(pfx + name, list(shape), F32, kind="ExternalInput").ap()

    fusedm = o_cand_fn is not None
    oT = din("oT", [D_MODEL, NT]) if not fusedm else None
    sel_in = din("sel4", [128, 4]) if fusedm else None
    xres = din("xres", [NT, D_MODEL]) if xres_fn is None else None
    w_o = din("w_o", [D_MODEL, D_MODEL])
    ln1_g = din("ln1_g", [D_MODEL]); ln1_b = din("ln1_b", [D_MODEL])
    ln2_g = din("ln2_g", [D_MODEL]); ln2_b = din("ln2_b", [D_MODEL])
    w_gu = din("w_gu", [E, D_MODEL, 2 * H])
    w_dn = din("w_dn", [E, H, D_MODEL])
    if moe:
        w_r = din("w_r", [D_MODEL, 8])
    y = nc.dram_tensor("y", [NT, D_MODEL], F32, kind="ExternalOutput").ap() if y_fn is None else None

    kb = KB(nc, st, prev, pfx)
    ident, identb = make_ident(kb)
    kb.eps_ln = kb.sb("eps_ln", [128, 1], F32)
    epsb = Buf("eps")
    kb.op("pool", lambda e: e.memset(kb.eps_ln[:], LN_EPS), writes=[epsb])

    acc = kb.sb("acc", [128, NTT, D_MODEL], F32)
    accb = [Buf("acc%d" % i) for i in range(NTT)]
    h1T = kb.sb("h1T", [128, 8, NT], BF16)
    h1Tb = [Buf("h1T%d" % i) for i in range(NTT)]
    stg = Stager(kb, 2, 2048)
    SLOT = 3 * 2048
    wbuf = kb.sb("wbuf", [128, 2 * SLOT], BF16)
    gt = kb.sb("gt", [128, D_MODEL], F32); bt = kb.sb("bt", [128, D_MODEL], F32)
    pb = Buf("lnparams")
    if fusedm:
        xr = [kb.sb("xr0", [128, D_MODEL], F32)] * 2
        xrb = [Buf("xr0")] * 2
        cand = kb.sb("cand", [128, D_MODEL], F32); candb = Buf("cand")
        osel = kb.sb("osel", [128, D_MODEL], F32); oselb = Buf("osel")
        oT_t = kb.sb("oT_t", [128, 8, 128], BF16); oT_tb = Buf("oT_t")
        sel = kb.sb("sel", [128, 4], F32); selb = Buf("sel")
    else:
        xr = [kb.sb("xr%d" % i, [128, D_MODEL], F32) for i in range(2)]
        xrb = [Buf("xr%d" % i) for i in range(2)]
    h1 = [kb.sb("h1_0", [128, D_MODEL], F32)] * 2
    h1b = [Buf("h1_0")] * 2
    st = kb.sb("st", [128, 12], F32); stb = Buf("st")
    mv = kb.sb("mv", [128, 2], F32); mvb = Buf("mv")
    rs = kb.sb("rs", [128, 1], F32); rsb = Buf("rs")
    lntmp = (st, stb, mv, mvb, rs, rsb)
    sil = [kb.sb("sil%d" % i, [128, 512], BF16) for i in range(2)]
    silb = [Buf("sil%d" % i) for i in range(2)]
    aT = [kb.sb("aT%d" % i, [128, GB, 512], BF16) for i in range(2)]
    aTb = [[Buf("aT%d_%d" % (i, j)) for j in range(GB)] for i in range(2)]
    if moe:
        gate = kb.sb("gate", [128, NTT, 8], F32)
        gateb = [Buf("gate%d" % i) for i in range(NTT)]
        wr_sb = kb.sb("wr_sb", [128, 8, 8], F32); wrb = Buf("wr")
        h1Tf = [kb.sb("h1Tf0", [128, 8, 128], F32)] * 2
        h1Tfb = [Buf("h1Tf0")] * 2
        lg = kb.sb("lg", [128, 8], F32); lgb = Buf("lg")
        mx8 = kb.sb("mx8", [128, 8], F32); mx8b = Buf("mx8")
        rt = kb.sb("rt", [128, 4], F32); rtb = Buf("rt")
        ex8 = kb.sb("ex8", [128, 8], F32); ex8b = Buf("ex8")

    pg = [kb.ps("pg%d" % i, [128, 512]) for i in range(2)]; pgb = [Buf("pg%d" % i) for i in range(2)]
    pu = [kb.ps("pu%d" % i, [128, 512]) for i in range(2)]; pub = [Buf("pu%d" % i) for i in range(2)]
    pd = [kb.ps("pd%d" % i, [128, 512]) for i in range(2)]; pdb = [Buf("pd%d" % i) for i in range(2)]
    pt = [kb.ps("pt%d" % i, [128, 512]) for i in range(2)]; ptb = [Buf("pt%d" % i) for i in range(2)]

    kb.dma("sp", gt[:], bcast_rows(ln1_g), writes=[pb])
    kb.dma("sp", bt[:], bcast_rows(ln1_b), writes=[pb])
    if moe and 'nowr' not in DBG:
        kb.dma("sp", wr_sb[:], w_r.rearrange("(kt p) e -> p kt e", p=128), writes=[wrb])
    wo_sb = wbuf[:, 0:8192].rearrange("p (kt n) -> p kt n", kt=8)
    wob = Buf("wo")
    w_o_v = w_o.rearrange("(kt p) n -> p kt n", p=128)
    for i in range(4):
        stg.load(wo_sb[:, 2 * i:2 * i + 2, :], [wob], w_o_v[:, 2 * i:2 * i + 2, :], 2, 1024)
    oT_sb = wbuf[:, 8192:12288].rearrange("p (kt t) -> p kt t", kt=8)
    oTb = Buf("oTs")
    wslot_b = [Buf("wslot0"), Buf("wslot1")]

    if fusedm:
        kb.dma("sp", sel[:], sel_in, writes=[selb])
    else:
        oT_v = oT.rearrange("(kt p) t -> p kt t", p=128)
    xres_v = xres.rearrange("(tt p) d -> tt p d", p=128) if xres is not None else None
    y_v = y.rearrange("(tt p) d -> tt p d", p=128) if y is not None else None

    evac_i = 0
    for c in range(NCH):
        if not fusedm:
            for i in range(2):
                stg.load(oT_sb[:, 4 * i:4 * i + 4, :], [oTb], oT_v[:, 4 * i:4 * i + 4, c * 512:(c + 1) * 512], 4, 512)
        for tl in range(4):
            tt = c * 4 + tl
            r = tt % 2
            if fusedm:
                for rk in range(4):
                    kb.dma("sp", cand[:].rearrange("p (g f) -> p g f", g=4), o_cand_fn(rk, tt), writes=[candb])
                    if rk == 0:
                        kb.op("dve", lambda e: e.tensor_scalar(osel[:], cand[:], sel[:, 0:1], None, ALU.mult),
                              reads=[candb, selb], writes=[oselb])
                    else:
                        kb.op("dve", lambda e, rk=rk: e.scalar_tensor_tensor(
                            out=osel[:], in0=cand[:], scalar=sel[:, rk:rk + 1], in1=osel[:], op0=ALU.mult, op1=ALU.add),
                            reads=[candb, selb, oselb], writes=[oselb])
                for half in range(2):
                    for j in range(4):
                        kt = half * 4 + j
                        kb.op("pe", lambda e, half=half, kt=kt, j=j: e.transpose(
                            pt[half][:, j * 128:(j + 1) * 128], osel[:, kt * 128:(kt + 1) * 128], ident[:]),
                            reads=[oselb, identb], writes=[ptb[half]])
                    kb.op("act", lambda e, half=half: e.copy(
                        oT_t[:, half * 4:(half + 1) * 4, :], pt[half][:].rearrange("p (j t) -> p j t", j=4)),
                        reads=[ptb[half]], writes=[oT_tb])
            kb.dma("sp", xr[r][:], xres_v[tt] if xres_fn is None else xres_fn(tt), writes=[xrb[r]])
            for nh in range(2):
                pi = evac_i % 2
                evac_i += 1
                for kt in range(8):
                    if fusedm:
                        kb.op("pe", lambda e, pi=pi, kt=kt, nh=nh: e.matmul(
                            pd[pi][:], oT_t[:, kt, :],
                            wo_sb[:, kt, nh * 512:(nh + 1) * 512], start=(kt == 0), stop=(kt == 7)),
                            reads=[oT_tb, wob], writes=[pdb[pi]])
                    else:
                        kb.op("pe", lambda e, pi=pi, kt=kt, tl=tl, nh=nh: e.matmul(
                            pd[pi][:], oT_sb[:, kt, tl * 128:(tl + 1) * 128],
                            wo_sb[:, kt, nh * 512:(nh + 1) * 512], start=(kt == 0), stop=(kt == 7)),
                            reads=[oTb, wob], writes=[pdb[pi]])
                kb.op("dve", lambda e, pi=pi, r=r, nh=nh: e.scalar_tensor_tensor(
                    out=xr[r][:, nh * 512:(nh + 1) * 512], in0=xr[r][:, nh * 512:(nh + 1) * 512],
                    scalar=DN_ALPHA, in1=pd[pi][:], op0=ALU.mult, op1=ALU.add),
                    reads=[xrb[r], pdb[pi]], writes=[xrb[r]])
            layer_norm_tile(kb, xr[r][:], xrb[r], h1[r][:], h1b[r], gt, bt, pb, lntmp)
            kb.op("act", lambda e, tt=tt, r=r: e.mul(acc[:, tt, :], h1[r][:], DN_ALPHA),
                  reads=[h1b[r]], writes=[accb[tt]])
            for half in range(2):
                pi = half
                for j in range(4):
                    kt = half * 4 + j
                    kb.op("pe", lambda e, pi=pi, r=r, kt=kt, j=j: e.transpose(
                        pt[pi][:, j * 128:(j + 1) * 128], h1[r][:, kt * 128:(kt + 1) * 128], ident[:]),
                        reads=[h1b[r], identb], writes=[ptb[pi]])
                kb.op("act", lambda e, pi=pi, tt=tt, half=half: e.copy(
                    h1T[:, half * 4:(half + 1) * 4, tt * 128:(tt + 1) * 128],
                    pt[pi][:].rearrange("p (j t) -> p j t", j=4)),
                    reads=[ptb[pi]], writes=[h1Tb[tt]])
                if moe and 'noh1tf' not in DBG:
                    kb.op("act", lambda e, pi=pi, r=r, half=half: e.copy(
                        h1Tf[r][:, half * 4:(half + 1) * 4, :],
                        pt[pi][:].rearrange("p (j t) -> p j t", j=4)),
                        reads=[ptb[pi]], writes=[h1Tfb[r]])
            if moe and DBG_NOROUTER:
                kb.op("pool", lambda e, tt=tt: e.memset(gate[:, tt, :], 0.5), writes=[gateb[tt]])
            if moe and not DBG_NOROUTER:
                for kt in range(8):
                    kb.op("pe", lambda e, r=r, kt=kt: e.matmul(
                        pg[0][:, 0:8], h1Tf[r][:, kt, :], wr_sb[:, kt, :], start=(kt == 0), stop=(kt == 7)),
                        reads=[h1Tfb[r], wrb], writes=[pgb[0]])
                kb.op("dve", lambda e: e.tensor_copy(lg[:], pg[0][:, 0:8]), reads=[pgb[0]], writes=[lgb])
                kb.op("dve", lambda e: e.max(mx8[:], lg[:]), reads=[lgb], writes=[mx8b])
                kb.op("dve", lambda e: e.tensor_scalar(rt[:, 0:1], mx8[:, 0:1], -1.0, None, ALU.mult),
                      reads=[mx8b], writes=[rtb])
                kb.op("act", lambda e: e.activation(out=ex8[:], in_=lg[:], func=AF.Exp, bias=rt[:, 0:1], scale=1.0),
                      reads=[lgb, rtb], writes=[ex8b])
                kb.op("act", lambda e: e.activation(out=rt[:, 1:2], in_=mx8[:, 1:2], func=AF.Exp, bias=rt[:, 0:1], scale=1.0),
                      reads=[mx8b, rtb], writes=[rtb])
                kb.op("dve", lambda e: e.tensor_scalar(rt[:, 2:3], rt[:, 1:2], 1.0, None, ALU.add),
                      reads=[rtb], writes=[rtb])
                kb.op("dve", lambda e: e.reciprocal(rt[:, 2:3], rt[:, 2:3]), reads=[rtb], writes=[rtb])
                kb.op("dve", lambda e: e.tensor_scalar(ex8[:], ex8[:], rt[:, 2:3], None, ALU.mult),
                      reads=[ex8b, rtb], writes=[ex8b])
                kb.op("dve", lambda e, tt=tt: e.scalar_tensor_tensor(
                    out=gate[:, tt, :], in0=lg[:], scalar=mx8[:, 1:2], in1=ex8[:], op0=ALU.is_ge, op1=ALU.mult),
                    reads=[lgb, mx8b, ex8b], writes=[gateb[tt]])

    kb.dma("sp", gt[:], bcast_rows(ln2_g), writes=[pb])
    kb.dma("sp", bt[:], bcast_rows(ln2_b), writes=[pb])

    w_gu_v = w_gu.rearrange("e (kt p) n -> e p kt n", p=128)
    w_dn_v = w_dn.rearrange("e (hb p) n -> e p hb n", p=128)
    gi = 0
    gu_i = 0
    d_i = 0
    sil_i = 0
    for e_ in range(E):
        for g in range(NG):
            sl = gi % 2
            gi += 1
            base = sl * SLOT
            wg_sb = wbuf[:, base:base + 2048].rearrange("p (kt n) -> p kt n", kt=8)
            wu_sb = wbuf[:, base + 2048:base + 4096].rearrange("p (kt n) -> p kt n", kt=8)
            wd_sb = wbuf[:, base + 4096:base + 6144].rearrange("p (hb n) -> p hb n", hb=GB)
            h0 = g * HG
            wb = wslot_b[sl]
            extra = [wob, oTb] if gi <= 2 else []
            stg.load(wg_sb, [wb] + extra, w_gu_v[e_, :, :, h0:h0 + HG], 8, HG)
            stg.load(wu_sb, [wb], w_gu_v[e_, :, :, H + h0:H + h0 + HG], 8, HG)
            stg.load(wd_sb, [wb], w_dn_v[e_, :, g * GB:(g + 1) * GB, :], GB, 1024)
            for c in range(NCH):
                ab = c % 2
                for blk in range(GB):
                    pi = gu_i % 2
                    gu_i += 1
                    for kt in range(8):
                        kb.op("pe", lambda e, pi=pi, kt=kt, blk=blk, c=c, wg_sb=wg_sb: e.matmul(
                            pg[pi][:], wg_sb[:, kt, blk * 128:(blk + 1) * 128],
                            h1T[:, kt, c * 512:(c + 1) * 512], start=(kt == 0), stop=(kt == 7)),
                            reads=[wb] + h1Tb[c * 4:(c + 1) * 4], writes=[pgb[pi]])
                    for kt in range(8):
                        kb.op("pe", lambda e, pi=pi, kt=kt, blk=blk, c=c, wu_sb=wu_sb: e.matmul(
                            pu[pi][:], wu_sb[:, kt, blk * 128:(blk + 1) * 128],
                            h1T[:, kt, c * 512:(c + 1) * 512], start=(kt == 0), stop=(kt == 7)),
                            reads=[wb], writes=[pub[pi]])
                    si = sil_i % 2
                    sil_i += 1
                    kb.op("act", lambda e, pi=pi, si=si: e.activation(out=sil[si][:], in_=pg[pi][:], func=AF.Silu),
                          reads=[pgb[pi]], writes=[silb[si]])
                    kb.op("dve", lambda e, pi=pi, si=si, ab=ab, blk=blk: e.tensor_tensor(
                        aT[ab][:, blk, :], pu[pi][:], sil[si][:], ALU.mult),
                        reads=[pub[pi], silb[si]], writes=[aTb[ab][blk]])
                for tl in range(4):
                    tt = c * 4 + tl
                    for nh in range(2):
                        pi = d_i % 2
                        d_i += 1
                        for blk in range(GB):
                            kb.op("pe", lambda e, pi=pi, ab=ab, blk=blk, tl=tl, nh=nh, wd_sb=wd_sb: e.matmul(
                                pd[pi][:], aT[ab][:, blk, tl * 128:(tl + 1) * 128],
                                wd_sb[:, blk, nh * 512:(nh + 1) * 512], start=(blk == 0), stop=(blk == GB - 1)),
                                reads=[aTb[ab][blk], wb], writes=[pdb[pi]])
                        if moe and 'nostt' not in DBG:
                            kb.op("dve", lambda e, pi=pi, tt=tt, nh=nh, e_=e_: e.scalar_tensor_tensor(
                                out=acc[:, tt, nh * 512:(nh + 1) * 512], in0=pd[pi][:],
                                scalar=gate[:, tt, e_:e_ + 1], in1=acc[:, tt, nh * 512:(nh + 1) * 512],
                                op0=ALU.mult, op1=ALU.add),
                                reads=[pdb[pi], gateb[tt], accb[tt]], writes=[accb[tt]])
                        else:
                            kb.op("dve", lambda e, pi=pi, tt=tt, nh=nh: e.tensor_tensor(
                                acc[:, tt, nh * 512:(nh + 1) * 512], pd[pi][:],
                                acc[:, tt, nh * 512:(nh + 1) * 512], ALU.add),
                                reads=[pdb[pi], accb[tt]], writes=[accb[tt]])

    outs = []
    for tt in range(NTT):
        r = tt % 2
        layer_norm_tile(kb, acc[:, tt, :], accb[tt], xr[r][:], xrb[r], gt, bt, pb, lntmp)
        outs.append(kb.dma("sp", y_v[tt] if y_fn is None else y_fn(tt), xr[r][:], reads=[xrb[r]]))
        if gather_fn is not None and gather_fn(tt) is not None:
            gs_, gd_ = gather_fn(tt)
            outs.append(kb.collective(gs_, gd_, deps=outs[-2:]))
    kb.wait_all("sp", outs)
    tickets = kb.final_tickets()
    kb.emit()
    return tickets


def build_nsa_phase(nc, st=None, prev=(), pfx="", o_dst_fn=None, gather_fn=None):
    T = SEQ
    ONLY = _os.environ.get('NSA_DBG', '')
    NEG = -30000.0

    def din(name, shape):
        return nc.dram_tensor(pfx + name, list(shape), F32, kind="ExternalInput").ap()

    xT = din("xT", [1024, T])
    wall = din("wall", [1024, 780])
    w1 = din("w1", [2, 2048, 256])
    peT = din("peT", [128, 32])
    w2k = din("w2k", [256, 128])
    w2v = din("w2v", [256, 64])
    ov = din("ov", [512, 128])
    dslc = din("dslc", [128, 512])
    dcmp = din("dcmp", [128, 512])
    nslope = din("nslope", [128, 4])
    relslc = din("relslc", [128, 64])
    relcmp = din("relcmp", [128, 16])
    o_out = None if o_dst_fn is not None else nc.dram_tensor("o", [T, 256], F32, kind="ExternalOutput").ap()

    kb = KB(nc, st, prev, pfx)
    ident, identb = make_ident(kb)

    QT = [kb.sb("QT%d" % i, [128, T], BF16) for i in range(2)]
    KsT2 = kb.sb("KsT2", [128, T], BF16)
    KwT2 = kb.sb("KwT2", [128, T], BF16)
    Vs1 = kb.sb("Vs1", [128, 64, 65], BF16)
    Vw1 = kb.sb("Vw1", [128, 64, 65], BF16)
    gates = kb.sb("gates", [128, 64, 12], F32)
    KcT2 = kb.sb("KcT2", [128, 512], BF16)
    VcX = kb.sb("VcX", [128, 4, 193], BF16)
    nsl = kb.sb("nsl", [128, 4], F32)
    misc = kb.sb("misc", [128, 512], F32)
    cpe = kb.sb("cpe", [128, 4], F32)
    QTb = Buf("QT"); KsTb = Buf("KsT"); KwTb = Buf("KwT"); Vsb = Buf("Vs"); Vwb = Buf("Vw")
    gatesb = Buf("gates"); KcTb = Buf("KcT"); VcXb = Buf("VcX"); nslb = Buf("nsl"); miscb = Buf("misc")
    cpeb = Buf("cpe")

    OVLW = 20800
    ovl = kb.sb("ovl", [128, OVLW], F32)
    off = [0]

    def carve(nwords):
        a = off[0]
        off[0] += nwords
        assert off[0] <= OVLW, off[0]
        return ovl[:, a:a + nwords]

    bk = [kb.ps("bk%d" % i, [128, 512]) for i in range(8)]
    bkb = [Buf("bk%d" % i) for i in range(8)]

    w1_sb = carve(4096).bitcast(BF16).rearrange("p (l h) -> p l h", l=32)
    xbf = [carve(1024).bitcast(BF16).rearrange("p (kt t) -> p kt t", kt=8) for _ in range(2)]
    xbfb = [Buf("xbf0"), Buf("xbf1")]
    KVcT = carve(4096).bitcast(BF16)
    KVcTb = Buf("KVcT")
    wall_sb = carve(3120).bitcast(BF16).rearrange("p (kt n) -> p kt n", kt=8)
    wallb = Buf("wall")
    w1b = Buf("w1")
    stg_aps = [carve(2048) for _ in range(2)]
    stg = Stager(kb, 2, 2048, aps=stg_aps)
    u_t = [carve(512) for _ in range(4)]
    ub = [Buf("u%d" % i) for i in range(4)]
    hidT = carve(1024).bitcast(BF16).rearrange("p (s n) -> p s n", s=4)
    hidTb = Buf("hidT")
    peT_sb = carve(16).bitcast(BF16)
    w2k_sb = carve(128).bitcast(BF16).rearrange("p (hh n) -> p hh n", hh=2)
    w2v_sb = carve(64).bitcast(BF16).rearrange("p (hh n) -> p hh n", hh=2)
    smallb = Buf("small")

    kb.dma("sp", nsl[:], nslope, writes=[nslb])
    wall_v = wall.rearrange("(kt p) n -> p kt n", p=128)
    for i in range(4):
        stg.load(wall_sb[:, 2 * i:2 * i + 2, :], [wallb], wall_v[:, 2 * i:2 * i + 2, :], 2, 780)
    kb.op("pool", lambda e: e.memset(Vs1[:, :, 64:65], 1.0), writes=[Vsb])
    kb.op("pool", lambda e: e.memset(Vw1[:, :, 64:65], 1.0), writes=[Vwb])
    kb.op("pool", lambda e: e.memset(VcX[:, :, 64:65], 1.0), writes=[VcXb])
    kb.op("pool", lambda e: e.memset(KcT2[:], 0.0), writes=[KcTb])
    kb.op("pool", lambda e: e.memset(hidT, 0.0), writes=[hidTb])

    xT_v = xT.rearrange("(kt p) t -> p kt t", p=128)
    ev = 0
    for c in range(32):
        xs = c % 2
        tok = slice(c * 256, (c + 1) * 256)
        stg.load(xbf[xs], [xbfb[xs]], xT_v[:, :, tok], 8, 256)
        for oi, (col0, dst, dstb, scale) in enumerate((
                (0, QT[0], QTb, 0.125), (128, QT[1], QTb, 0.125), (256, KVcT, KVcTb, 1.0),
                (384, KsT2, KsTb, 1.0), (512, KwT2, KwTb, 1.0))):
            pi = ev % 2
            ev += 1
            for kt in range(8):
                kb.op("pe", lambda e, pi=pi, kt=kt, xs=xs, col0=col0: e.matmul(
                    bk[pi][:, 0:256], wall_sb[:, kt, col0:col0 + 128], xbf[xs][:, kt, :],
                    start=(kt == 0), stop=(kt == 7)), reads=[wallb, xbfb[xs]], writes=[bkb[pi]])
            if oi % 2 == 0:
                kb.op("act", lambda e, pi=pi, dst=dst, tok=tok, scale=scale: e.mul(dst[:, tok], bk[pi][:, 0:256], scale),
                      reads=[bkb[pi]], writes=[dstb])
            else:
                kb.op("dve", lambda e, pi=pi, dst=dst, tok=tok, scale=scale: e.tensor_scalar(
                    dst[:, tok], bk[pi][:, 0:256], scale, None, ALU.mult), reads=[bkb[pi]], writes=[dstb])
        for tl in range(2):
            tix = c * 2 + tl
            pi = 2 + tl
            for kt in range(8):
                kb.op("pe", lambda e, pi=pi, kt=kt, xs=xs, tl=tl: e.matmul(
                    bk[pi][:, 0:140], xbf[xs][:, kt, tl * 128:(tl + 1) * 128], wall_sb[:, kt, 640:780],
                    start=(kt == 0), stop=(kt == 7)), reads=[wallb, xbfb[xs]], writes=[bkb[pi]])
            kb.op("dve", lambda e, pi=pi, tix=tix: e.tensor_copy(Vs1[:, tix, 0:64], bk[pi][:, 0:64]),
                  reads=[bkb[pi]], writes=[Vsb])
            kb.op("dve", lambda e, pi=pi, tix=tix: e.tensor_copy(Vw1[:, tix, 0:64], bk[pi][:, 64:128]),
                  reads=[bkb[pi]], writes=[Vwb])
            kb.op("dve", lambda e, pi=pi, tix=tix: e.tensor_copy(gates[:, tix, :], bk[pi][:, 128:140]),
                  reads=[bkb[pi]], writes=[gatesb])
            kb.op("act", lambda e, tix=tix: e.activation(out=gates[:, tix, :], in_=gates[:, tix, :],
                                                         func=AF.Sigmoid), reads=[gatesb], writes=[gatesb])

    for i in range(4):
        si, sview, sbuf_ = stg.acquire()
        v3 = sview[:, 0:2048].rearrange("p (l h) -> p l h", l=8)
        for s in range(2):
            kb.dma("sp", v3[s * 64:(s + 1) * 64], w1[s].rearrange("(l d) h -> d l h", d=64)[:, 8 * i:8 * i + 8, :],
                   writes=[sbuf_])
        kb.op("pool", lambda e, i=i, v3=v3: e.tensor_copy(w1_sb[:, 8 * i:8 * i + 8, :], v3), reads=[sbuf_], writes=[w1b])
    kb.dma("sp", misc[:, 0:32], peT, writes=[miscb])
    kb.op("pool", lambda e: e.tensor_copy(peT_sb, misc[:, 0:32]), reads=[miscb], writes=[smallb])
    kb.dma("sp", misc[:, 0:256].rearrange("p (hh n) -> p hh n", hh=2), w2k.rearrange("(hh p) n -> p hh n", p=128),
           writes=[miscb])
    kb.op("pool", lambda e: e.tensor_copy(w2k_sb, misc[:, 0:256].rearrange("p (hh n) -> p hh n", hh=2)),
          reads=[miscb], writes=[smallb])
    kb.dma("sp", misc[:, 0:128].rearrange("p (hh n) -> p hh n", hh=2), w2v.rearrange("(hh p) n -> p hh n", p=128),
           writes=[miscb])
    kb.op("pool", lambda e: e.tensor_copy(w2v_sb, misc[:, 0:128].rearrange("p (hh n) -> p hh n", hh=2)),
          reads=[miscb], writes=[smallb])
    kb.dma("sp", misc[:, 0:512].rearrange("p (i j) -> p i j", i=4), ov.rearrange("(i p) j -> p i j", p=128),
           writes=[miscb])
    kb.op("pool", lambda e: e.tensor_copy(VcX[:, :, 65:193], misc[:, 0:512].rearrange("p (i j) -> p i j", i=4)),
          reads=[miscb], writes=[VcXb])

    for s in range(2):
        ps_ = slice(s * 64, (s + 1) * 64)
        for hh in range(2):
            idx = s * 2 + hh
            ph = 4 + hh
            for l in range(32):
                kb.op("pe", lambda e, ph=ph, l=l, hh=hh, ps_=ps_: e.matmul(
                    bk[ph][:, 0:511], w1_sb[ps_, l, hh * 128:(hh + 1) * 128], KVcT[ps_, l:l + 8161:16],
                    start=(l == 0), stop=(l == 31)), reads=[w1b, KVcTb], writes=[bkb[ph]])
            for l in range(32):
                kb.op("pe", lambda e, l=l, hh=hh, ps_=ps_: e.matmul(
                    bk[6][:, 0:1], w1_sb[ps_, l, hh * 128:(hh + 1) * 128], peT_sb[ps_, l:l + 1],
                    start=(l == 0), stop=(l == 31)), reads=[w1b, smallb], writes=[bkb[6]])
            kb.op("dve", lambda e, idx=idx: e.tensor_copy(cpe[:, idx:idx + 1], bk[6][:, 0:1]), reads=[bkb[6]], writes=[cpeb])
            u, u2, w_, sg = u_t
            kb.op("dve", lambda e, ph=ph, idx=idx, u=u: e.tensor_scalar(u[:, 0:511], bk[ph][:, 0:511], cpe[:, idx:idx + 1], None, ALU.add),
                  reads=[bkb[ph], cpeb], writes=[ub[0]])
            kb.op("dve", lambda e, u=u, u2=u2: e.tensor_tensor(u2[:, 0:511], u[:, 0:511], u[:, 0:511], ALU.mult),
                  reads=[ub[0]], writes=[ub[1]])
            kb.op("dve", lambda e, u2=u2: e.tensor_scalar(u2[:, 0:511], u2[:, 0:511], 0.044715, 1.0, ALU.mult, ALU.add),
                  reads=[ub[1]], writes=[ub[1]])
            kb.op("dve", lambda e, u=u, u2=u2, w_=w_: e.tensor_tensor(w_[:, 0:511], u2[:, 0:511], u[:, 0:511], ALU.mult),
                  reads=[ub[0], ub[1]], writes=[ub[2]])
            kb.op("act", lambda e, w_=w_, sg=sg: e.activation(out=sg[:, 0:511], in_=w_[:, 0:511], func=AF.Sigmoid,
                                                              scale=1.5957691216057308), reads=[ub[2]], writes=[ub[3]])
            kb.op("dve", lambda e, idx=idx, u=u, sg=sg: e.tensor_tensor(hidT[:, idx, 0:511], u[:, 0:511], sg[:, 0:511], ALU.mult),
                  reads=[ub[0], ub[3]], writes=[hidTb])
    for hh in range(2):
        kb.op("pe", lambda e, hh=hh: e.matmul(bk[7][:, 0:511], w2k_sb[:, hh, :], hidT[:, hh, 0:511],
                                              start=(hh == 0), stop=(hh == 1)), reads=[smallb, hidTb], writes=[bkb[7]])
    kb.op("act", lambda e: e.copy(KcT2[:, 0:511], bk[7][:, 0:511]), reads=[bkb[7]], writes=[KcTb])
    for i in range(4):
        for hh in range(2):
            kb.op("pe", lambda e, hh=hh, i=i: e.matmul(bk[6][:, i * 64:(i + 1) * 64], hidT[:, 2 + hh, i * 128:(i + 1) * 128],
                                                       w2v_sb[:, hh, :], start=(hh == 0), stop=(hh == 1)),
                  reads=[smallb, hidTb], writes=[bkb[6]])
    kb.op("dve", lambda e: e.tensor_copy(VcX[:, :, 0:64], bk[6][:, 0:256].rearrange("p (i d) -> p i d", i=4)),
          reads=[bkb[6]], writes=[VcXb])

    kb.barrier()
    off[0] = 0
    Ebig = carve(4096).bitcast(BF16)
    negselT = carve(4096).bitcast(BF16)
    Bs = [carve(512) for _ in range(4)]
    Bc = [carve(512) for _ in range(4)]
    cbs = carve(256).rearrange("p (h m) -> p h m", h=4)
    cbc = carve(64).rearrange("p (h m) -> p h m", h=4)
    tt_ = [carve(512) for _ in range(4)]
    ttb = [Buf("t%d" % i) for i in range(4)]
    scr = tt_[0]
    dtab = tt_[1]
    rtab = tt_[2]
    pT = [carve(256).bitcast(BF16) for _ in range(4)]
    pTb = [Buf("pT%d" % i) for i in range(4)]
    oacc = [carve(1024).rearrange("p (q f) -> p q f", q=4) for _ in range(2)]
    oaccb = [Buf("oacc0"), Buf("oacc1")]
    imp = carve(512).rearrange("p (q j) -> p q j", q=4)
    impb = Buf("imp")
    sc = carve(512).rearrange("p (q j) -> p q j", q=4)
    sc2 = carve(512).rearrange("p (q j) -> p q j", q=4)
    nsel = carve(512).rearrange("p (q j) -> p q j", q=4)
    scb = [Buf("sc%d" % i) for i in range(4)]
    sc2b = [Buf("sc2%d" % i) for i in range(4)]
    nselb = [Buf("nsel%d" % i) for i in range(4)]
    mx = carve(64).rearrange("p (q j) -> p q j", q=4)
    mxb = [Buf("mx%d" % i) for i in range(4)]
    rd = carve(8)
    rdb = Buf("rd")
    stage = carve(772).rearrange("p (q w) -> p q w", q=4)
    stageb = [Buf("stage%d" % i) for i in range(4)]
    constb = Buf("const")
    Eb = Buf("Ebig")
    nsTb = Buf("negselT")

    kb.dma("sp", dtab, dslc, writes=[ttb[1]])
    for h in range(4):
        kb.op("dve", lambda e, h=h: e.tensor_scalar(Bs[h], dtab, nsl[:, h:h + 1], None, ALU.mult),
              reads=[ttb[1], nslb], writes=[constb])
    kb.dma("sp", dtab, dcmp, writes=[ttb[1]])
    for h in range(4):
        kb.op("dve", lambda e, h=h: e.tensor_scalar(Bc[h], dtab, nsl[:, h:h + 1], None, ALU.mult),
              reads=[ttb[1], nslb], writes=[constb])
    kb.dma("sp", rtab[:, 0:64], relslc, writes=[ttb[2]])
    for h in range(4):
        kb.op("dve", lambda e, h=h: e.tensor_scalar(cbs[:, h, :], rtab[:, 0:64], nsl[:, h:h + 1], None, ALU.mult),
              reads=[ttb[2], nslb], writes=[constb])
    kb.dma("sp", rtab[:, 0:16], relcmp, writes=[ttb[2]])
    for h in range(4):
        kb.op("dve", lambda e, h=h: e.tensor_scalar(cbc[:, h, :], rtab[:, 0:16], nsl[:, h:h + 1], None, ALU.mult),
              reads=[ttb[2], nslb], writes=[constb])
    scrb = ttb[0]
    for i in range(16):
        k0 = i * 512
        kb.op("pool", lambda e: e.memset(scr, 1.0), writes=[scrb])
        kb.op("pool", lambda e, k0=k0: e.affine_select(out=scr, in_=scr, pattern=[[1, 512]], compare_op=ALU.is_ge,
                                                       fill=kb.freg[0.0], base=k0, channel_multiplier=-64),
              reads=[scrb], writes=[scrb])
        kb.op("pool", lambda e, k0=k0: e.affine_select(out=scr, in_=scr, pattern=[[-1, 512]], compare_op=ALU.is_ge,
                                                       fill=kb.freg[0.0], base=63 - k0, channel_multiplier=64),
              reads=[scrb], writes=[scrb])
        kb.op("pool", lambda e, k0=k0: e.tensor_copy(Ebig[:, k0:k0 + 512], scr), reads=[scrb], writes=[Eb])

    o_v = o_out.rearrange("(c q p) f -> c p q f", p=128, q=4) if o_out is not None else None
    cnt = {"s": 0, "t": 0, "p": 0}
    outs = []

    SBANK = (0, 1, 6, 7)
    pend = []
    DEPTH = 3

    def pop_one():
        back, post, _tag = pend.pop(0)
        back()
        if post is not None:
            post()

    def push(back, post=None, tag=""):
        pend.append((back, post, tag))
        while len(pend) > DEPTH:
            pop_one()

    def flush():
        while pend:
            pop_one()

    def unit(h, c, KT, ksl, cb_ap, Bt, mask, acc_bank_views, Vrhs, first, last, sel, qs_min=0):
        hp = slice(64 * (h % 2), 64 * (h % 2) + 64)
        q_ap = QT[h // 2][hp, c * 512:(c + 1) * 512]
        si = SBANK[cnt["s"] % 4]; cnt["s"] += 1
        ti = cnt["t"] % 4; cnt["t"] += 1
        kb.op("pe", lambda e: e.matmul(bk[si][:], KT[hp, ksl], q_ap, start=True, stop=(sel is None)),
              reads=[QTb, KsTb, KwTb, KcTb], writes=[bkb[si]])
        if sel is not None:
            kb.op("pe", lambda e: e.matmul(bk[si][:], Ebig[:, ksl], negselT[:, c * 512:(c + 1) * 512],
                                           start=False, stop=True), reads=[Eb, nsTb], writes=[bkb[si]])
        kb.op("dve", lambda e: e.scalar_tensor_tensor(out=tt_[ti], in0=bk[si][:], scalar=cb_ap, in1=Bt,
                                                      op0=ALU.add, op1=ALU.add),
              reads=[bkb[si], constb], writes=[ttb[ti]])
        if mask is not None:
            pat, base, cm = mask
            kb.op("pool", lambda e: e.affine_select(out=tt_[ti], in_=tt_[ti], pattern=pat, compare_op=ALU.is_ge,
                                                    fill=kb.freg[NEG], base=base, channel_multiplier=cm),
                  reads=[ttb[ti]], writes=[ttb[ti]])
        kb.op("act", lambda e: e.activation(out=pT[ti], in_=tt_[ti], func=AF.Exp), reads=[ttb[ti]], writes=[pTb[ti]])

        def back():
            for qs in range(qs_min, 4):
                view, vb = acc_bank_views[qs]
                kb.op("pe", lambda e, qs=qs, view=view: e.matmul(view, pT[ti][:, qs * 128:(qs + 1) * 128], Vrhs,
                                                                 start=first,
                                                                 stop=(last(qs) if callable(last) else last)),
                      reads=[pTb[ti], Vsb, Vwb, VcXb], writes=[vb])
        return back

    def evac(W):
        for qs in range(4):
            kb.op("dve", lambda e, qs=qs: e.tensor_copy(stage[:, qs, 0:W], bk[2 + qs][:, 0:W]),
                  reads=[bkb[2 + qs]], writes=[stageb[qs]])

    def post_cmp(h, c, oa, oab):
        evac(193)
        for qs in range(4):
            kb.op("dve", lambda e, qs=qs: e.tensor_scalar(rd[:, qs:qs + 1], stage[:, qs, 64:65], 1e-30, None, ALU.max),
                  reads=[stageb[qs]], writes=[rdb])
        kb.op("dve", lambda e: e.reciprocal(rd[:, 0:4], rd[:, 0:4]), reads=[rdb], writes=[rdb])
        for qs in range(4):
            if h == 0:
                kb.op("dve", lambda e, qs=qs: e.tensor_scalar(
                    imp[:, qs, :], stage[:, qs, 65:193], rd[:, qs:qs + 1], None, ALU.mult),
                    reads=[stageb[qs], rdb], writes=[impb])
            else:
                kb.op("dve", lambda e, qs=qs: e.scalar_tensor_tensor(
                    out=imp[:, qs, :], in0=stage[:, qs, 65:193], scalar=rd[:, qs:qs + 1], in1=imp[:, qs, :],
                    op0=ALU.mult, op1=ALU.add), reads=[stageb[qs], rdb, impb], writes=[impb])
        kb.op("dve", lambda e: e.tensor_tensor(
            rd[:, 4:8], rd[:, 0:4], gates[:, 4 * c:4 * c + 4, h * 3 + 0], ALU.mult),
            reads=[rdb, gatesb], writes=[rdb])
        for qs in range(4):
            if ONLY in ('slc', 'win'):
                kb.op("dve", lambda e, qs=qs: e.tensor_scalar(
                    oa[:, qs, h * 64:(h + 1) * 64], stage[:, qs, 0:64], 0.0, None, ALU.mult),
                    reads=[stageb[qs], rdb], writes=[oab])
            else:
                kb.op("dve", lambda e, qs=qs: e.tensor_scalar(
                    oa[:, qs, h * 64:(h + 1) * 64], stage[:, qs, 0:64], rd[:, 4 + qs:5 + qs], None, ALU.mult),
                    reads=[stageb[qs], rdb], writes=[oab])

    def post_sw(h, c, br, oa, oab):
        evac(65)
        for qs in range(4):
            kb.op("dve", lambda e, qs=qs: e.tensor_scalar(rd[:, qs:qs + 1], stage[:, qs, 64:65], 1e-30, None, ALU.max),
                  reads=[stageb[qs]], writes=[rdb])
        kb.op("dve", lambda e: e.reciprocal(rd[:, 0:4], rd[:, 0:4]), reads=[rdb], writes=[rdb])
        kb.op("dve", lambda e: e.tensor_tensor(
            rd[:, 0:4], rd[:, 0:4], gates[:, 4 * c:4 * c + 4, h * 3 + br], ALU.mult),
            reads=[rdb, gatesb], writes=[rdb])
        for qs in range(4):
            if ONLY and ONLY != ('win' if br == 2 else 'slc'):
                kb.op("dve", lambda e, qs=qs: e.tensor_copy(rd[:, 4 + qs:5 + qs], stage[:, qs, 64:65]),
                      reads=[stageb[qs]], writes=[rdb])
                continue
            kb.op("dve", lambda e, qs=qs: e.scalar_tensor_tensor(
                out=oa[:, qs, h * 64:(h + 1) * 64], in0=stage[:, qs, 0:64], scalar=rd[:, qs:qs + 1],
                in1=oa[:, qs, h * 64:(h + 1) * 64], op0=ALU.mult, op1=ALU.add),
                reads=[stageb[qs], rdb, oab], writes=[oab])

    def selection(c):
        for qs in range(4):
            t0 = 512 * c + 128 * qs
            kb.op("pool", lambda e, qs=qs, t0=t0: e.affine_select(
                out=sc[:, qs, :], in_=imp[:, qs, :], pattern=[[-64, 128]], compare_op=ALU.is_ge, fill=kb.freg[-1e9],
                base=t0 - 128, channel_multiplier=1), reads=[impb], writes=[scb[qs]])
            kb.op("pool", lambda e, qs=qs: e.memset(sc[:, qs, 0:1], -1e9), writes=[scb[qs]])
            kb.op("dve", lambda e, qs=qs: e.max(mx[:, qs, 0:8], sc[:, qs, :]), reads=[scb[qs]], writes=[mxb[qs]])
            kb.op("dve", lambda e, qs=qs: e.match_replace(sc2[:, qs, :], mx[:, qs, 0:8], sc[:, qs, :], -2e9),
                  reads=[scb[qs], mxb[qs]], writes=[sc2b[qs]])
            kb.op("dve", lambda e, qs=qs: e.max(mx[:, qs, 8:16], sc2[:, qs, :]), reads=[sc2b[qs]], writes=[mxb[qs]])
            kb.op("dve", lambda e, qs=qs: e.tensor_scalar(nsel[:, qs, :], sc[:, qs, :], mx[:, qs, 12:13], NEG,
                                                          ALU.is_lt, ALU.mult),
                  reads=[scb[qs], mxb[qs]], writes=[nselb[qs]])
            kb.op("pool", lambda e, qs=qs, t0=t0: e.affine_select(
                out=nsel[:, qs, :], in_=nsel[:, qs, :], pattern=[[-64, 128]], compare_op=ALU.is_ge, fill=kb.freg[0.0],
                base=t0 - 128, channel_multiplier=1), reads=[nselb[qs]], writes=[nselb[qs]])
            kb.op("pool", lambda e, qs=qs: e.memset(nsel[:, qs, 0:1], 0.0), writes=[nselb[qs]])
            kb.op("pe", lambda e, qs=qs: e.transpose(bk[6][:, qs * 128:(qs + 1) * 128], nsel[:, qs, :], ident[:]),
                  reads=[nselb[qs], identb], writes=[bkb[6]])
        kb.op("act", lambda e: e.copy(negselT[:, c * 512:(c + 1) * 512], bk[6][:]), reads=[bkb[6]], writes=[nsTb])

    for c in range(16):
        oa = oacc[c % 2]
        oab = oaccb[c % 2]
        for h in range(4):
            views = [(bk[2 + qs][:, 0:193], bkb[2 + qs]) for qs in range(4)]
            ni = c // 4 + 1
            for i in range(ni):
                mask = None
                if i >= c // 4 - 1:
                    mask = ([[1, 512]], 512 * c - 2048 * i - 31, -16)
                bk_ = unit(h, c, KcT2, slice(i * 128, (i + 1) * 128), cbc[:, h, c - 4 * i:c - 4 * i + 1], Bc[h], mask,
                           views, VcX[:, i, :], i == 0, i == ni - 1, None)
                push(bk_, (lambda h=h, c=c, oa=oa, oab=oab: post_cmp(h, c, oa, oab)) if i == ni - 1 else None, "cmp")
        first_win = True
        for br, KT, V1 in ((2, KwT2, Vw1), (1, KsT2, Vs1)):
            if br == 1:
                pass
            for h in range(4):
                views = [(bk[2 + qs][:, 0:65], bkb[2 + qs]) for qs in range(4)]
                j0 = max(0, 4 * c - 4) if br == 2 else 0
                j1 = 4 * c + 3
                for j in range(j0, j1 + 1):
                    rel = 512 * c - 128 * j
                    qs_min = 0
                    if j >= 4 * c:
                        mask = ([[1, 512]], rel, -1)
                        qs_min = j - 4 * c
                    elif br == 2:
                        mask = ([[-1, 512]], 511 - rel, 1)
                    else:
                        mask = None
                    m = 4 * c - j + 3
                    if first_win and not any(tag == "cmp" for _, _, tag in pend):
                        selection(c)
                        first_win = False
                    bk_ = unit(h, c, KT, slice(j * 128, (j + 1) * 128), cbs[:, h, m:m + 1], Bs[h], mask, views,
                               V1[:, j, :], j == j0, (lambda qs, j=j, c=c: j == 4 * c + qs), True if br == 1 else None, qs_min)
                    push(bk_, (lambda h=h, c=c, br=br, oa=oa, oab=oab: post_sw(h, c, br, oa, oab)) if j == j1 else None)
        assert not first_win

        def store(c=c, oa=oa, oab=oab):
            outs.append(kb.dma("sp", o_v[c] if o_dst_fn is None else o_dst_fn(c), oa, reads=[oab]))
            if gather_fn is not None and gather_fn(c) is not None:
                gs_, gd_ = gather_fn(c)
                outs.append(kb.collective(gs_, gd_, deps=outs[-2:]))
        push(store, None)
    flush()
    kb.wait_all("sp", outs)
    tickets = kb.final_tickets()
    kb.emit()
    return tickets


def build_gla_phase(nc, st=None, prev=(), pfx="", x_src_fn=None, o_dst_fn=None, gather_fn=None):
    T = SEQ
    QSCALE = 128.0 ** -0.5

    def din(name, shape):
        return nc.dram_tensor(pfx + name, list(shape), F32, kind="ExternalInput").ap()

    xT = din("xT", [1024, T]) if x_src_fn is None else None
    wall = din("wall", [1024, 784])
    wg2 = din("wg2", [16, 128])
    bg2 = din("bg2", [1, 128])
    hng = din("hng", [256])
    lblk = din("lblk", [128, 128])
    ublk = din("ublk", [128, 128])
    o_out = None if o_dst_fn is not None else nc.dram_tensor("o", [T, 256], F32, kind="ExternalOutput").ap()

    kb = KB(nc, st, prev, pfx)
    if x_src_fn is not None:
        ident, identb = make_ident(kb)
        xtok = [kb.sb("xtok%d" % i, [128, D_MODEL], F32) for i in range(2)]
        xtokb = [Buf("xtok0"), Buf("xtok1")]
    one1 = kb.sb("one1", [128, 1], F32)
    epsr = kb.sb("epsr", [128, 1], F32)
    cb = Buf("consts")
    kb.op("pool", lambda e: e.memset(one1[:], 1.0), writes=[cb])
    kb.op("pool", lambda e: e.memset(epsr[:], RMS_EPS), writes=[cb])

    wall_sb = kb.sb("wall_sb", [128, 8, 784], BF16); wallb = Buf("wall")
    stg = Stager(kb, 2, 2048)
    xbf = [kb.sb("xbf%d" % i, [128, 8, 256], BF16) for i in range(2)]
    xbfb = [Buf("xbf0"), Buf("xbf1")]
    L01 = kb.sb("L01", [128, 128], F32)
    LS = kb.sb("LS", [128, 128], F32)
    US = kb.sb("US", [128, 128], F32)
    hn = kb.sb("hn", [128, 256], F32)
    wg2_sb = kb.sb("wg2_sb", [16, 128], BF16)
    bg2_sb = kb.sb("bg2_sb", [1, 128], BF16)
    ones_bf = kb.sb("ones_bf", [1, 128], BF16)
    misc = kb.sb("misc", [128, 128], F32); miscb = Buf("misc")

    qT_sb = kb.sb("qT_sb", [128, 256], F32); qTb = Buf("qT")
    kT_sb = kb.sb("kT_sb", [128, 256], F32); kTb = Buf("kT")
    alT_sb = kb.sb("alT_sb", [16, 256], BF16); alTb = Buf("alT")
    v_bf = kb.sb("v_bf", [128, 256], BF16); vb = Buf("v")
    k_tok = kb.sb("k_tok", [128, 128], F32); ktb = Buf("ktok")
    gs = kb.sb("gs", [128, 256], F32); gsb = Buf("gs")
    e1 = kb.sb("e1", [128, 128], F32); e1b = Buf("e1")
    la = kb.sb("la", [128, 128], F32); lab = Buf("la")
    bT_sb = kb.sb("bT_sb", [128, 2, 64], F32); bTb = Buf("bT")
    bd = kb.sb("bd", [128, 2, 64], F32); bdb = Buf("bd")
    eg = kb.sb("eg", [128, 128], F32); egb = Buf("eg")
    ieg = kb.sb("ieg", [128, 128], F32); iegb = Buf("ieg")
    eb = kb.sb("eb", [128, 128], F32); ebb = Buf("eb")
    erb = kb.sb("erb", [128, 128], F32); erbb = Buf("erb")
    qgT = kb.sb("qgT", [128, 128], BF16); qgb = Buf("qg")
    kgT = kb.sb("kgT", [128, 128], BF16); kgb = Buf("kg")
    qbP = [kb.sb("qbP%d" % i, [128, 128], BF16) for i in range(2)]; qbPb = [Buf("qbP0"), Buf("qbP1")]
    kbt = kb.sb("kbt", [128, 128], BF16); kbtb = Buf("kbt")
    AT = kb.sb("AT", [128, 128], BF16); ATb = Buf("AT")
    dec = kb.sb("dec", [128, 2], F32); decb = Buf("dec")
    S32 = kb.sb("S32", [128, 256], F32); S32b = Buf("S32")
    Sbf = [kb.sb("Sbf%d" % i, [128, 256], BF16) for i in range(3)]; Sbfb = [Buf("Sbf%d" % i) for i in range(3)]
    st = kb.sb("st", [128, 6], F32); stb = Buf("st")
    mv = kb.sb("mv", [128, 2], F32); mvb = Buf("mv")
    rs = kb.sb("rs", [128, 2], F32); rsb = Buf("rs")
    ot = [kb.sb("ot%d" % i, [128, 256], F32) for i in range(2)]; otb = [Buf("ot0"), Buf("ot1")]

    bk = [kb.ps("bk%d" % i, [128, 512]) for i in range(8)]
    bkb = [Buf("bk%d" % i) for i in range(8)]

    wall_v = wall.rearrange("(kt p) n -> p kt n", p=128)
    for i in range(4):
        stg.load(wall_sb[:, 2 * i:2 * i + 2, :], [wallb], wall_v[:, 2 * i:2 * i + 2, :], 2, 784)
    kb.dma("sp", L01[:], lblk, writes=[cb])
    kb.op("dve", lambda e: e.tensor_scalar(LS[:], L01[:], -1.0 / 16.0, None, ALU.mult), reads=[cb], writes=[cb])
    kb.dma("sp", misc[:], ublk, writes=[miscb])
    kb.op("dve", lambda e: e.tensor_scalar(US[:], misc[:], -1.0 / 16.0, None, ALU.mult), reads=[miscb], writes=[cb])
    kb.dma("sp", hn[:], bcast_rows(hng), writes=[cb])
    kb.dma("sp", misc[0:16, :], wg2, writes=[miscb])
    kb.op("dve", lambda e: e.tensor_copy(wg2_sb[:], misc[0:16, :]), reads=[miscb], writes=[cb])
    kb.dma("sp", misc[0:1, :], bg2, writes=[miscb])
    kb.op("dve", lambda e: e.tensor_copy(bg2_sb[:], misc[0:1, :]), reads=[miscb], writes=[cb])
    kb.op("pool", lambda e: e.memset(ones_bf[:], 1.0), writes=[cb])
    kb.op("pool", lambda e: e.memset(qbP[0][:], 0.0), writes=[qbPb[0]])
    kb.op("pool", lambda e: e.memset(qbP[1][:], 0.0), writes=[qbPb[1]])
    kb.op("pool", lambda e: e.memset(S32[:], 0.0), writes=[S32b])

    xT_v = xT.rearrange("(kt p) t -> p kt t", p=128) if x_src_fn is None else None
    o_v = o_out.rearrange("(m p) f -> m p f", p=128) if o_out is not None else None
    outs = []
    s_i = 0
    have_S = False
    for c in range(32):
        xs = c % 2
        if x_src_fn is None:
            stg.load(xbf[xs][:], [xbfb[xs]], xT_v[:, :, c * 256:(c + 1) * 256], 8, 256)
        else:
            for tl in range(2):
                xi = (c * 2 + tl) % 2
                kb.dma("sp", xtok[xi][:], x_src_fn(c * 2 + tl), writes=[xtokb[xi]])
                for half in range(2):
                    for j in range(4):
                        kt = half * 4 + j
                        kb.op("pe", lambda e, half=half, j=j, kt=kt, xi=xi: e.transpose(
                            bk[half][:, j * 128:(j + 1) * 128], xtok[xi][:, kt * 128:(kt + 1) * 128], ident[:]),
                            reads=[xtokb[xi], identb], writes=[bkb[half]])
                    kb.op("act", lambda e, half=half, xs=xs, tl=tl: e.copy(
                        xbf[xs][:, half * 4:(half + 1) * 4, tl * 128:(tl + 1) * 128],
                        bk[half][:].rearrange("p (j t) -> p j t", j=4)),
                        reads=[bkb[half]], writes=[xbfb[xs]])
        for kt in range(8):
            kb.op("pe", lambda e, kt=kt, xs=xs: e.matmul(bk[0][:, 0:256], wall_sb[:, kt, 0:128], xbf[xs][:, kt, :],
                                                         start=(kt == 0), stop=(kt == 7)),
                  reads=[wallb, xbfb[xs]], writes=[bkb[0]])
        kb.op("dve", lambda e: e.tensor_scalar(qT_sb[:], bk[0][:, 0:256], QSCALE, None, ALU.mult),
              reads=[bkb[0]], writes=[qTb])
        for kt in range(8):
            kb.op("pe", lambda e, kt=kt, xs=xs: e.matmul(bk[1][:, 0:256], wall_sb[:, kt, 128:256], xbf[xs][:, kt, :],
                                                         start=(kt == 0), stop=(kt == 7)),
                  reads=[wallb, xbfb[xs]], writes=[bkb[1]])
        kb.op("act", lambda e: e.copy(kT_sb[:], bk[1][:, 0:256]), reads=[bkb[1]], writes=[kTb])
        for kt in range(8):
            kb.op("pe", lambda e, kt=kt, xs=xs: e.matmul(bk[2][0:16, 0:256], wall_sb[:, kt, 768:784], xbf[xs][:, kt, :],
                                                         start=(kt == 0), stop=(kt == 7)),
                  reads=[wallb, xbfb[xs]], writes=[bkb[2]])
        kb.op("act", lambda e: e.copy(alT_sb[:], bk[2][0:16, 0:256]), reads=[bkb[2]], writes=[alTb])
        for tl in range(2):
            m = c * 2 + tl
            tk = slice(tl * 128, (tl + 1) * 128)
            for kt in range(8):
                kb.op("pe", lambda e, kt=kt, xs=xs, tk=tk: e.matmul(bk[3][:, 0:256], xbf[xs][:, kt, tk], wall_sb[:, kt, 256:512],
                                                                    start=(kt == 0), stop=(kt == 7)),
                      reads=[wallb, xbfb[xs]], writes=[bkb[3]])
            for kt in range(8):
                kb.op("pe", lambda e, kt=kt, xs=xs, tk=tk: e.matmul(bk[3][:, 256:384], xbf[xs][:, kt, tk], wall_sb[:, kt, 128:256],
                                                                    start=(kt == 0), stop=(kt == 7)),
                      reads=[wallb, xbfb[xs]], writes=[bkb[3]])
            kb.op("dve", lambda e: e.tensor_copy(v_bf[:], bk[3][:, 0:256]), reads=[bkb[3]], writes=[vb])
            kb.op("dve", lambda e: e.tensor_copy(k_tok[:], bk[3][:, 256:384]), reads=[bkb[3]], writes=[ktb])
            for kt in range(8):
                kb.op("pe", lambda e, kt=kt, xs=xs, tk=tk: e.matmul(bk[4][:, 0:256], xbf[xs][:, kt, tk], wall_sb[:, kt, 512:768],
                                                                    start=(kt == 0), stop=(kt == 7)),
                      reads=[wallb, xbfb[xs]], writes=[bkb[4]])
            kb.op("act", lambda e: e.activation(out=gs[:], in_=bk[4][:, 0:256], func=AF.Silu), reads=[bkb[4]], writes=[gsb])
            kb.op("pool", lambda e: e.tensor_tensor(gs[:], gs[:], hn[:], ALU.mult), reads=[gsb, cb], writes=[gsb])
            kb.op("pe", lambda e, tk=tk: e.matmul(bk[2][:, 256:384], alT_sb[:, tk], wg2_sb[:], start=True, stop=False),
                  reads=[alTb, cb], writes=[bkb[2]])
            kb.op("pe", lambda e: e.matmul(bk[2][:, 256:384], ones_bf[:], bg2_sb[:], start=False, stop=True),
                  reads=[cb], writes=[bkb[2]])
            kb.op("act", lambda e: e.activation(out=e1[:], in_=bk[2][:, 256:384], func=AF.Exp, scale=-1.0),
                  reads=[bkb[2]], writes=[e1b])
            kb.op("act", lambda e: e.activation(out=la[:], in_=e1[:], func=AF.Ln, bias=one1[:], scale=1.0),
                  reads=[e1b, cb], writes=[lab])
            kb.op("pe", lambda e: e.matmul(bk[5][:, 0:128], la[:], LS[:], start=True, stop=True),
                  reads=[lab, cb], writes=[bkb[5]])
            kb.op("pe", lambda e: e.matmul(bk[5][:, 128:256], US[:], la[:], start=True, stop=True),
                  reads=[lab, cb], writes=[bkb[5]])
            kb.op("act", lambda e: e.copy(bT_sb[:], bk[5][:, 0:128].rearrange("p (c t) -> p c t", c=2)),
                  reads=[bkb[5]], writes=[bTb])
            kb.op("act", lambda e: e.activation(out=erb[:], in_=bk[5][:, 128:256], func=AF.Exp), reads=[bkb[5]], writes=[erbb])
            for cc in range(2):
                kb.op("dve", lambda e, cc=cc: e.tensor_scalar(bd[:, cc, :], bT_sb[:, cc, :], bT_sb[:, cc, 32:33], None,
                                                              ALU.subtract), reads=[bTb], writes=[bdb])
            kb.op("act", lambda e: e.activation(out=eg[:], in_=bd[:].rearrange("p c t -> p (c t)"), func=AF.Exp),
                  reads=[bdb], writes=[egb])
            kb.op("act", lambda e: e.activation(out=eb[:], in_=bT_sb[:].rearrange("p c t -> p (c t)"), func=AF.Exp),
                  reads=[bTb], writes=[ebb])
            kb.op("act", lambda e: e.activation(out=dec[:], in_=bT_sb[:, :, 63], func=AF.Exp), reads=[bTb], writes=[decb])
            kb.op("dve", lambda e: e.reciprocal(ieg[:], eg[:]), reads=[egb], writes=[iegb])
            kb.op("dve", lambda e, tk=tk: e.tensor_tensor(qgT[:], qT_sb[:, tk], eg[:], ALU.mult), reads=[qTb, egb], writes=[qgb])
            kb.op("dve", lambda e, tk=tk: e.tensor_tensor(kgT[:], kT_sb[:, tk], ieg[:], ALU.mult), reads=[kTb, iegb], writes=[kgb])
            kb.op("dve", lambda e, tk=tk: e.tensor_tensor(qbP[0][:, 0:64], qT_sb[:, tk.start:tk.start + 64], eb[:, 0:64], ALU.mult),
                  reads=[qTb, ebb], writes=[qbPb[0]])
            kb.op("dve", lambda e, tk=tk: e.tensor_tensor(qbP[1][:, 64:128], qT_sb[:, tk.start + 64:tk.start + 128], eb[:, 64:128], ALU.mult),
                  reads=[qTb, ebb], writes=[qbPb[1]])
            kb.op("dve", lambda e: e.tensor_tensor(kbt[:], k_tok[:], erb[:], ALU.mult), reads=[ktb, erbb], writes=[kbtb])
            kb.op("pe", lambda e: e.matmul(bk[6][:, 0:128], kgT[:], qgT[:], start=True, stop=True),
                  reads=[kgb, qgb], writes=[bkb[6]])
            kb.op("dve", lambda e: e.tensor_tensor(AT[:], bk[6][:, 0:128], L01[:], ALU.mult), reads=[bkb[6], cb], writes=[ATb])
            s_prev = s_i
            terms = [(AT, ATb, v_bf, vb)]
            if have_S:
                terms.append((qbP[0], qbPb[0], Sbf[s_prev % 3], Sbfb[s_prev % 3]))
            for cc in range(2):
                pr = slice(cc * 64, (cc + 1) * 64)
                kb.op("pe", lambda e, pr=pr: e.matmul(bk[6][:, 128:384], kbt[pr, :], v_bf[pr, :], start=True, stop=True),
                      reads=[kbtb, vb], writes=[bkb[6]])
                kb.op("dve", lambda e, cc=cc: e.scalar_tensor_tensor(out=S32[:], in0=S32[:], scalar=dec[:, cc:cc + 1],
                                                                     in1=bk[6][:, 128:384], op0=ALU.mult, op1=ALU.add),
                      reads=[S32b, decb, bkb[6]], writes=[S32b])
                s_i += 1
                kb.op("act", lambda e, si=s_i: e.copy(Sbf[si % 3][:], S32[:]), reads=[S32b], writes=[Sbfb[s_i % 3]])
                if cc == 0:
                    terms.append((qbP[1], qbPb[1], Sbf[s_i % 3], Sbfb[s_i % 3]))
            have_S = True
            for ti, (l_, lb_, r_, rb_) in enumerate(terms):
                kb.op("pe", lambda e, l_=l_, r_=r_, ti=ti, n=len(terms): e.matmul(
                    bk[7][:, 0:256], l_[:], r_[:], start=(ti == 0), stop=(ti == n - 1)),
                    reads=[lb_, rb_], writes=[bkb[7]])
            kb.op("dve", lambda e: e.bn_stats(st[:], bk[7][:, 0:256]), reads=[bkb[7]], writes=[stb])
            kb.op("dve", lambda e: e.bn_aggr(mv[:], st[:]), reads=[stb], writes=[mvb])
            kb.op("dve", lambda e: e.tensor_tensor(rs[:, 0:1], mv[:, 0:1], mv[:, 0:1], ALU.mult), reads=[mvb], writes=[rsb])
            kb.op("dve", lambda e: e.tensor_tensor(rs[:, 0:1], rs[:, 0:1], mv[:, 1:2], ALU.add), reads=[mvb, rsb], writes=[rsb])
            kb.op("act", lambda e: e.activation(out=rs[:, 1:2], in_=rs[:, 0:1], func=AF.Sqrt, bias=epsr[:], scale=1.0),
                  reads=[rsb, cb], writes=[rsb])
            kb.op("dve", lambda e: e.reciprocal(rs[:, 1:2], rs[:, 1:2]), reads=[rsb], writes=[rsb])
            oi = m % 2
            kb.op("dve", lambda e, oi=oi: e.scalar_tensor_tensor(out=ot[oi][:], in0=bk[7][:, 0:256], scalar=rs[:, 1:2],
                                                                 in1=gs[:], op0=ALU.mult, op1=ALU.mult),
                  reads=[bkb[7], rsb, gsb], writes=[otb[oi]])
            outs.append(kb.dma("sp", o_v[m] if o_dst_fn is None else o_dst_fn(m), ot[oi][:], reads=[otb[oi]]))
            if gather_fn is not None and gather_fn(m) is not None:
                gs_, gd_ = gather_fn(m)
                outs.append(kb.collective(gs_, gd_, deps=outs[-8:]))
    kb.wait_all("sp", outs)
    tickets = kb.final_tickets()
    kb.emit()
    return tickets


def build_fused(nc):
    def internal(name, shape):
        return nc.dram_tensor(name, list(shape), F32, kind="Internal").ap()

    st = SemState(nc)
    oA = [internal("i_oA%d" % k, [1024, 256]) for k in range(8)]
    gA = [internal("i_gA%d" % k, [4 * 1024, 256]) for k in range(8)]
    x1 = [internal("i_x1%d" % k, [256, D_MODEL]) for k in range(8)]
    gX = [internal("i_gX%d" % k, [4 * 256, D_MODEL]) for k in range(8)]
    oC = [internal("i_oC%d" % k, [1024, 256]) for k in range(8)]
    gC = [internal("i_gC%d" % k, [4 * 1024, 256]) for k in range(8)]
    y = nc.dram_tensor("y", [NT, D_MODEL], F32, kind="ExternalOutput").ap()

    def mixer_cand(g_list):
        def fn(rk, tt):
            t0 = rk * NT + tt * 128
            k, i = t0 // 1024, t0 % 1024
            return g_list[k].rearrange("(g t) f -> t g f", g=4)[i:i + 128]
        return fn

    t = build_nsa_phase(
        nc, st, (), "a_",
        o_dst_fn=lambda c: oA[c // 2][(c % 2) * 512:(c % 2) * 512 + 512, :].rearrange("(q p) f -> p q f", p=128),
        gather_fn=lambda c: (oA[c // 2], gA[c // 2]) if c % 2 == 1 else None)
    t = build_ffn_phase(
        nc, False, 1, st, t, "b_", o_cand_fn=mixer_cand(gA),
        y_fn=lambda tt: x1[tt // 2][(tt % 2) * 128:(tt % 2) * 128 + 128, :],
        gather_fn=lambda tt: (x1[tt // 2], gX[tt // 2]) if tt % 2 == 1 else None)

    def x_src_fn(m):
        r, k, i = m // 16, (m % 16) // 2, (m % 2) * 128
        return gX[k][r * 256 + i:r * 256 + i + 128, :]

    t = build_gla_phase(
        nc, st, t, "c_", x_src_fn=x_src_fn,
        o_dst_fn=lambda m: oC[m // 8][(m % 8) * 128:(m % 8) * 128 + 128, :],
        gather_fn=lambda m: (oC[m // 8], gC[m // 8]) if m % 8 == 7 else None)
    t = build_ffn_phase(
        nc, True, 8, st, t, "d_", o_cand_fn=mixer_cand(gC),
        xres_fn=lambda tt: x1[tt // 2][(tt % 2) * 128:(tt % 2) * 128 + 128, :],
        y_fn=lambda tt: y[tt * 128:(tt + 1) * 128, :])
    st.es.close()
    return nc


_PROG_CACHE = {}


def _get_prog(key, builder):
    if key not in _PROG_CACHE:
        nc = bass.Bass("TRN2", target_bir_lowering=False)
        builder(nc)
        _PROG_CACHE[key] = nc
    return _PROG_CACHE[key]


def run_ffn_phase(moe, oT_list, xres_list, w_o, ln1_g, ln1_b, ln2_g, ln2_b, w_gu, w_dn, w_r=None):
    E = w_gu.shape[0]
    nc = _get_prog("ffn_%s_%d" % (moe, E), lambda nc: build_ffn_phase(nc, moe, E))
    in_maps = []
    for c in range(NCORES):
        m = {"oT": oT_list[c], "xres": xres_list[c], "w_o": w_o, "ln1_g": ln1_g, "ln1_b": ln1_b,
             "ln2_g": ln2_g, "ln2_b": ln2_b, "w_gu": w_gu, "w_dn": w_dn}
        if moe:
            m["w_r"] = w_r
        in_maps.append(m)
    res = run_bass_kernel_spmd(nc, in_maps, core_ids=list(range(NCORES)))
    return [r["y"] for r in res.results]


def _nsa_consts():
    kl = np.arange(128, dtype=np.float32)[:, None]
    ql = np.arange(512, dtype=np.float32)[None, :]
    dslc = np.ascontiguousarray(np.broadcast_to(ql - kl, (128, 512)).astype(np.float32))
    dcmp = np.ascontiguousarray(np.broadcast_to(ql - 16.0 * kl, (128, 512)).astype(np.float32))
    relslc = np.ascontiguousarray(np.broadcast_to((128.0 * np.arange(64) - 384.0)[None, :], (128, 64)).astype(np.float32))
    relcmp = np.ascontiguousarray(np.broadcast_to((512.0 * np.arange(16) - 31.0)[None, :], (128, 16)).astype(np.float32))
    n_cmp = (SEQ - 32) // 16 + 1
    cs = np.arange(n_cmp) * 16
    ce = cs + 31
    bs = np.arange(SEQ // 64) * 64
    be = bs + 63
    ov = np.zeros((512, 128), np.float32)
    ov[:n_cmp] = ((cs[:, None] <= be[None]) & (ce[:, None] >= bs[None])).astype(np.float32)
    return dslc, dcmp, relslc, relcmp, ov


def run_nsa_phase(x, w_in, pe_k, pe_v, wk1, wk2, wv1, wv2):
    nc = _get_prog("nsa", build_nsa_phase)
    dslc, dcmp, relslc, relcmp, ov = _nsa_consts()
    slopes = (2.0 ** (-8.0 * (np.arange(16, dtype=np.float32) + 1.0) / 16)).astype(np.float32)
    peT = np.ascontiguousarray(np.concatenate([pe_k.T, pe_v.T], axis=0))
    w1 = np.ascontiguousarray(np.stack([wk1, wv1]))
    w2k = np.ascontiguousarray(np.concatenate([wk2, wk2], axis=1))
    xTs = [np.ascontiguousarray(x[b].T) for b in range(BATCH)]
    in_maps = []
    for core in range(NCORES):
        b, g = core // 4, core % 4
        kvcol = lambda i: w_in[:, 1024 + i * 256 + g * 64:1024 + i * 256 + (g + 1) * 64]
        wall = np.concatenate([
            w_in[:, g * 256:(g + 1) * 256], kvcol(0), kvcol(1), kvcol(2), kvcol(2), kvcol(4), kvcol(4),
            kvcol(3), kvcol(5), w_in[:, 2560 + g * 12:2560 + (g + 1) * 12]], axis=1)
        nsl = np.ascontiguousarray(np.broadcast_to(-slopes[g * 4:(g + 1) * 4][None, :], (128, 4)).astype(np.float32))
        in_maps.append({"xT": xTs[b], "wall": np.ascontiguousarray(wall), "w1": w1, "peT": peT, "w2k": w2k,
                        "w2v": wv2, "ov": ov, "dslc": dslc, "dcmp": dcmp, "nslope": nsl, "relslc": relslc,
                        "relcmp": relcmp})
    res = run_bass_kernel_spmd(nc, in_maps, core_ids=list(range(NCORES)))
    o = np.empty((BATCH, SEQ, 1024), np.float32)
    for core in range(NCORES):
        b, g = core // 4, core % 4
        o[b, :, g * 256:(g + 1) * 256] = res.results[core]["o"]
    return o


def _gla_consts():
    t = np.arange(128)
    same = (t[:, None] // 64) == (t[None, :] // 64)
    lblk = (same & (t[:, None] <= t[None, :])).astype(np.float32)
    ublk = (same & (t[:, None] > t[None, :])).astype(np.float32)
    return lblk, ublk


def run_gla_phase(x, w_in, w_gate2, b_gate2, head_norm_g):
    nc = _get_prog("gla", build_gla_phase)
    lblk, ublk = _gla_consts()
    xTs = [np.ascontiguousarray(x[b].T) for b in range(BATCH)]
    in_maps = []
    for core in range(NCORES):
        b, h = core // 4, core % 4
        wall = np.concatenate([
            w_in[:, h * 128:(h + 1) * 128], w_in[:, 512 + h * 128:512 + (h + 1) * 128],
            w_in[:, 1024 + h * 256:1024 + (h + 1) * 256], w_in[:, 2048 + h * 256:2048 + (h + 1) * 256],
            w_in[:, 3072:3088]], axis=1)
        in_maps.append({"xT": xTs[b], "wall": np.ascontiguousarray(wall),
                        "wg2": np.ascontiguousarray(w_gate2[:, h * 128:(h + 1) * 128]),
                        "bg2": np.ascontiguousarray(b_gate2[None, h * 128:(h + 1) * 128]),
                        "hng": np.ascontiguousarray(head_norm_g[h * 256:(h + 1) * 256]),
                        "lblk": lblk, "ublk": ublk})
    res = run_bass_kernel_spmd(nc, in_maps, core_ids=list(range(NCORES)))
    o = np.empty((BATCH, SEQ, 1024), np.float32)
    for core in range(NCORES):
        b, h = core // 4, core % 4
        o[b, :, h * 256:(h + 1) * 256] = res.results[core]["o"]
    return o


def kernel(**inputs):
    g = lambda k: np.ascontiguousarray(np.asarray(inputs[k], dtype=np.float32))
    x = g("x")
    NTOK = BATCH * SEQ
    nc = _get_prog("fused", build_fused)
    xf = x.reshape(NTOK, D_MODEL)
    xTs = [np.ascontiguousarray(x[b].T) for b in range(BATCH)]
    w_in0 = g("l0_w_in")
    dslc, dcmp, relslc, relcmp, ov = _nsa_consts()
    slopes = (2.0 ** (-8.0 * (np.arange(16, dtype=np.float32) + 1.0) / 16)).astype(np.float32)
    peT = np.ascontiguousarray(np.concatenate([g("l0_cmp_pe_k").T, g("l0_cmp_pe_v").T], axis=0))
    w1 = np.ascontiguousarray(np.stack([g("l0_cmp_wk1"), g("l0_cmp_wv1")]))
    wk2 = g("l0_cmp_wk2")
    w2k = np.ascontiguousarray(np.concatenate([wk2, wk2], axis=1))
    w2v = g("l0_cmp_wv2")
    w_in1 = g("l1_w_in")
    wg2 = g("l1_w_gate2"); bg2 = g("l1_b_gate2"); hng = g("l1_head_norm_g")
    lblk, ublk = _gla_consts()
    shared = {
        "a_w1": w1, "a_peT": peT, "a_w2k": w2k, "a_w2v": w2v, "a_ov": ov, "a_dslc": dslc, "a_dcmp": dcmp,
        "a_relslc": relslc, "a_relcmp": relcmp,
        "b_w_o": g("l0_w_o"), "b_ln1_g": g("l0_ln1_g"), "b_ln1_b": g("l0_ln1_b"), "b_ln2_g": g("l0_ln2_g"),
        "b_ln2_b": g("l0_ln2_b"), "b_w_gu": g("l0_ffn_w_gu")[None], "b_w_dn": g("l0_ffn_w_down")[None],
        "c_lblk": lblk, "c_ublk": ublk,
        "d_w_o": g("l1_w_o"), "d_ln1_g": g("l1_ln1_g"), "d_ln1_b": g("l1_ln1_b"), "d_ln2_g": g("l1_ln2_g"),
        "d_ln2_b": g("l1_ln2_b"), "d_w_gu": g("l1_moe_w_gu"), "d_w_dn": g("l1_moe_w_down"), "d_w_r": g("l1_router"),
    }
    in_maps = []
    for core in range(NCORES):
        b, r = core // 4, core % 4
        kvcol = lambda i: w_in0[:, 1024 + i * 256 + r * 64:1024 + i * 256 + (r + 1) * 64]
        wall_a = np.concatenate([
            w_in0[:, r * 256:(r + 1) * 256], kvcol(0), kvcol(1), kvcol(2), kvcol(2), kvcol(4), kvcol(4),
            kvcol(3), kvcol(5), w_in0[:, 2560 + r * 12:2560 + (r + 1) * 12]], axis=1)
        nsl = np.ascontiguousarray(np.broadcast_to(-slopes[r * 4:(r + 1) * 4][None, :], (128, 4)).astype(np.float32))
        wall_c = np.concatenate([
            w_in1[:, r * 128:(r + 1) * 128], w_in1[:, 512 + r * 128:512 + (r + 1) * 128],
            w_in1[:, 1024 + r * 256:1024 + (r + 1) * 256], w_in1[:, 2048 + r * 256:2048 + (r + 1) * 256],
            w_in1[:, 3072:3088]], axis=1)
        sel4 = np.zeros((128, 4), np.float32)
        sel4[:, r] = 1.0
        m = dict(shared)
        m.update({
            "a_xT": xTs[b], "a_wall": np.ascontiguousarray(wall_a), "a_nslope": nsl,
            "b_sel4": sel4, "b_xres": np.ascontiguousarray(xf[core * NT:(core + 1) * NT]),
            "c_wall": np.ascontiguousarray(wall_c), "c_wg2": np.ascontiguousarray(wg2[:, r * 128:(r + 1) * 128]),
            "c_bg2": np.ascontiguousarray(bg2[None, r * 128:(r + 1) * 128]),
            "c_hng": np.ascontiguousarray(hng[r * 256:(r + 1) * 256]),
            "d_sel4": sel4,
        })
        in_maps.append(m)
    res = run_bass_kernel_spmd(nc, in_maps, core_ids=list(range(NCORES)))
    return np.concatenate([r_["y"] for r_ in res.results], axis=0).reshape(BATCH, SEQ, D_MODEL).astype(np.float32)
```

```python
import math
from contextlib import ExitStack

import numpy as np
import concourse.bass as bass
import concourse.mybir as mybir
from concourse.bass_utils import run_bass_kernel_spmd

F32 = mybir.dt.float32
BF16 = mybir.dt.bfloat16
AF = mybir.ActivationFunctionType
ALU = mybir.AluOpType
AX = mybir.AxisListType

D_MODEL = 1024
BATCH = 2
SEQ = 8192
DN_ALPHA = 4 ** 0.25
LN_EPS = 1e-5
RMS_EPS = 1e-6
NCORES = 8
NT = 2048


class Buf:
    __slots__ = ("name", "last_write", "readers")

    def __init__(self, name):
        self.name = name
        self.last_write = None
        self.readers = {}


class SemState:
    def __init__(self, nc):
        self.es = ExitStack()
        self.sem = {}
        self.cnt = {}
        self.dsem = {}
        self.dsem_rr = {}
        for e in KB.ENGS:
            self.sem[e] = self.es.enter_context(nc.semaphore("s_" + e))
            self.cnt[e] = 0
        self.sem["cc"] = self.es.enter_context(nc.semaphore("s_cc"))
        self.cnt["cc"] = 0
        for q, n in (("sp", 16), ("pool", 4), ("act", 2)):
            lst = []
            for i in range(n):
                key = "d_%s_%d" % (q, i)
                self.sem[key] = self.es.enter_context(nc.semaphore(key))
                self.cnt[key] = 0
                lst.append(key)
            self.dsem[q] = lst
            self.dsem_rr[q] = 0


class KB:
    ENGS = ("pe", "act", "dve", "pool", "sp")

    def __init__(self, nc, st=None, prev=(), pfx=""):
        self.nc = nc
        self.pfx = pfx
        self.es = ExitStack()
        self.own_st = st is None
        if st is None:
            st = SemState(nc)
        self.st = st
        self.sem = st.sem
        self.cnt = st.cnt
        self.dsem = st.dsem
        self.dsem_rr = st.dsem_rr
        self.start_val = dict(st.cnt)
        self.waited = {}
        self.prog = {}
        for e in self.ENGS:
            self.waited[e] = {}
            self.prog[e] = []
        for e in self.ENGS:
            waits = self._need(e, list(prev))
            if waits:
                self.prog[e].append((waits, None, None))
        self.n_ins = 0

    def final_tickets(self):
        return [(k, v) for k, v in self.cnt.items() if v > 0]

    def sb(self, name, shape, dt):
        n = 1
        for d in shape[1:]:
            n *= d
        self.sb_bytes = getattr(self, "sb_bytes", 0) + n * (2 if dt == BF16 else 4)
        assert self.sb_bytes <= 178000, ("SBUF budget exceeded", name, self.sb_bytes)
        return self.es.enter_context(self.nc.sbuf_tensor(self.pfx + name, list(shape), dt))

    def ps(self, name, shape, dt=F32):
        return self.es.enter_context(self.nc.psum_tensor(self.pfx + name, list(shape), dt))

    def _need(self, eng, tickets):
        waits = []
        w = self.waited[eng]
        for t in tickets:
            if t is None:
                continue
            key, val = t
            if w.get(key, 0) < val:
                w[key] = val
                waits.append((key, val))
        return waits

    def _deps(self, eng, reads, writes, extra):
        tickets = list(extra)
        for b in reads:
            t = b.last_write
            if t is not None and not (t[0] == eng and eng == "pe"):
                tickets.append(t)
        for b in writes:
            t = b.last_write
            if t is not None and not (t[0] == eng and eng == "pe"):
                tickets.append(t)
            for k, v in b.readers.items():
                if not (k == eng and eng == "pe"):
                    tickets.append((k, v))
        return tickets

    def op(self, eng, fn, reads=(), writes=(), deps=()):
        waits = self._need(eng, self._deps(eng, reads, writes, deps))
        self.cnt[eng] += 1
        t = (eng, self.cnt[eng])
        self.prog[eng].append((waits, fn, (eng, 1)))
        for b in reads:
            b.readers[t[0]] = max(b.readers.get(t[0], 0), t[1])
        for b in writes:
            b.last_write = t
            b.readers = {}
        self.n_ins += 1
        return t

    def dma(self, q, out, in_, reads=(), writes=(), deps=()):
        lst = self.dsem[q]
        key = lst[self.dsem_rr[q] % len(lst)]
        self.dsem_rr[q] += 1
        tickets = self._deps("dma", reads, writes, deps)
        if self.cnt[key] > 0:
            tickets.append((key, self.cnt[key]))
        waits = self._need(q, tickets)
        self.cnt[key] += 16
        t = (key, self.cnt[key])

        def fn(e, out=out, in_=in_):
            return e.dma_start(out=out, in_=in_)

        self.prog[q].append((waits, fn, (key, 16)))
        for b in reads:
            b.readers[t[0]] = max(b.readers.get(t[0], 0), t[1])
        for b in writes:
            b.last_write = t
            b.readers = {}
        self.n_ins += 1
        return t

    def barrier(self):
        tickets = [(e, self.cnt[e]) for e in self.ENGS if self.cnt[e] > 0]
        for q in self.dsem:
            for key in self.dsem[q]:
                if self.cnt[key] > 0:
                    tickets.append((key, self.cnt[key]))
        for e in self.ENGS:
            waits = self._need(e, tickets)
            self.prog[e].append((waits, None, None))

    def collective(self, src, dst, deps=()):
        waits = self._need("pool", list(deps))
        self.cnt["cc"] += 1
        t = ("cc", self.cnt["cc"])

        def fn(e, src=src, dst=dst):
            return e.collective_compute("AllGather", ALU.bypass, replica_groups=[[0, 1, 2, 3], [4, 5, 6, 7]],
                                        ins=[src], outs=[dst])

        self.prog["pool"].append((waits, fn, ("cc", 1)))
        return t

    def wait_all(self, eng, tickets):
        waits = self._need(eng, tickets)
        self.prog[eng].append((waits, None, None))

    def check(self):
        val = dict(self.start_val)
        pc = {e: 0 for e in self.ENGS}
        progress = True
        while progress:
            progress = False
            for e in self.ENGS:
                lst = self.prog[e]
                while pc[e] < len(lst):
                    waits, fn, inc = lst[pc[e]]
                    if any(val[k] < v for k, v in waits):
                        break
                    if inc is not None:
                        val[inc[0]] += inc[1]
                    pc[e] += 1
                    progress = True
        for e in self.ENGS:
            if pc[e] < len(self.prog[e]):
                waits = self.prog[e][pc[e]][0]
                raise RuntimeError("deadlock: engine %s stuck at %d/%d waiting %s (vals %s)" % (
                    e, pc[e], len(self.prog[e]), waits, {k: val[k] for k, _ in waits}))

    def emit(self):
        self.check()
        nc = self.nc
        prog = self.prog
        sem = self.sem

        def run(e, lst):
            for waits, fn, inc in lst:
                for key, val in waits:
                    e.wait_ge(sem[key], val)
                if fn is not None:
                    ins = fn(e)
                    ins.then_inc(sem[inc[0]], inc[1])

        with nc.Block() as block:
            @block.tensor
            def _(e):
                run(e, prog["pe"])

            @block.scalar
            def _(e):
                run(e, prog["act"])

            @block.vector
            def _(e):
                run(e, prog["dve"])

            @block.gpsimd
            def _(e):
                self.freg = {v: e.to_reg(v) for v in (0.0, -30000.0, -1e9)}
                run(e, prog["pool"])

            @block.sync
            def _(e):
                run(e, prog["sp"])
        self.es.close()
        if self.own_st:
            self.st.es.close()


def bcast_rows(ap1d, nparts=128):
    n = ap1d.shape[0]
    return bass.AP(ap1d.tensor, ap1d.offset, [[0, nparts], [1, n]])


def make_ident(kb, name="ident"):
    ident = kb.sb(name, [128, 128], F32)
    b = Buf(name)
    kb.op("pool", lambda e: e.memset(ident[:], 1.0), writes=[b])
    kb.op("pool", lambda e: e.affine_select(out=ident[:], in_=ident[:], pattern=[[-1, 128]],
                                            compare_op=ALU.is_equal, fill=0.0, base=0,
                                            channel_multiplier=1), reads=[b], writes=[b])
    return ident, b


import os as _os
DBG_NOROUTER = 'norouter' in _os.environ.get('MOE_DBG', '')
DBG_H = 'bigh' in _os.environ.get('MOE_DBG', '')
DBG = _os.environ.get('MOE_DBG', '')


class Stager:
    def __init__(self, kb, n, elems, name="stg", aps=None):
        self.kb = kb
        if aps is None:
            aps = [kb.sb("%s%d" % (name, i), [128, elems], F32)[:] for i in range(n)]
        self.t = aps
        self.b = [Buf("%s%d" % (name, i)) for i in range(n)]
        self.i = 0

    def acquire(self):
        i = self.i % len(self.t)
        self.i += 1
        return i, self.t[i], self.b[i]

    def load(self, dst_ap, dst_bufs, src_ap, a, b, eng="pool", dma_deps=()):
        kb = self.kb
        i = self.i % len(self.t)
        self.i += 1
        view = self.t[i][:, 0:a * b].rearrange("p (a b) -> p a b", a=a)
        kb.dma("sp", view, src_ap, writes=[self.b[i]], deps=list(dma_deps))
        if eng == "act":
            return kb.op("act", lambda e: e.copy(dst_ap, view), reads=[self.b[i]], writes=list(dst_bufs))
        return kb.op(eng, lambda e: e.tensor_copy(dst_ap, view), reads=[self.b[i]], writes=list(dst_bufs))


def layer_norm_tile(kb, h, hb, out, outb, gt, bt, pb, tmp):
    st, stb, mv, mvb, rs, rsb = tmp
    kb.op("dve", lambda e: e.bn_stats(st[:, 0:6], h[:, 0:512]), reads=[hb], writes=[stb])
    kb.op("dve", lambda e: e.bn_stats(st[:, 6:12], h[:, 512:1024]), reads=[hb, stb], writes=[stb])
    kb.op("dve", lambda e: e.bn_aggr(mv[:], st[:]), reads=[stb], writes=[mvb])
    kb.op("act", lambda e: e.activation(out=rs[:], in_=mv[:, 1:2], func=AF.Sqrt, bias=kb.eps_ln[:], scale=1.0),
          reads=[mvb], writes=[rsb])
    kb.op("dve", lambda e: e.reciprocal(rs[:], rs[:]), reads=[rsb], writes=[rsb])
    kb.op("dve", lambda e: e.tensor_scalar(out, h, mv[:, 0:1], rs[:, 0:1], ALU.subtract, ALU.mult),
          reads=[hb, mvb, rsb], writes=[outb])
    kb.op("pool", lambda e: e.tensor_tensor(out, out, gt[:], ALU.mult), reads=[outb, pb], writes=[outb])
    kb.op("pool", lambda e: e.tensor_tensor(out, out, bt[:], ALU.add), reads=[outb, pb], writes=[outb])


def build_ffn_phase(nc, moe, E=None, st=None, prev=(), pfx="", o_cand_fn=None, xres_fn=None, y_fn=None, gather_fn=None):
    if E is None:
        E = 8 if moe else 1
    H = 3584 if (moe or DBG_H) else 2816
    GB = 2
    NG = (H // 128) // GB
    HG = GB * 128
    NTT = NT // 128
    NCH = NT // 512

    def din(name, shape):
        return nc.dram_tensor(pfx + name, list(shape), F32, kind="ExternalInput").ap()

    fusedm = o_cand_fn is not None
    oT = din("oT", [D_MODEL, NT]) if not fusedm else None
    sel_in = din("sel4", [128, 4]) if fusedm else None
    xres = din("xres", [NT, D_MODEL]) if xres_fn is None else None
    w_o = din("w_o", [D_MODEL, D_MODEL])
    ln1_g = din("ln1_g", [D_MODEL]); ln1_b = din("ln1_b", [D_MODEL])
    ln2_g = din("ln2_g", [D_MODEL]); ln2_b = din("ln2_b", [D_MODEL])
    w_gu = din("w_gu", [E, D_MODEL, 2 * H])
    w_dn = din("w_dn", [E, H, D_MODEL])
    if moe:
        w_r = din("w_r", [D_MODEL, 8])
    y = nc.dram_tensor("y", [NT, D_MODEL], F32, kind="ExternalOutput").ap() if y_fn is None else None

    kb = KB(nc, st, prev, pfx)
    ident, identb = make_ident(kb)
    kb.eps_ln = kb.sb("eps_ln", [128, 1], F32)
    epsb = Buf("eps")
    kb.op("pool", lambda e: e.memset(kb.eps_ln[:], LN_EPS), writes=[epsb])

    acc = kb.sb("acc", [128, NTT, D_MODEL], F32)
    accb = [Buf("acc%d" % i) for i in range(NTT)]
    h1T = kb.sb("h1T", [128, 8, NT], BF16)
    h1Tb = [Buf("h1T%d" % i) for i in range(NTT)]
    stg = Stager(kb, 2, 2048)
    SLOT = 3 * 2048
    wbuf = kb.sb("wbuf", [128, 2 * SLOT], BF16)
    gt = kb.sb("gt", [128, D_MODEL], F32); bt = kb.sb("bt", [128, D_MODEL], F32)
    pb = Buf("lnparams")
    if fusedm:
        xr = [kb.sb("xr0", [128, D_MODEL], F32)] * 2
        xrb = [Buf("xr0")] * 2
        cand = None
        osel = kb.sb("osel", [128, D_MODEL], F32); oselb = Buf("osel")
        oT_t = kb.sb("oT_t", [128, 8, 128], BF16); oT_tb = Buf("oT_t")
        sel = kb.sb("sel", [128, 4], F32); selb = Buf("sel")
    else:
        xr = [kb.sb("xr%d" % i, [128, D_MODEL], F32) for i in range(2)]
        xrb = [Buf("xr%d" % i) for i in range(2)]
    h1 = [kb.sb("h1_0", [128, D_MODEL], F32)] * 2
    h1b = [Buf("h1_0")] * 2
    st = kb.sb("st", [128, 12], F32); stb = Buf("st")
    mv = kb.sb("mv", [128, 2], F32); mvb = Buf("mv")
    rs = kb.sb("rs", [128, 1], F32); rsb = Buf("rs")
    lntmp = (st, stb, mv, mvb, rs, rsb)
    sil = [kb.sb("sil%d" % i, [128, 512], BF16) for i in range(2)]
    silb = [Buf("sil%d" % i) for i in range(2)]
    aT = [kb.sb("aT%d" % i, [128, GB, 512], BF16) for i in range(2)]
    aTb = [[Buf("aT%d_%d" % (i, j)) for j in range(GB)] for i in range(2)]
    if moe:
        gate = kb.sb("gate", [128, NTT, 8], F32)
        gateb = [Buf("gate%d" % i) for i in range(NTT)]
        wr_sb = kb.sb("wr_sb", [128, 8, 8], F32); wrb = Buf("wr")
        h1Tf = [kb.sb("h1Tf0", [128, 8, 128], F32)] * 2
        h1Tfb = [Buf("h1Tf0")] * 2
        lg = kb.sb("lg", [128, 8], F32); lgb = Buf("lg")
        mx8 = kb.sb("mx8", [128, 8], F32); mx8b = Buf("mx8")
        rt = kb.sb("rt", [128, 4], F32); rtb = Buf("rt")
        ex8 = kb.sb("ex8", [128, 8], F32); ex8b = Buf("ex8")

    pg = [kb.ps("pg%d" % i, [128, 512]) for i in range(2)]; pgb = [Buf("pg%d" % i) for i in range(2)]
    pu = [kb.ps("pu%d" % i, [128, 512]) for i in range(2)]; pub = [Buf("pu%d" % i) for i in range(2)]
    pd = [kb.ps("pd%d" % i, [128, 512]) for i in range(2)]; pdb = [Buf("pd%d" % i) for i in range(2)]
    pt = [kb.ps("pt%d" % i, [128, 512]) for i in range(2)]; ptb = [Buf("pt%d" % i) for i in range(2)]

    kb.dma("sp", gt[:], bcast_rows(ln1_g), writes=[pb])
    kb.dma("sp", bt[:], bcast_rows(ln1_b), writes=[pb])
    if moe and 'nowr' not in DBG:
        kb.dma("sp", wr_sb[:], w_r.rearrange("(kt p) e -> p kt e", p=128), writes=[wrb])
    wo_sb = wbuf[:, 0:8192].rearrange("p (kt n) -> p kt n", kt=8)
    wob = Buf("wo")
    w_o_v = w_o.rearrange("(kt p) n -> p kt n", p=128)
    wo_stage_done = []
    for i in range(4):
        wo_stage_done.append(stg.load(wo_sb[:, 2 * i:2 * i + 2, :], [wob], w_o_v[:, 2 * i:2 * i + 2, :], 2, 1024))
    oT_sb = wbuf[:, 8192:12288].rearrange("p (kt t) -> p kt t", kt=8)
    oTb = Buf("oTs")
    wslot_b = [Buf("wslot0"), Buf("wslot1")]

    if fusedm:
        kb.dma("sp", sel[:], sel_in, writes=[selb])
        cands = [stg.t[i // 2][:, (i % 2) * 1024:(i % 2) * 1024 + 1024] for i in range(4)]
        candbs = [Buf("cand%d" % i) for i in range(4)]
    else:
        oT_v = oT.rearrange("(kt p) t -> p kt t", p=128)
    xres_v = xres.rearrange("(tt p) d -> tt p d", p=128) if xres is not None else None
    y_v = y.rearrange("(tt p) d -> tt p d", p=128) if y is not None else None

    evac_i = 0
    for c in range(NCH):
        if not fusedm:
            for i in range(2):
                stg.load(oT_sb[:, 4 * i:4 * i + 4, :], [oTb], oT_v[:, 4 * i:4 * i + 4, c * 512:(c + 1) * 512], 4, 512)
        for tl in range(4):
            tt = c * 4 + tl
            r = tt % 2
            if fusedm:
                for rk in range(4):
                    cd, cdb = cands[rk], candbs[rk]
                    kb.dma("sp", cd.rearrange("p (g f) -> p g f", g=4), o_cand_fn(rk, tt), writes=[cdb],
                           deps=wo_stage_done)
                    if rk == 0:
                        kb.op("dve", lambda e, cd=cd: e.tensor_scalar(osel[:], cd, sel[:, 0:1], None, ALU.mult),
                              reads=[cdb, selb], writes=[oselb])
                    else:
                        kb.op("dve", lambda e, rk=rk, cd=cd: e.scalar_tensor_tensor(
                            out=osel[:], in0=cd, scalar=sel[:, rk:rk + 1], in1=osel[:], op0=ALU.mult, op1=ALU.add),
                            reads=[cdb, selb, oselb], writes=[oselb])
                for half in range(2):
                    for j in range(4):
                        kt = half * 4 + j
                        kb.op("pe", lambda e, half=half, kt=kt, j=j: e.transpose(
                            pt[half][:, j * 128:(j + 1) * 128], osel[:, kt * 128:(kt + 1) * 128], ident[:]),
                            reads=[oselb, identb], writes=[ptb[half]])
                    kb.op("act", lambda e, half=half: e.copy(
                        oT_t[:, half * 4:(half + 1) * 4, :], pt[half][:].rearrange("p (j t) -> p j t", j=4)),
                        reads=[ptb[half]], writes=[oT_tb])
            kb.dma("sp", xr[r][:], xres_v[tt] if xres_fn is None else xres_fn(tt), writes=[xrb[r]])
            for nh in range(2):
                pi = evac_i % 2
                evac_i += 1
                for kt in range(8):
                    if fusedm:
                        kb.op("pe", lambda e, pi=pi, kt=kt, nh=nh: e.matmul(
                            pd[pi][:], oT_t[:, kt, :],
                            wo_sb[:, kt, nh * 512:(nh + 1) * 512], start=(kt == 0), stop=(kt == 7)),
                            reads=[oT_tb, wob], writes=[pdb[pi]])
                    else:
                        kb.op("pe", lambda e, pi=pi, kt=kt, tl=tl, nh=nh: e.matmul(
                            pd[pi][:], oT_sb[:, kt, tl * 128:(tl + 1) * 128],
                            wo_sb[:, kt, nh * 512:(nh + 1) * 512], start=(kt == 0), stop=(kt == 7)),
                            reads=[oTb, wob], writes=[pdb[pi]])
                kb.op("dve", lambda e, pi=pi, r=r, nh=nh: e.scalar_tensor_tensor(
                    out=xr[r][:, nh * 512:(nh + 1) * 512], in0=xr[r][:, nh * 512:(nh + 1) * 512],
                    scalar=DN_ALPHA, in1=pd[pi][:], op0=ALU.mult, op1=ALU.add),
                    reads=[xrb[r], pdb[pi]], writes=[xrb[r]])
            layer_norm_tile(kb, xr[r][:], xrb[r], h1[r][:], h1b[r], gt, bt, pb, lntmp)
            kb.op("act", lambda e, tt=tt, r=r: e.mul(acc[:, tt, :], h1[r][:], DN_ALPHA),
                  reads=[h1b[r]], writes=[accb[tt]])
            for half in range(2):
                pi = half
                for j in range(4):
                    kt = half * 4 + j
                    kb.op("pe", lambda e, pi=pi, r=r, kt=kt, j=j: e.transpose(
                        pt[pi][:, j * 128:(j + 1) * 128], h1[r][:, kt * 128:(kt + 1) * 128], ident[:]),
                        reads=[h1b[r], identb], writes=[ptb[pi]])
                kb.op("act", lambda e, pi=pi, tt=tt, half=half: e.copy(
                    h1T[:, half * 4:(half + 1) * 4, tt * 128:(tt + 1) * 128],
                    pt[pi][:].rearrange("p (j t) -> p j t", j=4)),
                    reads=[ptb[pi]], writes=[h1Tb[tt]])
                if moe and 'noh1tf' not in DBG:
                    kb.op("act", lambda e, pi=pi, r=r, half=half: e.copy(
                        h1Tf[r][:, half * 4:(half + 1) * 4, :],
                        pt[pi][:].rearrange("p (j t) -> p j t", j=4)),
                        reads=[ptb[pi]], writes=[h1Tfb[r]])
            if moe and DBG_NOROUTER:
                kb.op("pool", lambda e, tt=tt: e.memset(gate[:, tt, :], 0.5), writes=[gateb[tt]])
            if moe and not DBG_NOROUTER:
                for kt in range(8):
                    kb.op("pe", lambda e, r=r, kt=kt: e.matmul(
                        pg[0][:, 0:8], h1Tf[r][:, kt, :], wr_sb[:, kt, :], start=(kt == 0), stop=(kt == 7)),
                        reads=[h1Tfb[r], wrb], writes=[pgb[0]])
                kb.op("dve", lambda e: e.tensor_copy(lg[:], pg[0][:, 0:8]), reads=[pgb[0]], writes=[lgb])
                kb.op("dve", lambda e: e.max(mx8[:], lg[:]), reads=[lgb], writes=[mx8b])
                kb.op("dve", lambda e: e.tensor_scalar(rt[:, 0:1], mx8[:, 0:1], -1.0, None, ALU.mult),
                      reads=[mx8b], writes=[rtb])
                kb.op("act", lambda e: e.activation(out=ex8[:], in_=lg[:], func=AF.Exp, bias=rt[:, 0:1], scale=1.0),
                      reads=[lgb, rtb], writes=[ex8b])
                kb.op("act", lambda e: e.activation(out=rt[:, 1:2], in_=mx8[:, 1:2], func=AF.Exp, bias=rt[:, 0:1], scale=1.0),
                      reads=[mx8b, rtb], writes=[rtb])
                kb.op("dve", lambda e: e.tensor_scalar(rt[:, 2:3], rt[:, 1:2], 1.0, None, ALU.add),
                      reads=[rtb], writes=[rtb])
                kb.op("dve", lambda e: e.reciprocal(rt[:, 2:3], rt[:, 2:3]), reads=[rtb], writes=[rtb])
                kb.op("dve", lambda e: e.tensor_scalar(ex8[:], ex8[:], rt[:, 2:3], None, ALU.mult),
                      reads=[ex8b, rtb], writes=[ex8b])
                kb.op("dve", lambda e, tt=tt: e.scalar_tensor_tensor(
                    out=gate[:, tt, :], in0=lg[:], scalar=mx8[:, 1:2], in1=ex8[:], op0=ALU.is_ge, op1=ALU.mult),
                    reads=[lgb, mx8b, ex8b], writes=[gateb[tt]])

    kb.dma("sp", gt[:], bcast_rows(ln2_g), writes=[pb])
    kb.dma("sp", bt[:], bcast_rows(ln2_b), writes=[pb])

    w_gu_v = w_gu.rearrange("e (kt p) n -> e p kt n", p=128)
    w_dn_v = w_dn.rearrange("e (hb p) n -> e p hb n", p=128)
    gi = 0
    gu_i = 0
    d_i = 0
    sil_i = 0
    for e_ in range(E):
        for g in range(NG):
            sl = gi % 2
            gi += 1
            base = sl * SLOT
            wg_sb = wbuf[:, base:base + 2048].rearrange("p (kt n) -> p kt n", kt=8)
            wu_sb = wbuf[:, base + 2048:base + 4096].rearrange("p (kt n) -> p kt n", kt=8)
            wd_sb = wbuf[:, base + 4096:base + 6144].rearrange("p (hb n) -> p hb n", hb=GB)
            h0 = g * HG
            wb = wslot_b[sl]
            extra = [wob, oTb] if gi <= 2 else []
            cdeps = []
            if fusedm and gi == 1:
                for cb_ in candbs:
                    cdeps += list(cb_.readers.items())
                    if cb_.last_write is not None:
                        cdeps.append(cb_.last_write)
            stg.load(wg_sb, [wb] + extra, w_gu_v[e_, :, :, h0:h0 + HG], 8, HG, dma_deps=cdeps)
            stg.load(wu_sb, [wb], w_gu_v[e_, :, :, H + h0:H + h0 + HG], 8, HG, dma_deps=cdeps)
            stg.load(wd_sb, [wb], w_dn_v[e_, :, g * GB:(g + 1) * GB, :], GB, 1024)
            for c in range(NCH):
                ab = c % 2
                for blk in range(GB):
                    pi = gu_i % 2
                    gu_i += 1
                    for kt in range(8):
                        kb.op("pe", lambda e, pi=pi, kt=kt, blk=blk, c=c, wg_sb=wg_sb: e.matmul(
                            pg[pi][:], wg_sb[:, kt, blk * 128:(blk + 1) * 128],
                            h1T[:, kt, c * 512:(c + 1) * 512], start=(kt == 0), stop=(kt == 7)),
                            reads=[wb] + h1Tb[c * 4:(c + 1) * 4], writes=[pgb[pi]])
                    for kt in range(8):
                        kb.op("pe", lambda e, pi=pi, kt=kt, blk=blk, c=c, wu_sb=wu_sb: e.matmul(
                            pu[pi][:], wu_sb[:, kt, blk * 128:(blk + 1) * 128],
                            h1T[:, kt, c * 512:(c + 1) * 512], start=(kt == 0), stop=(kt == 7)),
                            reads=[wb], writes=[pub[pi]])
                    si = sil_i % 2
                    sil_i += 1
                    kb.op("act", lambda e, pi=pi, si=si: e.activation(out=sil[si][:], in_=pg[pi][:], func=AF.Silu),
                          reads=[pgb[pi]], writes=[silb[si]])
                    kb.op("dve", lambda e, pi=pi, si=si, ab=ab, blk=blk: e.tensor_tensor(
                        aT[ab][:, blk, :], pu[pi][:], sil[si][:], ALU.mult),
                        reads=[pub[pi], silb[si]], writes=[aTb[ab][blk]])
                for tl in range(4):
                    tt = c * 4 + tl
                    for nh in range(2):
                        pi = d_i % 2
                        d_i += 1
                        for blk in range(GB):
                            kb.op("pe", lambda e, pi=pi, ab=ab, blk=blk, tl=tl, nh=nh, wd_sb=wd_sb: e.matmul(
                                pd[pi][:], aT[ab][:, blk, tl * 128:(tl + 1) * 128],
                                wd_sb[:, blk, nh * 512:(nh + 1) * 512], start=(blk == 0), stop=(blk == GB - 1)),
                                reads=[aTb[ab][blk], wb], writes=[pdb[pi]])
                        if moe and 'nostt' not in DBG:
                            kb.op("dve", lambda e, pi=pi, tt=tt, nh=nh, e_=e_: e.scalar_tensor_tensor(
                                out=acc[:, tt, nh * 512:(nh + 1) * 512], in0=pd[pi][:],
                                scalar=gate[:, tt, e_:e_ + 1], in1=acc[:, tt, nh * 512:(nh + 1) * 512],
                                op0=ALU.mult, op1=ALU.add),
                                reads=[pdb[pi], gateb[tt], accb[tt]], writes=[accb[tt]])
                        else:
                            kb.op("dve", lambda e, pi=pi, tt=tt, nh=nh: e.tensor_tensor(
                                acc[:, tt, nh * 512:(nh + 1) * 512], pd[pi][:],
                                acc[:, tt, nh * 512:(nh + 1) * 512], ALU.add),
                                reads=[pdb[pi], accb[tt]], writes=[accb[tt]])

    outs = []
    for tt in range(NTT):
        r = tt % 2
        layer_norm_tile(kb, acc[:, tt, :], accb[tt], xr[r][:], xrb[r], gt, bt, pb, lntmp)
        outs.append(kb.dma("sp", y_v[tt] if y_fn is None else y_fn(tt), xr[r][:], reads=[xrb[r]]))
        if gather_fn is not None and gather_fn(tt) is not None:
            gs_, gd_ = gather_fn(tt)
            outs.append(kb.collective(gs_, gd_, deps=outs[-2:]))
    kb.wait_all("sp", outs)
    tickets = kb.final_tickets()
    kb.emit()
    return tickets


def build_nsa_phase(nc, st=None, prev=(), pfx="", o_dst_fn=None, gather_fn=None):
    T = SEQ
    ONLY = _os.environ.get('NSA_DBG', '')
    NEG = -30000.0

    def din(name, shape):
        return nc.dram_tensor(pfx + name, list(shape), F32, kind="ExternalInput").ap()

    xT = din("xT", [1024, T])
    wall = din("wall", [1024, 780])
    w1 = din("w1", [2, 2048, 256])
    peT = din("peT", [128, 32])
    w2k = din("w2k", [256, 128])
    w2v = din("w2v", [256, 64])
    ov = din("ov", [512, 128])
    dslc = din("dslc", [128, 512])
    dcmp = din("dcmp", [128, 512])
    nslope = din("nslope", [128, 4])
    relslc = din("relslc", [128, 64])
    relcmp = din("relcmp", [128, 16])
    o_out = None if o_dst_fn is not None else nc.dram_tensor("o", [T, 256], F32, kind="ExternalOutput").ap()

    kb = KB(nc, st, prev, pfx)
    ident, identb = make_ident(kb)

    QT = [kb.sb("QT%d" % i, [128, T], BF16) for i in range(2)]
    KsT2 = kb.sb("KsT2", [128, T], BF16)
    KwT2 = kb.sb("KwT2", [128, T], BF16)
    Vs1 = kb.sb("Vs1", [128, 64, 65], BF16)
    Vw1 = kb.sb("Vw1", [128, 64, 65], BF16)
    gates = kb.sb("gates", [128, 64, 12], F32)
    KcT2 = kb.sb("KcT2", [128, 512], BF16)
    VcX = kb.sb("VcX", [128, 4, 193], BF16)
    nsl = kb.sb("nsl", [128, 4], F32)
    misc = kb.sb("misc", [128, 512], F32)
    cpe = kb.sb("cpe", [128, 4], F32)
    QTb = Buf("QT"); KsTb = Buf("KsT"); KwTb = Buf("KwT"); Vsb = Buf("Vs"); Vwb = Buf("Vw")
    gatesb = Buf("gates"); KcTb = Buf("KcT"); VcXb = Buf("VcX"); nslb = Buf("nsl"); miscb = Buf("misc")
    cpeb = Buf("cpe")

    OVLW = 20800
    ovl = kb.sb("ovl", [128, OVLW], F32)
    off = [0]

    def carve(nwords):
        a = off[0]
        off[0] += nwords
        assert off[0] <= OVLW, off[0]
        return ovl[:, a:a + nwords]

    bk = [kb.ps("bk%d" % i, [128, 512]) for i in range(8)]
    bkb = [Buf("bk%d" % i) for i in range(8)]

    w1_sb = carve(4096).bitcast(BF16).rearrange("p (l h) -> p l h", l=32)
    xbf = [carve(1024).bitcast(BF16).rearrange("p (kt t) -> p kt t", kt=8) for _ in range(2)]
    xbfb = [Buf("xbf0"), Buf("xbf1")]
    KVcT = carve(4096).bitcast(BF16)
    KVcTb = Buf("KVcT")
    wall_sb = carve(3120).bitcast(BF16).rearrange("p (kt n) -> p kt n", kt=8)
    wallb = Buf("wall")
    w1b = Buf("w1")
    stg_aps = [carve(2048) for _ in range(2)]
    stg = Stager(kb, 2, 2048, aps=stg_aps)
    u_t = [carve(512) for _ in range(4)]
    ub = [Buf("u%d" % i) for i in range(4)]
    hidT = carve(1024).bitcast(BF16).rearrange("p (s n) -> p s n", s=4)
    hidTb = Buf("hidT")
    peT_sb = carve(16).bitcast(BF16)
    w2k_sb = carve(128).bitcast(BF16).rearrange("p (hh n) -> p hh n", hh=2)
    w2v_sb = carve(64).bitcast(BF16).rearrange("p (hh n) -> p hh n", hh=2)
    smallb = Buf("small")

    kb.dma("sp", nsl[:], nslope, writes=[nslb])
    wall_v = wall.rearrange("(kt p) n -> p kt n", p=128)
    for i in range(4):
        stg.load(wall_sb[:, 2 * i:2 * i + 2, :], [wallb], wall_v[:, 2 * i:2 * i + 2, :], 2, 780)
    kb.op("pool", lambda e: e.memset(Vs1[:, :, 64:65], 1.0), writes=[Vsb])
    kb.op("pool", lambda e: e.memset(Vw1[:, :, 64:65], 1.0), writes=[Vwb])
    kb.op("pool", lambda e: e.memset(VcX[:, :, 64:65], 1.0), writes=[VcXb])
    kb.op("pool", lambda e: e.memset(KcT2[:], 0.0), writes=[KcTb])
    kb.op("pool", lambda e: e.memset(hidT, 0.0), writes=[hidTb])

    xT_v = xT.rearrange("(kt p) t -> p kt t", p=128)
    ev = 0
    for c in range(32):
        xs = c % 2
        tok = slice(c * 256, (c + 1) * 256)
        stg.load(xbf[xs], [xbfb[xs]], xT_v[:, :, tok], 8, 256)
        for oi, (col0, dst, dstb, scale) in enumerate((
                (0, QT[0], QTb, 0.125), (128, QT[1], QTb, 0.125), (256, KVcT, KVcTb, 1.0),
                (384, KsT2, KsTb, 1.0), (512, KwT2, KwTb, 1.0))):
            pi = ev % 2
            ev += 1
            for kt in range(8):
                kb.op("pe", lambda e, pi=pi, kt=kt, xs=xs, col0=col0: e.matmul(
                    bk[pi][:, 0:256], wall_sb[:, kt, col0:col0 + 128], xbf[xs][:, kt, :],
                    start=(kt == 0), stop=(kt == 7)), reads=[wallb, xbfb[xs]], writes=[bkb[pi]])
            if oi % 2 == 0:
                kb.op("act", lambda e, pi=pi, dst=dst, tok=tok, scale=scale: e.mul(dst[:, tok], bk[pi][:, 0:256], scale),
                      reads=[bkb[pi]], writes=[dstb])
            else:
                kb.op("dve", lambda e, pi=pi, dst=dst, tok=tok, scale=scale: e.tensor_scalar(
                    dst[:, tok], bk[pi][:, 0:256], scale, None, ALU.mult), reads=[bkb[pi]], writes=[dstb])
        for tl in range(2):
            tix = c * 2 + tl
            pi = 2 + tl
            for kt in range(8):
                kb.op("pe", lambda e, pi=pi, kt=kt, xs=xs, tl=tl: e.matmul(
                    bk[pi][:, 0:140], xbf[xs][:, kt, tl * 128:(tl + 1) * 128], wall_sb[:, kt, 640:780],
                    start=(kt == 0), stop=(kt == 7)), reads=[wallb, xbfb[xs]], writes=[bkb[pi]])
            kb.op("dve", lambda e, pi=pi, tix=tix: e.tensor_copy(Vs1[:, tix, 0:64], bk[pi][:, 0:64]),
                  reads=[bkb[pi]], writes=[Vsb])
            kb.op("dve", lambda e, pi=pi, tix=tix: e.tensor_copy(Vw1[:, tix, 0:64], bk[pi][:, 64:128]),
                  reads=[bkb[pi]], writes=[Vwb])
            kb.op("dve", lambda e, pi=pi, tix=tix: e.tensor_copy(gates[:, tix, :], bk[pi][:, 128:140]),
                  reads=[bkb[pi]], writes=[gatesb])
            kb.op("act", lambda e, tix=tix: e.activation(out=gates[:, tix, :], in_=gates[:, tix, :],
                                                         func=AF.Sigmoid), reads=[gatesb], writes=[gatesb])

    for i in range(4):
        si, sview, sbuf_ = stg.acquire()
        v3 = sview[:, 0:2048].rearrange("p (l h) -> p l h", l=8)
        for s in range(2):
            kb.dma("sp", v3[s * 64:(s + 1) * 64], w1[s].rearrange("(l d) h -> d l h", d=64)[:, 8 * i:8 * i + 8, :],
                   writes=[sbuf_])
        kb.op("pool", lambda e, i=i, v3=v3: e.tensor_copy(w1_sb[:, 8 * i:8 * i + 8, :], v3), reads=[sbuf_], writes=[w1b])
    kb.dma("sp", misc[:, 0:32], peT, writes=[miscb])
    kb.op("pool", lambda e: e.tensor_copy(peT_sb, misc[:, 0:32]), reads=[miscb], writes=[smallb])
    kb.dma("sp", misc[:, 0:256].rearrange("p (hh n) -> p hh n", hh=2), w2k.rearrange("(hh p) n -> p hh n", p=128),
           writes=[miscb])
    kb.op("pool", lambda e: e.tensor_copy(w2k_sb, misc[:, 0:256].rearrange("p (hh n) -> p hh n", hh=2)),
          reads=[miscb], writes=[smallb])
    kb.dma("sp", misc[:, 0:128].rearrange("p (hh n) -> p hh n", hh=2), w2v.rearrange("(hh p) n -> p hh n", p=128),
           writes=[miscb])
    kb.op("pool", lambda e: e.tensor_copy(w2v_sb, misc[:, 0:128].rearrange("p (hh n) -> p hh n", hh=2)),
          reads=[miscb], writes=[smallb])
    kb.dma("sp", misc[:, 0:512].rearrange("p (i j) -> p i j", i=4), ov.rearrange("(i p) j -> p i j", p=128),
           writes=[miscb])
    kb.op("pool", lambda e: e.tensor_copy(VcX[:, :, 65:193], misc[:, 0:512].rearrange("p (i j) -> p i j", i=4)),
          reads=[miscb], writes=[VcXb])

    for s in range(2):
        ps_ = slice(s * 64, (s + 1) * 64)
        for hh in range(2):
            idx = s * 2 + hh
            ph = 4 + hh
            for l in range(32):
                kb.op("pe", lambda e, ph=ph, l=l, hh=hh, ps_=ps_: e.matmul(
                    bk[ph][:, 0:511], w1_sb[ps_, l, hh * 128:(hh + 1) * 128], KVcT[ps_, l:l + 8161:16],
                    start=(l == 0), stop=(l == 31)), reads=[w1b, KVcTb], writes=[bkb[ph]])
            for l in range(32):
                kb.op("pe", lambda e, l=l, hh=hh, ps_=ps_: e.matmul(
                    bk[6][:, 0:1], w1_sb[ps_, l, hh * 128:(hh + 1) * 128], peT_sb[ps_, l:l + 1],
                    start=(l == 0), stop=(l == 31)), reads=[w1b, smallb], writes=[bkb[6]])
            kb.op("dve", lambda e, idx=idx: e.tensor_copy(cpe[:, idx:idx + 1], bk[6][:, 0:1]), reads=[bkb[6]], writes=[cpeb])
            u, u2, w_, sg = u_t
            kb.op("dve", lambda e, ph=ph, idx=idx, u=u: e.tensor_scalar(u[:, 0:511], bk[ph][:, 0:511], cpe[:, idx:idx + 1], None, ALU.add),
                  reads=[bkb[ph], cpeb], writes=[ub[0]])
            kb.op("dve", lambda e, u=u, u2=u2: e.tensor_tensor(u2[:, 0:511], u[:, 0:511], u[:, 0:511], ALU.mult),
                  reads=[ub[0]], writes=[ub[1]])
            kb.op("dve", lambda e, u2=u2: e.tensor_scalar(u2[:, 0:511], u2[:, 0:511], 0.044715, 1.0, ALU.mult, ALU.add),
                  reads=[ub[1]], writes=[ub[1]])
            kb.op("dve", lambda e, u=u, u2=u2, w_=w_: e.tensor_tensor(w_[:, 0:511], u2[:, 0:511], u[:, 0:511], ALU.mult),
                  reads=[ub[0], ub[1]], writes=[ub[2]])
            kb.op("act", lambda e, w_=w_, sg=sg: e.activation(out=sg[:, 0:511], in_=w_[:, 0:511], func=AF.Sigmoid,
                                                              scale=1.5957691216057308), reads=[ub[2]], writes=[ub[3]])
            kb.op("dve", lambda e, idx=idx, u=u, sg=sg: e.tensor_tensor(hidT[:, idx, 0:511], u[:, 0:511], sg[:, 0:511], ALU.mult),
                  reads=[ub[0], ub[3]], writes=[hidTb])
    for hh in range(2):
        kb.op("pe", lambda e, hh=hh: e.matmul(bk[7][:, 0:511], w2k_sb[:, hh, :], hidT[:, hh, 0:511],
                                              start=(hh == 0), stop=(hh == 1)), reads=[smallb, hidTb], writes=[bkb[7]])
    kb.op("act", lambda e: e.copy(KcT2[:, 0:511], bk[7][:, 0:511]), reads=[bkb[7]], writes=[KcTb])
    for i in range(4):
        for hh in range(2):
            kb.op("pe", lambda e, hh=hh, i=i: e.matmul(bk[6][:, i * 64:(i + 1) * 64], hidT[:, 2 + hh, i * 128:(i + 1) * 128],
                                                       w2v_sb[:, hh, :], start=(hh == 0), stop=(hh == 1)),
                  reads=[smallb, hidTb], writes=[bkb[6]])
    kb.op("dve", lambda e: e.tensor_copy(VcX[:, :, 0:64], bk[6][:, 0:256].rearrange("p (i d) -> p i d", i=4)),
          reads=[bkb[6]], writes=[VcXb])

    kb.barrier()
    off[0] = 0
    Ebig = carve(4096).bitcast(BF16)
    negselT = carve(4096).bitcast(BF16)
    Bs = [carve(512) for _ in range(4)]
    Bc = [carve(512) for _ in range(4)]
    cbs = carve(256).rearrange("p (h m) -> p h m", h=4)
    cbc = carve(64).rearrange("p (h m) -> p h m", h=4)
    tt_ = [carve(512) for _ in range(4)]
    ttb = [Buf("t%d" % i) for i in range(4)]
    scr = tt_[0]
    dtab = tt_[1]
    rtab = tt_[2]
    pT = [carve(256).bitcast(BF16) for _ in range(4)]
    pTb = [Buf("pT%d" % i) for i in range(4)]
    oacc = [carve(1024).rearrange("p (q f) -> p q f", q=4) for _ in range(2)]
    oaccb = [Buf("oacc0"), Buf("oacc1")]
    imp = carve(512).rearrange("p (q j) -> p q j", q=4)
    impb = Buf("imp")
    sc = carve(512).rearrange("p (q j) -> p q j", q=4)
    sc2 = carve(512).rearrange("p (q j) -> p q j", q=4)
    nsel = carve(512).rearrange("p (q j) -> p q j", q=4)
    scb = [Buf("sc%d" % i) for i in range(4)]
    sc2b = [Buf("sc2%d" % i) for i in range(4)]
    nselb = [Buf("nsel%d" % i) for i in range(4)]
    mx = carve(64).rearrange("p (q j) -> p q j", q=4)
    mxb = [Buf("mx%d" % i) for i in range(4)]
    rd = carve(8)
    rdb = Buf("rd")
    stage = carve(772).rearrange("p (q w) -> p q w", q=4)
    stageb = [Buf("stage%d" % i) for i in range(4)]
    constb = Buf("const")
    Eb = Buf("Ebig")
    nsTb = Buf("negselT")

    kb.dma("sp", dtab, dslc, writes=[ttb[1]])
    for h in range(4):
        kb.op("dve", lambda e, h=h: e.tensor_scalar(Bs[h], dtab, nsl[:, h:h + 1], None, ALU.mult),
              reads=[ttb[1], nslb], writes=[constb])
    kb.dma("sp", dtab, dcmp, writes=[ttb[1]])
    for h in range(4):
        kb.op("dve", lambda e, h=h: e.tensor_scalar(Bc[h], dtab, nsl[:, h:h + 1], None, ALU.mult),
              reads=[ttb[1], nslb], writes=[constb])
    kb.dma("sp", rtab[:, 0:64], relslc, writes=[ttb[2]])
    for h in range(4):
        kb.op("dve", lambda e, h=h: e.tensor_scalar(cbs[:, h, :], rtab[:, 0:64], nsl[:, h:h + 1], None, ALU.mult),
              reads=[ttb[2], nslb], writes=[constb])
    kb.dma("sp", rtab[:, 0:16], relcmp, writes=[ttb[2]])
    for h in range(4):
        kb.op("dve", lambda e, h=h: e.tensor_scalar(cbc[:, h, :], rtab[:, 0:16], nsl[:, h:h + 1], None, ALU.mult),
              reads=[ttb[2], nslb], writes=[constb])
    scrb = ttb[0]
    for i in range(16):
        k0 = i * 512
        kb.op("pool", lambda e: e.memset(scr, 1.0), writes=[scrb])
        kb.op("pool", lambda e, k0=k0: e.affine_select(out=scr, in_=scr, pattern=[[1, 512]], compare_op=ALU.is_ge,
                                                       fill=kb.freg[0.0], base=k0, channel_multiplier=-64),
              reads=[scrb], writes=[scrb])
        kb.op("pool", lambda e, k0=k0: e.affine_select(out=scr, in_=scr, pattern=[[-1, 512]], compare_op=ALU.is_ge,
                                                       fill=kb.freg[0.0], base=63 - k0, channel_multiplier=64),
              reads=[scrb], writes=[scrb])
        kb.op("pool", lambda e, k0=k0: e.tensor_copy(Ebig[:, k0:k0 + 512], scr), reads=[scrb], writes=[Eb])

    o_v = o_out.rearrange("(c q p) f -> c p q f", p=128, q=4) if o_out is not None else None
    cnt = {"s": 0, "t": 0, "p": 0}
    outs = []

    SBANK = (0, 1, 6, 7)
    pend = []
    DEPTH = 3

    def pop_one():
        back, post, _tag = pend.pop(0)
        back()
        if post is not None:
            post()

    def push(back, post=None, tag=""):
        pend.append((back, post, tag))
        while len(pend) > DEPTH:
            pop_one()

    def flush():
        while pend:
            pop_one()

    def unit(h, c, KT, ksl, cb_ap, Bt, mask, acc_bank_views, Vrhs, first, last, sel, qs_min=0):
        hp = slice(64 * (h % 2), 64 * (h % 2) + 64)
        q_ap = QT[h // 2][hp, c * 512:(c + 1) * 512]
        si = SBANK[cnt["s"] % 4]; cnt["s"] += 1
        ti = cnt["t"] % 4; cnt["t"] += 1
        kb.op("pe", lambda e: e.matmul(bk[si][:], KT[hp, ksl], q_ap, start=True, stop=(sel is None)),
              reads=[QTb, KsTb, KwTb, KcTb], writes=[bkb[si]])
        if sel is not None:
            kb.op("pe", lambda e: e.matmul(bk[si][:], Ebig[:, ksl], negselT[:, c * 512:(c + 1) * 512],
                                           start=False, stop=True), reads=[Eb, nsTb], writes=[bkb[si]])
        kb.op("dve", lambda e: e.scalar_tensor_tensor(out=tt_[ti], in0=bk[si][:], scalar=cb_ap, in1=Bt,
                                                      op0=ALU.add, op1=ALU.add),
              reads=[bkb[si], constb], writes=[ttb[ti]])
        if mask is not None:
            pat, base, cm = mask
            kb.op("pool", lambda e: e.affine_select(out=tt_[ti], in_=tt_[ti], pattern=pat, compare_op=ALU.is_ge,
                                                    fill=kb.freg[NEG], base=base, channel_multiplier=cm),
                  reads=[ttb[ti]], writes=[ttb[ti]])
        kb.op("act", lambda e: e.activation(out=pT[ti], in_=tt_[ti], func=AF.Exp), reads=[ttb[ti]], writes=[pTb[ti]])

        def back():
            for qs in range(qs_min, 4):
                view, vb = acc_bank_views[qs]
                kb.op("pe", lambda e, qs=qs, view=view: e.matmul(view, pT[ti][:, qs * 128:(qs + 1) * 128], Vrhs,
                                                                 start=first,
                                                                 stop=(last(qs) if callable(last) else last)),
                      reads=[pTb[ti], Vsb, Vwb, VcXb], writes=[vb])
        return back

    def evac(W):
        for qs in range(4):
            kb.op("dve", lambda e, qs=qs: e.tensor_copy(stage[:, qs, 0:W], bk[2 + qs][:, 0:W]),
                  reads=[bkb[2 + qs]], writes=[stageb[qs]])

    def post_cmp(h, c, oa, oab):
        evac(193)
        for qs in range(4):
            kb.op("dve", lambda e, qs=qs: e.tensor_scalar(rd[:, qs:qs + 1], stage[:, qs, 64:65], 1e-30, None, ALU.max),
                  reads=[stageb[qs]], writes=[rdb])
        kb.op("dve", lambda e: e.reciprocal(rd[:, 0:4], rd[:, 0:4]), reads=[rdb], writes=[rdb])
        for qs in range(4):
            if h == 0:
                kb.op("dve", lambda e, qs=qs: e.tensor_scalar(
                    imp[:, qs, :], stage[:, qs, 65:193], rd[:, qs:qs + 1], None, ALU.mult),
                    reads=[stageb[qs], rdb], writes=[impb])
            else:
                kb.op("dve", lambda e, qs=qs: e.scalar_tensor_tensor(
                    out=imp[:, qs, :], in0=stage[:, qs, 65:193], scalar=rd[:, qs:qs + 1], in1=imp[:, qs, :],
                    op0=ALU.mult, op1=ALU.add), reads=[stageb[qs], rdb, impb], writes=[impb])
        kb.op("dve", lambda e: e.tensor_tensor(
            rd[:, 4:8], rd[:, 0:4], gates[:, 4 * c:4 * c + 4, h * 3 + 0], ALU.mult),
            reads=[rdb, gatesb], writes=[rdb])
        for qs in range(4):
            if ONLY in ('slc', 'win'):
                kb.op("dve", lambda e, qs=qs: e.tensor_scalar(
                    oa[:, qs, h * 64:(h + 1) * 64], stage[:, qs, 0:64], 0.0, None, ALU.mult),
                    reads=[stageb[qs], rdb], writes=[oab])
            else:
                kb.op("dve", lambda e, qs=qs: e.tensor_scalar(
                    oa[:, qs, h * 64:(h + 1) * 64], stage[:, qs, 0:64], rd[:, 4 + qs:5 + qs], None, ALU.mult),
                    reads=[stageb[qs], rdb], writes=[oab])

    def post_sw(h, c, br, oa, oab):
        evac(65)
        for qs in range(4):
            kb.op("dve", lambda e, qs=qs: e.tensor_scalar(rd[:, qs:qs + 1], stage[:, qs, 64:65], 1e-30, None, ALU.max),
                  reads=[stageb[qs]], writes=[rdb])
        kb.op("dve", lambda e: e.reciprocal(rd[:, 0:4], rd[:, 0:4]), reads=[rdb], writes=[rdb])
        kb.op("dve", lambda e: e.tensor_tensor(
            rd[:, 0:4], rd[:, 0:4], gates[:, 4 * c:4 * c + 4, h * 3 + br], ALU.mult),
            reads=[rdb, gatesb], writes=[rdb])
        for qs in range(4):
            if ONLY and ONLY != ('win' if br == 2 else 'slc'):
                kb.op("dve", lambda e, qs=qs: e.tensor_copy(rd[:, 4 + qs:5 + qs], stage[:, qs, 64:65]),
                      reads=[stageb[qs]], writes=[rdb])
                continue
            kb.op("dve", lambda e, qs=qs: e.scalar_tensor_tensor(
                out=oa[:, qs, h * 64:(h + 1) * 64], in0=stage[:, qs, 0:64], scalar=rd[:, qs:qs + 1],
                in1=oa[:, qs, h * 64:(h + 1) * 64], op0=ALU.mult, op1=ALU.add),
                reads=[stageb[qs], rdb, oab], writes=[oab])

    def selection(c):
        for qs in range(4):
            t0 = 512 * c + 128 * qs
            kb.op("pool", lambda e, qs=qs, t0=t0: e.affine_select(
                out=sc[:, qs, :], in_=imp[:, qs, :], pattern=[[-64, 128]], compare_op=ALU.is_ge, fill=kb.freg[-1e9],
                base=t0 - 128, channel_multiplier=1), reads=[impb], writes=[scb[qs]])
            kb.op("pool", lambda e, qs=qs: e.memset(sc[:, qs, 0:1], -1e9), writes=[scb[qs]])
            kb.op("dve", lambda e, qs=qs: e.max(mx[:, qs, 0:8], sc[:, qs, :]), reads=[scb[qs]], writes=[mxb[qs]])
            kb.op("dve", lambda e, qs=qs: e.match_replace(sc2[:, qs, :], mx[:, qs, 0:8], sc[:, qs, :], -2e9),
                  reads=[scb[qs], mxb[qs]], writes=[sc2b[qs]])
            kb.op("dve", lambda e, qs=qs: e.max(mx[:, qs, 8:16], sc2[:, qs, :]), reads=[sc2b[qs]], writes=[mxb[qs]])
            kb.op("dve", lambda e, qs=qs: e.tensor_scalar(nsel[:, qs, :], sc[:, qs, :], mx[:, qs, 12:13], NEG,
                                                          ALU.is_lt, ALU.mult),
                  reads=[scb[qs], mxb[qs]], writes=[nselb[qs]])
            kb.op("pool", lambda e, qs=qs, t0=t0: e.affine_select(
                out=nsel[:, qs, :], in_=nsel[:, qs, :], pattern=[[-64, 128]], compare_op=ALU.is_ge, fill=kb.freg[0.0],
                base=t0 - 128, channel_multiplier=1), reads=[nselb[qs]], writes=[nselb[qs]])
            kb.op("pool", lambda e, qs=qs: e.memset(nsel[:, qs, 0:1], 0.0), writes=[nselb[qs]])
            kb.op("pe", lambda e, qs=qs: e.transpose(bk[6][:, qs * 128:(qs + 1) * 128], nsel[:, qs, :], ident[:]),
                  reads=[nselb[qs], identb], writes=[bkb[6]])
        kb.op("act", lambda e: e.copy(negselT[:, c * 512:(c + 1) * 512], bk[6][:]), reads=[bkb[6]], writes=[nsTb])

    for c in range(16):
        oa = oacc[c % 2]
        oab = oaccb[c % 2]
        for h in range(4):
            views = [(bk[2 + qs][:, 0:193], bkb[2 + qs]) for qs in range(4)]
            ni = c // 4 + 1
            for i in range(ni):
                mask = None
                if i >= c // 4 - 1:
                    mask = ([[1, 512]], 512 * c - 2048 * i - 31, -16)
                bk_ = unit(h, c, KcT2, slice(i * 128, (i + 1) * 128), cbc[:, h, c - 4 * i:c - 4 * i + 1], Bc[h], mask,
                           views, VcX[:, i, :], i == 0, i == ni - 1, None)
                push(bk_, (lambda h=h, c=c, oa=oa, oab=oab: post_cmp(h, c, oa, oab)) if i == ni - 1 else None, "cmp")
        first_win = True
        for br, KT, V1 in ((2, KwT2, Vw1), (1, KsT2, Vs1)):
            if br == 1:
                pass
            for h in range(4):
                views = [(bk[2 + qs][:, 0:65], bkb[2 + qs]) for qs in range(4)]
                j0 = max(0, 4 * c - 4) if br == 2 else 0
                j1 = 4 * c + 3
                for j in range(j0, j1 + 1):
                    rel = 512 * c - 128 * j
                    qs_min = 0
                    if j >= 4 * c:
                        mask = ([[1, 512]], rel, -1)
                        qs_min = j - 4 * c
                    elif br == 2:
                        mask = ([[-1, 512]], 511 - rel, 1)
                    else:
                        mask = None
                    m = 4 * c - j + 3
                    if first_win and not any(tag == "cmp" for _, _, tag in pend):
                        selection(c)
                        first_win = False
                    bk_ = unit(h, c, KT, slice(j * 128, (j + 1) * 128), cbs[:, h, m:m + 1], Bs[h], mask, views,
                               V1[:, j, :], j == j0, (lambda qs, j=j, c=c: j == 4 * c + qs), True if br == 1 else None, qs_min)
                    push(bk_, (lambda h=h, c=c, br=br, oa=oa, oab=oab: post_sw(h, c, br, oa, oab)) if j == j1 else None)
        assert not first_win

        def store(c=c, oa=oa, oab=oab):
            outs.append(kb.dma("sp", o_v[c] if o_dst_fn is None else o_dst_fn(c), oa, reads=[oab]))
            if gather_fn is not None and gather_fn(c) is not None:
                gs_, gd_ = gather_fn(c)
                outs.append(kb.collective(gs_, gd_, deps=outs[-2:]))
        push(store, None)
    flush()
    kb.wait_all("sp", outs)
    tickets = kb.final_tickets()
    kb.emit()
    return tickets


def build_gla_phase(nc, st=None, prev=(), pfx="", x_src_fn=None, o_dst_fn=None, gather_fn=None):
    T = SEQ
    QSCALE = 128.0 ** -0.5

    def din(name, shape):
        return nc.dram_tensor(pfx + name, list(shape), F32, kind="ExternalInput").ap()

    xT = din("xT", [1024, T]) if x_src_fn is None else None
    wall = din("wall", [1024, 784])
    wg2 = din("wg2", [16, 128])
    bg2 = din("bg2", [1, 128])
    hng = din("hng", [256])
    lblk = din("lblk", [128, 128])
    ublk = din("ublk", [128, 128])
    o_out = None if o_dst_fn is not None else nc.dram_tensor("o", [T, 256], F32, kind="ExternalOutput").ap()

    kb = KB(nc, st, prev, pfx)
    if x_src_fn is not None:
        ident, identb = make_ident(kb)
        xtok = [kb.sb("xtok%d" % i, [128, D_MODEL], F32) for i in range(2)]
        xtokb = [Buf("xtok0"), Buf("xtok1")]
    one1 = kb.sb("one1", [128, 1], F32)
    epsr = kb.sb("epsr", [128, 1], F32)
    cb = Buf("consts")
    kb.op("pool", lambda e: e.memset(one1[:], 1.0), writes=[cb])
    kb.op("pool", lambda e: e.memset(epsr[:], RMS_EPS), writes=[cb])

    wall_sb = kb.sb("wall_sb", [128, 8, 784], BF16); wallb = Buf("wall")
    stg = Stager(kb, 2, 2048)
    xbf = [kb.sb("xbf%d" % i, [128, 8, 256], BF16) for i in range(2)]
    xbfb = [Buf("xbf0"), Buf("xbf1")]
    L01 = kb.sb("L01", [128, 128], F32)
    LS = kb.sb("LS", [128, 128], F32)
    US = kb.sb("US", [128, 128], F32)
    hn = kb.sb("hn", [128, 256], F32)
    wg2_sb = kb.sb("wg2_sb", [16, 128], BF16)
    bg2_sb = kb.sb("bg2_sb", [1, 128], BF16)
    ones_bf = kb.sb("ones_bf", [1, 128], BF16)
    misc = kb.sb("misc", [128, 128], F32); miscb = Buf("misc")

    qT_sb = kb.sb("qT_sb", [128, 256], F32); qTb = Buf("qT")
    kT_sb = kb.sb("kT_sb", [128, 256], F32); kTb = Buf("kT")
    alT_sb = kb.sb("alT_sb", [16, 256], BF16); alTb = Buf("alT")
    v_bf = kb.sb("v_bf", [128, 256], BF16); vb = Buf("v")
    k_tok = kb.sb("k_tok", [128, 128], F32); ktb = Buf("ktok")
    gs = kb.sb("gs", [128, 256], F32); gsb = Buf("gs")
    e1 = kb.sb("e1", [128, 128], F32); e1b = Buf("e1")
    la = kb.sb("la", [128, 128], F32); lab = Buf("la")
    bT_sb = kb.sb("bT_sb", [128, 2, 64], F32); bTb = Buf("bT")
    bd = kb.sb("bd", [128, 2, 64], F32); bdb = Buf("bd")
    eg = kb.sb("eg", [128, 128], F32); egb = Buf("eg")
    ieg = kb.sb("ieg", [128, 128], F32); iegb = Buf("ieg")
    eb = kb.sb("eb", [128, 128], F32); ebb = Buf("eb")
    erb = kb.sb("erb", [128, 128], F32); erbb = Buf("erb")
    qgT = kb.sb("qgT", [128, 128], BF16); qgb = Buf("qg")
    kgT = kb.sb("kgT", [128, 128], BF16); kgb = Buf("kg")
    qbP = [kb.sb("qbP%d" % i, [128, 128], BF16) for i in range(2)]; qbPb = [Buf("qbP0"), Buf("qbP1")]
    kbt = kb.sb("kbt", [128, 128], BF16); kbtb = Buf("kbt")
    AT = kb.sb("AT", [128, 128], BF16); ATb = Buf("AT")
    dec = kb.sb("dec", [128, 2], F32); decb = Buf("dec")
    S32 = kb.sb("S32", [128, 256], F32); S32b = Buf("S32")
    Sbf = [kb.sb("Sbf%d" % i, [128, 256], BF16) for i in range(3)]; Sbfb = [Buf("Sbf%d" % i) for i in range(3)]
    st = kb.sb("st", [128, 6], F32); stb = Buf("st")
    mv = kb.sb("mv", [128, 2], F32); mvb = Buf("mv")
    rs = kb.sb("rs", [128, 2], F32); rsb = Buf("rs")
    ot = [kb.sb("ot%d" % i, [128, 256], F32) for i in range(2)]; otb = [Buf("ot0"), Buf("ot1")]

    bk = [kb.ps("bk%d" % i, [128, 512]) for i in range(8)]
    bkb = [Buf("bk%d" % i) for i in range(8)]

    wall_v = wall.rearrange("(kt p) n -> p kt n", p=128)
    for i in range(4):
        stg.load(wall_sb[:, 2 * i:2 * i + 2, :], [wallb], wall_v[:, 2 * i:2 * i + 2, :], 2, 784)
    kb.dma("sp", L01[:], lblk, writes=[cb])
    kb.op("dve", lambda e: e.tensor_scalar(LS[:], L01[:], -1.0 / 16.0, None, ALU.mult), reads=[cb], writes=[cb])
    kb.dma("sp", misc[:], ublk, writes=[miscb])
    kb.op("dve", lambda e: e.tensor_scalar(US[:], misc[:], -1.0 / 16.0, None, ALU.mult), reads=[miscb], writes=[cb])
    kb.dma("sp", hn[:], bcast_rows(hng), writes=[cb])
    kb.dma("sp", misc[0:16, :], wg2, writes=[miscb])
    kb.op("dve", lambda e: e.tensor_copy(wg2_sb[:], misc[0:16, :]), reads=[miscb], writes=[cb])
    kb.dma("sp", misc[0:1, :], bg2, writes=[miscb])
    kb.op("dve", lambda e: e.tensor_copy(bg2_sb[:], misc[0:1, :]), reads=[miscb], writes=[cb])
    kb.op("pool", lambda e: e.memset(ones_bf[:], 1.0), writes=[cb])
    kb.op("pool", lambda e: e.memset(qbP[0][:], 0.0), writes=[qbPb[0]])
    kb.op("pool", lambda e: e.memset(qbP[1][:], 0.0), writes=[qbPb[1]])
    kb.op("pool", lambda e: e.memset(S32[:], 0.0), writes=[S32b])

    xT_v = xT.rearrange("(kt p) t -> p kt t", p=128) if x_src_fn is None else None
    o_v = o_out.rearrange("(m p) f -> m p f", p=128) if o_out is not None else None
    outs = []
    s_i = 0
    have_S = False
    for c in range(32):
        xs = c % 2
        if x_src_fn is None:
            stg.load(xbf[xs][:], [xbfb[xs]], xT_v[:, :, c * 256:(c + 1) * 256], 8, 256)
        else:
            for tl in range(2):
                xi = (c * 2 + tl) % 2
                kb.dma("sp", xtok[xi][:], x_src_fn(c * 2 + tl), writes=[xtokb[xi]])
                for half in range(2):
                    for j in range(4):
                        kt = half * 4 + j
                        kb.op("pe", lambda e, half=half, j=j, kt=kt, xi=xi: e.transpose(
                            bk[half][:, j * 128:(j + 1) * 128], xtok[xi][:, kt * 128:(kt + 1) * 128], ident[:]),
                            reads=[xtokb[xi], identb], writes=[bkb[half]])
                    kb.op("act", lambda e, half=half, xs=xs, tl=tl: e.copy(
                        xbf[xs][:, half * 4:(half + 1) * 4, tl * 128:(tl + 1) * 128],
                        bk[half][:].rearrange("p (j t) -> p j t", j=4)),
                        reads=[bkb[half]], writes=[xbfb[xs]])
        for kt in range(8):
            kb.op("pe", lambda e, kt=kt, xs=xs: e.matmul(bk[0][:, 0:256], wall_sb[:, kt, 0:128], xbf[xs][:, kt, :],
                                                         start=(kt == 0), stop=(kt == 7)),
                  reads=[wallb, xbfb[xs]], writes=[bkb[0]])
        kb.op("dve", lambda e: e.tensor_scalar(qT_sb[:], bk[0][:, 0:256], QSCALE, None, ALU.mult),
              reads=[bkb[0]], writes=[qTb])
        for kt in range(8):
            kb.op("pe", lambda e, kt=kt, xs=xs: e.matmul(bk[1][:, 0:256], wall_sb[:, kt, 128:256], xbf[xs][:, kt, :],
                                                         start=(kt == 0), stop=(kt == 7)),
                  reads=[wallb, xbfb[xs]], writes=[bkb[1]])
        kb.op("act", lambda e: e.copy(kT_sb[:], bk[1][:, 0:256]), reads=[bkb[1]], writes=[kTb])
        for kt in range(8):
            kb.op("pe", lambda e, kt=kt, xs=xs: e.matmul(bk[2][0:16, 0:256], wall_sb[:, kt, 768:784], xbf[xs][:, kt, :],
                                                         start=(kt == 0), stop=(kt == 7)),
                  reads=[wallb, xbfb[xs]], writes=[bkb[2]])
        kb.op("act", lambda e: e.copy(alT_sb[:], bk[2][0:16, 0:256]), reads=[bkb[2]], writes=[alTb])
        for tl in range(2):
            m = c * 2 + tl
            tk = slice(tl * 128, (tl + 1) * 128)
            for kt in range(8):
                kb.op("pe", lambda e, kt=kt, xs=xs, tk=tk: e.matmul(bk[3][:, 0:256], xbf[xs][:, kt, tk], wall_sb[:, kt, 256:512],
                                                                    start=(kt == 0), stop=(kt == 7)),
                      reads=[wallb, xbfb[xs]], writes=[bkb[3]])
            for kt in range(8):
                kb.op("pe", lambda e, kt=kt, xs=xs, tk=tk: e.matmul(bk[3][:, 256:384], xbf[xs][:, kt, tk], wall_sb[:, kt, 128:256],
                                                                    start=(kt == 0), stop=(kt == 7)),
                      reads=[wallb, xbfb[xs]], writes=[bkb[3]])
            kb.op("dve", lambda e: e.tensor_copy(v_bf[:], bk[3][:, 0:256]), reads=[bkb[3]], writes=[vb])
            kb.op("dve", lambda e: e.tensor_copy(k_tok[:], bk[3][:, 256:384]), reads=[bkb[3]], writes=[ktb])
            for kt in range(8):
                kb.op("pe", lambda e, kt=kt, xs=xs, tk=tk: e.matmul(bk[4][:, 0:256], xbf[xs][:, kt, tk], wall_sb[:, kt, 512:768],
                                                                    start=(kt == 0), stop=(kt == 7)),
                      reads=[wallb, xbfb[xs]], writes=[bkb[4]])
            kb.op("act", lambda e: e.activation(out=gs[:], in_=bk[4][:, 0:256], func=AF.Silu), reads=[bkb[4]], writes=[gsb])
            kb.op("pool", lambda e: e.tensor_tensor(gs[:], gs[:], hn[:], ALU.mult), reads=[gsb, cb], writes=[gsb])
            kb.op("pe", lambda e, tk=tk: e.matmul(bk[2][:, 256:384], alT_sb[:, tk], wg2_sb[:], start=True, stop=False),
                  reads=[alTb, cb], writes=[bkb[2]])
            kb.op("pe", lambda e: e.matmul(bk[2][:, 256:384], ones_bf[:], bg2_sb[:], start=False, stop=True),
                  reads=[cb], writes=[bkb[2]])
            kb.op("act", lambda e: e.activation(out=e1[:], in_=bk[2][:, 256:384], func=AF.Exp, scale=-1.0),
                  reads=[bkb[2]], writes=[e1b])
            kb.op("act", lambda e: e.activation(out=la[:], in_=e1[:], func=AF.Ln, bias=one1[:], scale=1.0),
                  reads=[e1b, cb], writes=[lab])
            kb.op("pe", lambda e: e.matmul(bk[5][:, 0:128], la[:], LS[:], start=True, stop=True),
                  reads=[lab, cb], writes=[bkb[5]])
            kb.op("pe", lambda e: e.matmul(bk[5][:, 128:256], US[:], la[:], start=True, stop=True),
                  reads=[lab, cb], writes=[bkb[5]])
            kb.op("act", lambda e: e.copy(bT_sb[:], bk[5][:, 0:128].rearrange("p (c t) -> p c t", c=2)),
                  reads=[bkb[5]], writes=[bTb])
            kb.op("act", lambda e: e.activation(out=erb[:], in_=bk[5][:, 128:256], func=AF.Exp), reads=[bkb[5]], writes=[erbb])
            for cc in range(2):
                kb.op("dve", lambda e, cc=cc: e.tensor_scalar(bd[:, cc, :], bT_sb[:, cc, :], bT_sb[:, cc, 32:33], None,
                                                              ALU.subtract), reads=[bTb], writes=[bdb])
            kb.op("act", lambda e: e.activation(out=eg[:], in_=bd[:].rearrange("p c t -> p (c t)"), func=AF.Exp),
                  reads=[bdb], writes=[egb])
            kb.op("act", lambda e: e.activation(out=eb[:], in_=bT_sb[:].rearrange("p c t -> p (c t)"), func=AF.Exp),
                  reads=[bTb], writes=[ebb])
            kb.op("act", lambda e: e.activation(out=dec[:], in_=bT_sb[:, :, 63], func=AF.Exp), reads=[bTb], writes=[decb])
            kb.op("dve", lambda e: e.reciprocal(ieg[:], eg[:]), reads=[egb], writes=[iegb])
            kb.op("dve", lambda e, tk=tk: e.tensor_tensor(qgT[:], qT_sb[:, tk], eg[:], ALU.mult), reads=[qTb, egb], writes=[qgb])
            kb.op("dve", lambda e, tk=tk: e.tensor_tensor(kgT[:], kT_sb[:, tk], ieg[:], ALU.mult), reads=[kTb, iegb], writes=[kgb])
            kb.op("dve", lambda e, tk=tk: e.tensor_tensor(qbP[0][:, 0:64], qT_sb[:, tk.start:tk.start + 64], eb[:, 0:64], ALU.mult),
                  reads=[qTb, ebb], writes=[qbPb[0]])
            kb.op("dve", lambda e, tk=tk: e.tensor_tensor(qbP[1][:, 64:128], qT_sb[:, tk.start + 64:tk.start + 128], eb[:, 64:128], ALU.mult),
                  reads=[qTb, ebb], writes=[qbPb[1]])
            kb.op("dve", lambda e: e.tensor_tensor(kbt[:], k_tok[:], erb[:], ALU.mult), reads=[ktb, erbb], writes=[kbtb])
            kb.op("pe", lambda e: e.matmul(bk[6][:, 0:128], kgT[:], qgT[:], start=True, stop=True),
                  reads=[kgb, qgb], writes=[bkb[6]])
            kb.op("dve", lambda e: e.tensor_tensor(AT[:], bk[6][:, 0:128], L01[:], ALU.mult), reads=[bkb[6], cb], writes=[ATb])
            s_prev = s_i
            terms = [(AT, ATb, v_bf, vb)]
            if have_S:
                terms.append((qbP[0], qbPb[0], Sbf[s_prev % 3], Sbfb[s_prev % 3]))
            for cc in range(2):
                pr = slice(cc * 64, (cc + 1) * 64)
                kb.op("pe", lambda e, pr=pr: e.matmul(bk[6][:, 128:384], kbt[pr, :], v_bf[pr, :], start=True, stop=True),
                      reads=[kbtb, vb], writes=[bkb[6]])
                kb.op("dve", lambda e, cc=cc: e.scalar_tensor_tensor(out=S32[:], in0=S32[:], scalar=dec[:, cc:cc + 1],
                                                                     in1=bk[6][:, 128:384], op0=ALU.mult, op1=ALU.add),
                      reads=[S32b, decb, bkb[6]], writes=[S32b])
                s_i += 1
                kb.op("act", lambda e, si=s_i: e.copy(Sbf[si % 3][:], S32[:]), reads=[S32b], writes=[Sbfb[s_i % 3]])
                if cc == 0:
                    terms.append((qbP[1], qbPb[1], Sbf[s_i % 3], Sbfb[s_i % 3]))
            have_S = True
            for ti, (l_, lb_, r_, rb_) in enumerate(terms):
                kb.op("pe", lambda e, l_=l_, r_=r_, ti=ti, n=len(terms): e.matmul(
                    bk[7][:, 0:256], l_[:], r_[:], start=(ti == 0), stop=(ti == n - 1)),
                    reads=[lb_, rb_], writes=[bkb[7]])
            kb.op("dve", lambda e: e.bn_stats(st[:], bk[7][:, 0:256]), reads=[bkb[7]], writes=[stb])
            kb.op("dve", lambda e: e.bn_aggr(mv[:], st[:]), reads=[stb], writes=[mvb])
            kb.op("dve", lambda e: e.tensor_tensor(rs[:, 0:1], mv[:, 0:1], mv[:, 0:1], ALU.mult), reads=[mvb], writes=[rsb])
            kb.op("dve", lambda e: e.tensor_tensor(rs[:, 0:1], rs[:, 0:1], mv[:, 1:2], ALU.add), reads=[mvb, rsb], writes=[rsb])
            kb.op("act", lambda e: e.activation(out=rs[:, 1:2], in_=rs[:, 0:1], func=AF.Sqrt, bias=epsr[:], scale=1.0),
                  reads=[rsb, cb], writes=[rsb])
            kb.op("dve", lambda e: e.reciprocal(rs[:, 1:2], rs[:, 1:2]), reads=[rsb], writes=[rsb])
            oi = m % 2
            kb.op("dve", lambda e, oi=oi: e.scalar_tensor_tensor(out=ot[oi][:], in0=bk[7][:, 0:256], scalar=rs[:, 1:2],
                                                                 in1=gs[:], op0=ALU.mult, op1=ALU.mult),
                  reads=[bkb[7], rsb, gsb], writes=[otb[oi]])
            outs.append(kb.dma("sp", o_v[m] if o_dst_fn is None else o_dst_fn(m), ot[oi][:], reads=[otb[oi]]))
            if gather_fn is not None and gather_fn(m) is not None:
                gs_, gd_ = gather_fn(m)
                outs.append(kb.collective(gs_, gd_, deps=outs[-8:]))
    kb.wait_all("sp", outs)
    tickets = kb.final_tickets()
    kb.emit()
    return tickets


def build_fused(nc):
    def internal(name, shape):
        return nc.dram_tensor(name, list(shape), F32, kind="Internal").ap()

    st = SemState(nc)
    oA = [internal("i_oA%d" % k, [1024, 256]) for k in range(8)]
    gA = [internal("i_gA%d" % k, [4 * 1024, 256]) for k in range(8)]
    x1 = [internal("i_x1%d" % k, [256, D_MODEL]) for k in range(8)]
    gX = [internal("i_gX%d" % k, [4 * 256, D_MODEL]) for k in range(8)]
    oC = [internal("i_oC%d" % k, [1024, 256]) for k in range(8)]
    gC = [internal("i_gC%d" % k, [4 * 1024, 256]) for k in range(8)]
    y = nc.dram_tensor("y", [NT, D_MODEL], F32, kind="ExternalOutput").ap()

    def mixer_cand(g_list):
        def fn(rk, tt):
            t0 = rk * NT + tt * 128
            k, i = t0 // 1024, t0 % 1024
            return g_list[k].rearrange("(g t) f -> t g f", g=4)[i:i + 128]
        return fn

    t = build_nsa_phase(
        nc, st, (), "a_",
        o_dst_fn=lambda c: oA[c // 2][(c % 2) * 512:(c % 2) * 512 + 512, :].rearrange("(q p) f -> p q f", p=128),
        gather_fn=lambda c: (oA[c // 2], gA[c // 2]) if c % 2 == 1 else None)
    t = build_ffn_phase(
        nc, False, 1, st, t, "b_", o_cand_fn=mixer_cand(gA),
        y_fn=lambda tt: x1[tt // 2][(tt % 2) * 128:(tt % 2) * 128 + 128, :],
        gather_fn=lambda tt: (x1[tt // 2], gX[tt // 2]) if tt % 2 == 1 else None)

    def x_src_fn(m):
        r, k, i = m // 16, (m % 16) // 2, (m % 2) * 128
        return gX[k][r * 256 + i:r * 256 + i + 128, :]

    t = build_gla_phase(
        nc, st, t, "c_", x_src_fn=x_src_fn,
        o_dst_fn=lambda m: oC[m // 8][(m % 8) * 128:(m % 8) * 128 + 128, :],
        gather_fn=lambda m: (oC[m // 8], gC[m // 8]) if m % 8 == 7 else None)
    t = build_ffn_phase(
        nc, True, 8, st, t, "d_", o_cand_fn=mixer_cand(gC),
        xres_fn=lambda tt: x1[tt // 2][(tt % 2) * 128:(tt % 2) * 128 + 128, :],
        y_fn=lambda tt: y[tt * 128:(tt + 1) * 128, :])
    st.es.close()
    return nc


_PROG_CACHE = {}


def _get_prog(key, builder):
    if key not in _PROG_CACHE:
        nc = bass.Bass("TRN2", target_bir_lowering=False)
        builder(nc)
        _PROG_CACHE[key] = nc
    return _PROG_CACHE[key]


def run_ffn_phase(moe, oT_list, xres_list, w_o, ln1_g, ln1_b, ln2_g, ln2_b, w_gu, w_dn, w_r=None):
    E = w_gu.shape[0]
    nc = _get_prog("ffn_%s_%d" % (moe, E), lambda nc: build_ffn_phase(nc, moe, E))
    in_maps = []
    for c in range(NCORES):
        m = {"oT": oT_list[c], "xres": xres_list[c], "w_o": w_o, "ln1_g": ln1_g, "ln1_b": ln1_b,
             "ln2_g": ln2_g, "ln2_b": ln2_b, "w_gu": w_gu, "w_dn": w_dn}
        if moe:
            m["w_r"] = w_r
        in_maps.append(m)
    res = run_bass_kernel_spmd(nc, in_maps, core_ids=list(range(NCORES)))
    return [r["y"] for r in res.results]


def _nsa_consts():
    kl = np.arange(128, dtype=np.float32)[:, None]
    ql = np.arange(512, dtype=np.float32)[None, :]
    dslc = np.ascontiguousarray(np.broadcast_to(ql - kl, (128, 512)).astype(np.float32))
    dcmp = np.ascontiguousarray(np.broadcast_to(ql - 16.0 * kl, (128, 512)).astype(np.float32))
    relslc = np.ascontiguousarray(np.broadcast_to((128.0 * np.arange(64) - 384.0)[None, :], (128, 64)).astype(np.float32))
    relcmp = np.ascontiguousarray(np.broadcast_to((512.0 * np.arange(16) - 31.0)[None, :], (128, 16)).astype(np.float32))
    n_cmp = (SEQ - 32) // 16 + 1
    cs = np.arange(n_cmp) * 16
    ce = cs + 31
    bs = np.arange(SEQ // 64) * 64
    be = bs + 63
    ov = np.zeros((512, 128), np.float32)
    ov[:n_cmp] = ((cs[:, None] <= be[None]) & (ce[:, None] >= bs[None])).astype(np.float32)
    return dslc, dcmp, relslc, relcmp, ov


def run_nsa_phase(x, w_in, pe_k, pe_v, wk1, wk2, wv1, wv2):
    nc = _get_prog("nsa", build_nsa_phase)
    dslc, dcmp, relslc, relcmp, ov = _nsa_consts()
    slopes = (2.0 ** (-8.0 * (np.arange(16, dtype=np.float32) + 1.0) / 16)).astype(np.float32)
    peT = np.ascontiguousarray(np.concatenate([pe_k.T, pe_v.T], axis=0))
    w1 = np.ascontiguousarray(np.stack([wk1, wv1]))
    w2k = np.ascontiguousarray(np.concatenate([wk2, wk2], axis=1))
    xTs = [np.ascontiguousarray(x[b].T) for b in range(BATCH)]
    in_maps = []
    for core in range(NCORES):
        b, g = core // 4, core % 4
        kvcol = lambda i: w_in[:, 1024 + i * 256 + g * 64:1024 + i * 256 + (g + 1) * 64]
        wall = np.concatenate([
            w_in[:, g * 256:(g + 1) * 256], kvcol(0), kvcol(1), kvcol(2), kvcol(2), kvcol(4), kvcol(4),
            kvcol(3), kvcol(5), w_in[:, 2560 + g * 12:2560 + (g + 1) * 12]], axis=1)
        nsl = np.ascontiguousarray(np.broadcast_to(-slopes[g * 4:(g + 1) * 4][None, :], (128, 4)).astype(np.float32))
        in_maps.append({"xT": xTs[b], "wall": np.ascontiguousarray(wall), "w1": w1, "peT": peT, "w2k": w2k,
                        "w2v": wv2, "ov": ov, "dslc": dslc, "dcmp": dcmp, "nslope": nsl, "relslc": relslc,
                        "relcmp": relcmp})
    res = run_bass_kernel_spmd(nc, in_maps, core_ids=list(range(NCORES)))
    o = np.empty((BATCH, SEQ, 1024), np.float32)
    for core in range(NCORES):
        b, g = core // 4, core % 4
        o[b, :, g * 256:(g + 1) * 256] = res.results[core]["o"]
    return o


def _gla_consts():
    t = np.arange(128)
    same = (t[:, None] // 64) == (t[None, :] // 64)
    lblk = (same & (t[:, None] <= t[None, :])).astype(np.float32)
    ublk = (same & (t[:, None] > t[None, :])).astype(np.float32)
    return lblk, ublk


def run_gla_phase(x, w_in, w_gate2, b_gate2, head_norm_g):
    nc = _get_prog("gla", build_gla_phase)
    lblk, ublk = _gla_consts()
    xTs = [np.ascontiguousarray(x[b].T) for b in range(BATCH)]
    in_maps = []
    for core in range(NCORES):
        b, h = core // 4, core % 4
        wall = np.concatenate([
            w_in[:, h * 128:(h + 1) * 128], w_in[:, 512 + h * 128:512 + (h + 1) * 128],
            w_in[:, 1024 + h * 256:1024 + (h + 1) * 256], w_in[:, 2048 + h * 256:2048 + (h + 1) * 256],
            w_in[:, 3072:3088]], axis=1)
        in_maps.append({"xT": xTs[b], "wall": np.ascontiguousarray(wall),
                        "wg2": np.ascontiguousarray(w_gate2[:, h * 128:(h + 1) * 128]),
                        "bg2": np.ascontiguousarray(b_gate2[None, h * 128:(h + 1) * 128]),
                        "hng": np.ascontiguousarray(head_norm_g[h * 256:(h + 1) * 256]),
                        "lblk": lblk, "ublk": ublk})
    res = run_bass_kernel_spmd(nc, in_maps, core_ids=list(range(NCORES)))
    o = np.empty((BATCH, SEQ, 1024), np.float32)
    for core in range(NCORES):
        b, h = core // 4, core % 4
        o[b, :, h * 256:(h + 1) * 256] = res.results[core]["o"]
    return o


def kernel(**inputs):
    g = lambda k: np.ascontiguousarray(np.asarray(inputs[k], dtype=np.float32))
    x = g("x")
    NTOK = BATCH * SEQ
    nc = _get_prog("fused", build_fused)
    xf = x.reshape(NTOK, D_MODEL)
    xTs = [np.ascontiguousarray(x[b].T) for b in range(BATCH)]
    w_in0 = g("l0_w_in")
    dslc, dcmp, relslc, relcmp, ov = _nsa_consts()
    slopes = (2.0 ** (-8.0 * (np.arange(16, dtype=np.float32) + 1.0) / 16)).astype(np.float32)
    peT = np.ascontiguousarray(np.concatenate([g("l0_cmp_pe_k").T, g("l0_cmp_pe_v").T], axis=0))
    w1 = np.ascontiguousarray(np.stack([g("l0_cmp_wk1"), g("l0_cmp_wv1")]))
    wk2 = g("l0_cmp_wk2")
    w2k = np.ascontiguousarray(np.concatenate([wk2, wk2], axis=1))
    w2v = g("l0_cmp_wv2")
    w_in1 = g("l1_w_in")
    wg2 = g("l1_w_gate2"); bg2 = g("l1_b_gate2"); hng = g("l1_head_norm_g")
    lblk, ublk = _gla_consts()
    shared = {
        "a_w1": w1, "a_peT": peT, "a_w2k": w2k, "a_w2v": w2v, "a_ov": ov, "a_dslc": dslc, "a_dcmp": dcmp,
        "a_relslc": relslc, "a_relcmp": relcmp,
        "b_w_o": g("l0_w_o"), "b_ln1_g": g("l0_ln1_g"), "b_ln1_b": g("l0_ln1_b"), "b_ln2_g": g("l0_ln2_g"),
        "b_ln2_b": g("l0_ln2_b"), "b_w_gu": g("l0_ffn_w_gu")[None], "b_w_dn": g("l0_ffn_w_down")[None],
        "c_lblk": lblk, "c_ublk": ublk,
        "d_w_o": g("l1_w_o"), "d_ln1_g": g("l1_ln1_g"), "d_ln1_b": g("l1_ln1_b"), "d_ln2_g": g("l1_ln2_g"),
        "d_ln2_b": g("l1_ln2_b"), "d_w_gu": g("l1_moe_w_gu"), "d_w_dn": g("l1_moe_w_down"), "d_w_r": g("l1_router"),
    }
    in_maps = []
    for core in range(NCORES):
        b, r = core // 4, core % 4
        kvcol = lambda i: w_in0[:, 1024 + i * 256 + r * 64:1024 + i * 256 + (r + 1) * 64]
        wall_a = np.concatenate([
            w_in0[:, r * 256:(r + 1) * 256], kvcol(0), kvcol(1), kvcol(2), kvcol(2), kvcol(4), kvcol(4),
            kvcol(3), kvcol(5), w_in0[:, 2560 + r * 12:2560 + (r + 1) * 12]], axis=1)
        nsl = np.ascontiguousarray(np.broadcast_to(-slopes[r * 4:(r + 1) * 4][None, :], (128, 4)).astype(np.float32))
        wall_c = np.concatenate([
            w_in1[:, r * 128:(r + 1) * 128], w_in1[:, 512 + r * 128:512 + (r + 1) * 128],
            w_in1[:, 1024 + r * 256:1024 + (r + 1) * 256], w_in1[:, 2048 + r * 256:2048 + (r + 1) * 256],
            w_in1[:, 3072:3088]], axis=1)
        sel4 = np.zeros((128, 4), np.float32)
        sel4[:, r] = 1.0
        m = dict(shared)
        m.update({
            "a_xT": xTs[b], "a_wall": np.ascontiguousarray(wall_a), "a_nslope": nsl,
            "b_sel4": sel4, "b_xres": np.ascontiguousarray(xf[core * NT:(core + 1) * NT]),
            "c_wall": np.ascontiguousarray(wall_c), "c_wg2": np.ascontiguousarray(wg2[:, r * 128:(r + 1) * 128]),
            "c_bg2": np.ascontiguousarray(bg2[None, r * 128:(r + 1) * 128]),
            "c_hng": np.ascontiguousarray(hng[r * 256:(r + 1) * 256]),
            "d_sel4": sel4,
        })
        in_maps.append(m)
    res = run_bass_kernel_spmd(nc, in_maps, core_ids=list(range(NCORES)))
    return np.concatenate([r_["y"] for r_ in res.results], axis=0).reshape(BATCH, SEQ, D_MODEL).astype(np.float32)
```
